# Optimizing a Trainium2 kernel written in Bass

```python
import jax, jax.numpy as jnp
from jax import lax
import numpy as np

D_MODEL = 1024
BATCH = 32
SEQ = 256
DEPTH = 4
DEC_BATCH = 8
DEC_SEQ = 2048
PAST_LEN = 512

GRID_W = 64
N_MIXERS = 4
EPS = 1e-6
Q_BLOCK = 128
ROPE_THETA = 10000.0
POOL_WINDOWS = (2, 4, 8, 16)
POOL_GROUPS = 4
POOL_GW = D_MODEL // POOL_GROUPS
MLA_HEADS = 8
MLA_Q_LORA = 3 * D_MODEL // 8
MLA_KV_LORA = D_MODEL // 4
MLA_NOPE = 128
MLA_ROPE = 64
MLA_V = 128
MLA_SCALE = (MLA_NOPE + MLA_ROPE) ** -0.5
CONV_WIDTH = 31
GQA_HEADS = 8
GQA_KV_HEADS = 2
GQA_HEAD_DIM = 128
GQA_SCALE = GQA_HEAD_DIM ** -0.5
N_EXPERTS = 16
EXPERT_FF = D_MODEL // 2
EC_CAPACITY = 2
N_POOL = len(range(0, DEPTH, N_MIXERS))
N_MLA = len(range(1, DEPTH, N_MIXERS))
N_CONV = len(range(2, DEPTH, N_MIXERS))
N_GQA = len(range(3, DEPTH, N_MIXERS))

kernel_name = 'hybrid_diffusion_ctx_prefix_step'


def _rms(x, g):
    xf = x.astype(jnp.float32)
    y = xf * lax.rsqrt(jnp.mean(xf * xf, axis=-1, keepdims=True) + EPS)
    return (y * g.astype(jnp.float32)).astype(x.dtype)


def _modulate(x, g, shift, scale):
    return _rms(x, g) * (1 + scale[:, None, :]) + shift[:, None, :]


def _adaln(cond, w, b):
    return jnp.split(jax.nn.silu(cond) @ w + b, 6, axis=-1)


def _axial_rope_tables(n_tokens, rot_dim):
    rows = n_tokens // GRID_W
    row = jnp.repeat(jnp.arange(rows), GRID_W).astype(jnp.float32)
    col = jnp.tile(jnp.arange(GRID_W), rows).astype(jnp.float32)
    axis_dim = rot_dim // 2
    inv = ROPE_THETA ** (-jnp.arange(0, axis_dim, 2, dtype=jnp.float32) / axis_dim)
    ang = jnp.concatenate([row[:, None] * inv, col[:, None] * inv], axis=-1)
    return jnp.cos(ang), jnp.sin(ang)


def _rope(x, cos, sin):
    xf = x.astype(jnp.float32)
    half = x.shape[-1] // 2
    x1, x2 = xf[..., :half], xf[..., half:]
    cb, sb = cos[None, :, None, :], sin[None, :, None, :]
    return jnp.concatenate([x1 * cb - x2 * sb, x1 * sb + x2 * cb], axis=-1).astype(x.dtype)


def _attention(q_ctx, k_ctx, v_ctx, scale, q_lat=None, k_lat=None, v_lat=None):
    B, S, Hkv, G, _ = q_ctx.shape
    nb = S // Q_BLOCK

    def to_blocks(q):
        return jnp.moveaxis(q.reshape(B, nb, Q_BLOCK, *q.shape[2:]), 1, 0)

    if q_lat is None:
        qs, keys, v = (to_blocks(q_ctx),), (k_ctx,), v_ctx
    else:
        qs, keys = (to_blocks(q_ctx), to_blocks(q_lat)), (k_ctx, k_lat)
        v = jnp.concatenate([v_ctx, v_lat], axis=1)

    def block(qb):
        s = jnp.concatenate(
            [jnp.einsum('bqhgd,bkhd->bhgqk', qi, ki).astype(jnp.float32) for qi, ki in zip(qb, keys)],
            axis=-1) * scale
        p = jax.nn.softmax(s, axis=-1).astype(v.dtype)
        return jnp.einsum('bhgqk,bkhd->bqhgd', p, v)

    o = lax.map(block, qs)
    return jnp.moveaxis(o, 0, 1).reshape(B, S, -1)


def _pool_mixer(h, w, scale):
    B, S, _ = h.shape
    hf = h.astype(jnp.float32)
    cs = jnp.concatenate([jnp.zeros((B, 1, D_MODEL), jnp.float32), jnp.cumsum(hf, axis=1)], axis=1)
    t = jnp.arange(S)
    groups = []
    for gi, win in enumerate(POOL_WINDOWS):
        lo = jnp.clip(t - win // 2, 0, S)
        hi = jnp.clip(t + win // 2, 0, S)
        sl = slice(gi * POOL_GW, (gi + 1) * POOL_GW)
        csg = cs[:, :, sl]
        mean = (csg[:, hi] - csg[:, lo]) / (hi - lo).astype(jnp.float32)[None, :, None]
        groups.append(mean - hf[:, :, sl])
    pooled = jnp.stack(groups, axis=2).astype(h.dtype)
    y = jnp.einsum('bsgc,gcd->bsgd', pooled, w).reshape(B, S, D_MODEL)
    return y * scale


def _mla_project(h, w_down, g_q, w_uq, g_kv):
    B, S, _ = h.shape
    d = h @ w_down
    cq, ckv, krope = jnp.split(d, [MLA_Q_LORA, MLA_Q_LORA + MLA_KV_LORA], axis=-1)
    q = (_rms(cq, g_q) @ w_uq).reshape(B, S, MLA_HEADS, MLA_NOPE + MLA_ROPE)
    return q[..., :MLA_NOPE], q[..., MLA_NOPE:], _rms(ckv, g_kv), krope


def _mla_expand(ckv, krope, w_ukv):
    B, L, _ = ckv.shape
    kv = (ckv @ w_ukv).reshape(B, L, MLA_HEADS, MLA_NOPE + MLA_V)
    k = jnp.concatenate(
        [kv[..., :MLA_NOPE], jnp.broadcast_to(krope[:, :, None, :], (B, L, MLA_HEADS, MLA_ROPE))], axis=-1)
    return k, kv[..., MLA_NOPE:]


def _mla_context(h, w_down, g_q, w_uq, g_kv, w_ukv, w_o):
    qn, qr, ckv, kr = _mla_project(h, w_down, g_q, w_uq, g_kv)
    k, v = _mla_expand(ckv, kr, w_ukv)
    q = jnp.concatenate([qn, qr], axis=-1)[:, :, :, None, :]
    return _attention(q, k, v, MLA_SCALE) @ w_o, ckv, kr


def _mla_latent(h, ctx_ckv, ctx_kr, w_down, g_q, w_uq, g_kv, w_ukv, w_o):
    qn, qr, ckv, kr = _mla_project(h, w_down, g_q, w_uq, g_kv)
    cos, sin = _axial_rope_tables(h.shape[1], MLA_ROPE)
    q_lat = jnp.concatenate([qn, _rope(qr, cos, sin)], axis=-1)[:, :, :, None, :]
    q_ctx = jnp.concatenate([qn, qr], axis=-1)[:, :, :, None, :]
    k_lat, v_lat = _mla_expand(ckv, _rope(kr[:, :, None, :], cos, sin)[:, :, 0], w_ukv)
    k_ctx, v_ctx = _mla_expand(ctx_ckv, ctx_kr, w_ukv)
    return _attention(q_ctx, k_ctx, v_ctx, MLA_SCALE, q_lat, k_lat, v_lat) @ w_o


def _conv_module(h, w_pw1, b_pw1, w_dw, b_dw, ln_g, ln_b, w_pw2, b_pw2):
    u = h @ w_pw1 + b_pw1
    u = u[..., :D_MODEL] * jax.nn.sigmoid(u[..., D_MODEL:])
    u = lax.conv_general_dilated(
        u, w_dw[:, None, :], window_strides=(1,),
        padding=[(CONV_WIDTH // 2, CONV_WIDTH // 2)],
        dimension_numbers=('NWC', 'WIO', 'NWC'), feature_group_count=D_MODEL) + b_dw
    uf = u.astype(jnp.float32)
    mu = jnp.mean(uf, axis=-1, keepdims=True)
    var = jnp.mean(jnp.square(uf - mu), axis=-1, keepdims=True)
    u = ((uf - mu) * lax.rsqrt(var + EPS) * ln_g + ln_b).astype(h.dtype)
    return jax.nn.silu(u) @ w_pw2 + b_pw2


def _gqa_project(h, w_qkv, g_q, g_k):
    B, S, _ = h.shape
    qkv = (h @ w_qkv).reshape(B, S, GQA_HEADS + 2 * GQA_KV_HEADS, GQA_HEAD_DIM)
    q, k, v = jnp.split(qkv, [GQA_HEADS, GQA_HEADS + GQA_KV_HEADS], axis=2)
    return _rms(q, g_q), _rms(k, g_k), v


def _group(q):
    B, S, H, d = q.shape
    return q.reshape(B, S, GQA_KV_HEADS, H // GQA_KV_HEADS, d)


def _gqa_context(h, w_qkv, g_q, g_k, w_o):
    q, k, v = _gqa_project(h, w_qkv, g_q, g_k)
    return _attention(_group(q), k, v, GQA_SCALE) @ w_o, k, v


def _gqa_latent(h, ctx_k, ctx_v, w_qkv, g_q, g_k, w_o):
    q, k, v = _gqa_project(h, w_qkv, g_q, g_k)
    cos, sin = _axial_rope_tables(h.shape[1], GQA_HEAD_DIM)
    o = _attention(_group(q), ctx_k, ctx_v, GQA_SCALE, _group(_rope(q, cos, sin)), _rope(k, cos, sin), v)
    return o @ w_o


def _expert_choice(h, router_w, w_gate, w_up, w_down):
    B, S, _ = h.shape
    cap = EC_CAPACITY * S // N_EXPERTS
    aff = jax.nn.softmax((h @ router_w).astype(jnp.float32), axis=-1)
    gates, idx = lax.top_k(jnp.swapaxes(aff, 1, 2), cap)
    xg = jax.vmap(lambda xb, ib: xb[ib])(h, idx)
    hid = jax.nn.silu(jnp.einsum('becd,edf->becf', xg, w_gate)) * jnp.einsum('becd,edf->becf', xg, w_up)
    yo = jnp.einsum('becf,efd->becd', hid, w_down) * gates[..., None].astype(h.dtype)
    return jax.vmap(lambda ib, vb: jnp.zeros((S, D_MODEL), vb.dtype).at[ib.reshape(-1)].add(
        vb.reshape(-1, D_MODEL)))(idx, yo)


def setup_inputs(seed: int = 0) -> dict:
    key = jax.random.key(seed)
    ks = iter(jax.random.split(key, 40))
    D = D_MODEL

    def nrm(shape, scale=1.0):
        return jax.random.normal(next(ks), shape, jnp.float32) * scale

    def gain(shape):
        return 1.0 + 0.05 * nrm(shape)

    return {
        'x_prompt': nrm((BATCH, SEQ, D)),
        'x_sample': nrm((DEC_BATCH, DEC_SEQ, D)),
        'c': nrm((DEC_BATCH, D)),
        'cache_mla_ckv': nrm((DEC_BATCH, N_MLA, PAST_LEN, MLA_KV_LORA)),
        'cache_mla_krope': nrm((DEC_BATCH, N_MLA, PAST_LEN, MLA_ROPE)),
        'cache_gqa_k': nrm((DEC_BATCH, N_GQA, PAST_LEN, GQA_KV_HEADS, GQA_HEAD_DIM)),
        'cache_gqa_v': nrm((DEC_BATCH, N_GQA, PAST_LEN, GQA_KV_HEADS, GQA_HEAD_DIM)),
        'c_ctx': nrm((D,)),
        'ada_w': nrm((DEPTH, D, 6 * D), 0.3 * D ** -0.5),
        'ada_b': nrm((DEPTH, 6 * D), 0.02),
        'norm1_g': gain((DEPTH, D)),
        'norm2_g': gain((DEPTH, D)),
        'final_g': gain((D,)),
        'pool_w': nrm((N_POOL, POOL_GROUPS, POOL_GW, POOL_GW), POOL_GW ** -0.5),
        'pool_scale': gain((N_POOL, D)),
        'mla_w_down': nrm((N_MLA, D, MLA_Q_LORA + MLA_KV_LORA + MLA_ROPE), D ** -0.5),
        'mla_g_q': gain((N_MLA, MLA_Q_LORA)),
        'mla_w_uq': nrm((N_MLA, MLA_Q_LORA, MLA_HEADS * (MLA_NOPE + MLA_ROPE)), MLA_Q_LORA ** -0.5),
        'mla_g_kv': gain((N_MLA, MLA_KV_LORA)),
        'mla_w_ukv': nrm((N_MLA, MLA_KV_LORA, MLA_HEADS * (MLA_NOPE + MLA_V)), MLA_KV_LORA ** -0.5),
        'mla_w_o': nrm((N_MLA, MLA_HEADS * MLA_V, D), (MLA_HEADS * MLA_V) ** -0.5),
        'conv_w_pw1': nrm((N_CONV, D, 2 * D), D ** -0.5),
        'conv_b_pw1': nrm((N_CONV, 2 * D), 0.02),
        'conv_w_dw': nrm((N_CONV, CONV_WIDTH, D), CONV_WIDTH ** -0.5),
        'conv_b_dw': nrm((N_CONV, D), 0.02),
        'conv_ln_g': gain((N_CONV, D)),
        'conv_ln_b': nrm((N_CONV, D), 0.02),
        'conv_w_pw2': nrm((N_CONV, D, D), D ** -0.5),
        'conv_b_pw2': nrm((N_CONV, D), 0.02),
        'gqa_w_qkv': nrm((N_GQA, D, (GQA_HEADS + 2 * GQA_KV_HEADS) * GQA_HEAD_DIM), D ** -0.5),
        'gqa_g_q': gain((N_GQA, GQA_HEAD_DIM)),
        'gqa_g_k': gain((N_GQA, GQA_HEAD_DIM)),
        'gqa_w_o': nrm((N_GQA, GQA_HEADS * GQA_HEAD_DIM, D), (GQA_HEADS * GQA_HEAD_DIM) ** -0.5),
        'router_w': nrm((DEPTH, D, N_EXPERTS), D ** -0.5),
        'moe_w_gate': nrm((DEPTH, N_EXPERTS, D, EXPERT_FF), D ** -0.5),
        'moe_w_up': nrm((DEPTH, N_EXPERTS, D, EXPERT_FF), D ** -0.5),
        'moe_w_down': nrm((DEPTH, N_EXPERTS, EXPERT_FF, D), EXPERT_FF ** -0.5),
    }


def reference(x_prompt, x_sample, c, cache_mla_ckv, cache_mla_krope, cache_gqa_k, cache_gqa_v, c_ctx,
              ada_w, ada_b, norm1_g, norm2_g, final_g, pool_w, pool_scale,
              mla_w_down, mla_g_q, mla_w_uq, mla_g_kv, mla_w_ukv, mla_w_o,
              conv_w_pw1, conv_b_pw1, conv_w_dw, conv_b_dw, conv_ln_g, conv_ln_b, conv_w_pw2, conv_b_pw2,
              gqa_w_qkv, gqa_g_q, gqa_g_k, gqa_w_o,
              router_w, moe_w_gate, moe_w_up, moe_w_down):
    xc, xl = x_prompt, x_sample
    new_ckv, new_kr, new_k, new_v = [], [], [], []
    for l in range(DEPTH):
        kind, j = l % N_MIXERS, l // N_MIXERS
        mc = _adaln(c_ctx[None, :], ada_w[l], ada_b[l])
        ml = _adaln(c, ada_w[l], ada_b[l])
        hc = _modulate(xc, norm1_g[l], mc[0], mc[1])
        hl = _modulate(xl, norm1_g[l], ml[0], ml[1])
        if kind == 0:
            oc = _pool_mixer(hc, pool_w[j], pool_scale[j])
            ol = _pool_mixer(hl, pool_w[j], pool_scale[j])
        elif kind == 1:
            mla_p = (mla_w_down[j], mla_g_q[j], mla_w_uq[j], mla_g_kv[j], mla_w_ukv[j], mla_w_o[j])
            oc, ckv, kr = _mla_context(hc, *mla_p)
            ol = _mla_latent(hl, cache_mla_ckv[:, j], cache_mla_krope[:, j], *mla_p)
            new_ckv.append(ckv)
            new_kr.append(kr)
        elif kind == 2:
            conv_p = (conv_w_pw1[j], conv_b_pw1[j], conv_w_dw[j], conv_b_dw[j],
                      conv_ln_g[j], conv_ln_b[j], conv_w_pw2[j], conv_b_pw2[j])
            oc = _conv_module(hc, *conv_p)
            ol = _conv_module(hl, *conv_p)
        else:
            gqa_p = (gqa_w_qkv[j], gqa_g_q[j], gqa_g_k[j], gqa_w_o[j])
            oc, k, v = _gqa_context(hc, *gqa_p)
            ol = _gqa_latent(hl, cache_gqa_k[:, j], cache_gqa_v[:, j], *gqa_p)
            new_k.append(k)
            new_v.append(v)
        xc = xc + mc[2][:, None, :] * oc
        xl = xl + ml[2][:, None, :] * ol
        moe_p = (router_w[l], moe_w_gate[l], moe_w_up[l], moe_w_down[l])
        xc = xc + mc[5][:, None, :] * _expert_choice(_modulate(xc, norm2_g[l], mc[3], mc[4]), *moe_p)
        xl = xl + ml[5][:, None, :] * _expert_choice(_modulate(xl, norm2_g[l], ml[3], ml[4]), *moe_p)
    y_prompt = _rms(xc, final_g)
    y_sample = _rms(xl, final_g)
    state_mla_ckv = jnp.stack(new_ckv, axis=1)
    state_mla_krope = jnp.stack(new_kr, axis=1)
    state_gqa_k = jnp.stack(new_k, axis=1)
    state_gqa_v = jnp.stack(new_v, axis=1)
    return (y_prompt, y_sample, state_mla_ckv, state_mla_krope, state_gqa_k, state_gqa_v)
```

```python
import contextlib
import numpy as np
import concourse.bass as bass
import concourse.mybir as mybir
from concourse.bass_utils import run_bass_kernel_spmd

F32 = mybir.dt.float32
BF16 = mybir.dt.bfloat16
AF = mybir.ActivationFunctionType
ALU = mybir.AluOpType
AX = mybir.AxisListType

D = 1024
KC = 8
NE = 16
FF = 512
EPS = 1e-6
NCORES = 8


class Res:
    __slots__ = ("name", "w", "r", "excl")

    def __init__(self, name, excl=False):
        self.name = name
        self.w = None
        self.r = {}
        self.excl = excl


class Eng:
    def __init__(self, name, eng, sem):
        self.name = name
        self.eng = eng
        self.sem = sem
        self.count = 0
        self.waited = {}


class KB:
    def __init__(self, nc):
        self.nc = nc
        self.stack = contextlib.ExitStack()
        self.sems = {}
        self.dma_tot = {}
        self.E = {}
        for name, e in (("pe", nc.tensor), ("act", nc.scalar), ("dve", nc.vector),
                        ("pool", nc.gpsimd), ("sp", nc.sync)):
            s = self.stack.enter_context(nc.semaphore("s_" + name))
            self.sems["s_" + name] = s
            self.E[name] = Eng(name, e, "s_" + name)
        self.n_wait = 0
        self.n_ins = 0
        self.snaps = {}

    def sbuf(self, name, shape, dtype):
        return self.stack.enter_context(self.nc.sbuf_tensor(name, list(shape), dtype))

    def psum(self, name, shape, dtype=F32):
        return self.stack.enter_context(self.nc.psum_tensor(name, list(shape), dtype))

    def dsem(self, key):
        if key not in self.sems:
            self.sems[key] = self.stack.enter_context(self.nc.semaphore(key))
            self.dma_tot[key] = 0
        return key

    def _wait(self, E, key, val):
        if key in self.dma_tot:
            val = max(val, self.dma_tot[key])
        if E.waited.get(key, 0) >= val:
            return
        E.eng.wait_ge(self.sems[key], val)
        E.waited[key] = val
        self.n_wait += 1
        snap = self.snaps.get((key, val))
        if snap:
            for k2, v2 in snap.items():
                if E.waited.get(k2, 0) < v2:
                    E.waited[k2] = v2

    def _dep(self, E, tok):
        key, val = tok
        if E.name == "pe" and key == "s_pe":
            return
        self._wait(E, key, val)

    def _sync(self, E, reads, writes):
        for res in reads:
            if res.w is not None:
                self._dep(E, res.w)
            if res.excl:
                for k, v in res.r.items():
                    if k != E.sem:
                        self._dep(E, (k, v))
        for res in writes:
            if res.w is not None and res.w[0] != E.sem:
                self._dep(E, res.w)
            for k, v in res.r.items():
                if k != E.sem:
                    self._dep(E, (k, v))

    def _mark(self, tok, reads, writes):
        key, val = tok
        for res in reads:
            if res.r.get(key, 0) < val:
                res.r[key] = val
        for res in writes:
            res.w = tok
            res.r = {}

    def op(self, ename, fn, reads=(), writes=()):
        E = self.E[ename]
        self._sync(E, reads, writes)
        ins = fn(E.eng)
        E.count += 1
        ins.then_inc(self.sems[E.sem], 1)
        self.snaps[(E.sem, E.count)] = dict(E.waited)
        self._mark((E.sem, E.count), reads, writes)
        self.n_ins += 1
        return ins

    def dma(self, qname, pairs, sem_key, reads=(), writes=(), **kw):
        E = self.E[qname]
        self.dsem(sem_key)
        self._sync(E, reads, writes)
        for (o, i) in pairs:
            ins = E.eng.dma_start(out=o, in_=i, **kw)
            ins.then_inc(self.sems[sem_key], 16)
            self.dma_tot[sem_key] += 16
            self.n_ins += 1
        tok = (sem_key, self.dma_tot[sem_key])
        self._mark(tok, reads, writes)
        return tok

    def barrier(self):
        for E in self.E.values():
            for F in self.E.values():
                if F is not E and F.count:
                    self._wait(E, F.sem, F.count)
            for k, tot in self.dma_tot.items():
                if tot:
                    self._wait(E, k, tot)

    def close(self):
        self.stack.close()


class Arena:
    def __init__(self, kb, nbytes):
        self.kb = kb
        self.n = nbytes
        self.t = kb.sbuf("arena", [128, nbytes // 4], F32)
        self.off = 0
        self.top = nbytes

    def reset(self):
        self.kb.barrier()
        self.off = 0
        self.top = self.n

    def release_top(self):
        self.kb.barrier()
        self.top = self.n

    def mark(self):
        return self.off

    def release(self, mark):
        self.kb.barrier()
        self.off = mark

    def alloc(self, name, shape, dtype, parts=128, top=False):
        esz = 2 if dtype == BF16 else 4
        n = int(np.prod(shape))
        nb = (n * esz + 31) // 32 * 32
        assert self.off + nb <= self.top, f"arena overflow at {name}: {self.off}+{nb}>{self.top}"
        if top:
            self.top -= nb
            start = self.top
        else:
            start = self.off
            self.off += nb
        v = self.t[0:parts, start // 4:(start + nb) // 4]
        if dtype != F32:
            v = v.bitcast(dtype)
        v = v[:, 0:n]
        if len(shape) == 2:
            v = v.rearrange("p (a b) -> p a b", a=shape[0])
        elif len(shape) == 3:
            v = v.rearrange("p (a b c) -> p a b c", a=shape[0], b=shape[1])
        return v, Res(name)


class Grp:
    def __init__(self, name, T, nseq, S, ci):
        self.name = name
        self.T = T
        self.nseq = nseq
        self.S = S
        self.ci = ci
        self.NT = T // 128
        self.NB = T // 512
        self.C = S // 8
        self.NSLOT = nseq * self.C
        self.NCC = self.NSLOT // 128
        self.lat = (ci == 0)


class Prog:
    def __init__(self, depth=4, do_moe=True, do_mixer=True, debug=False, groups="LC", stop=""):
        self.depth = depth
        self.groups = groups
        self.stop = stop
        self.debug = debug
        self.do_moe = do_moe
        self.do_mixer = do_mixer
        nc = self.nc = bass.Bass("TRN2", target_bir_lowering=False)
        self.kb = kb = KB(nc)
        self.din = {}
        self.dout = {}

    def inp(self, name, shape, dt=F32):
        ap = self.nc.dram_tensor(name, list(shape), dt, kind="ExternalInput").ap()
        self.din[name] = ap
        return ap

    def outp(self, name, shape):
        ap = self.nc.dram_tensor(name, list(shape), F32, kind="ExternalOutput").ap()
        self.dout[name] = ap
        return ap

    def dbg(self, name, ap, res):
        if not getattr(self, "debug", False) or name in self.dout:
            return
        shp = list(ap.shape)
        d = self.nc.dram_tensor("dbg_" + name, shp, ap.dtype, kind="ExternalOutput").ap()
        self.dout[name] = d
        self.kb.dma("sp", [(d, ap)], "d_dbg", reads=[res])

    def next_ps(self, pin=False):
        i = self.ps_i
        while i in self.ps_pinned:
            i = (i + 1) % 8
        self.ps_i = (i + 1) % 8
        if pin:
            self.ps_pinned.add(i)
        return self.ps[i], self.psr[i]

    def unpin(self, psr):
        self.ps_pinned.discard(self.psr.index(psr))

    def mm(self, out, lhsT, rhs, start, stop, reads, writes):
        self.kb.op("pe", lambda e: e.matmul(out, lhsT, rhs, start=start, stop=stop), reads=reads, writes=writes)

    def tr(self, out, in_, ident, reads, writes):
        self.kb.op("pe", lambda e: e.transpose(out, in_, ident), reads=reads, writes=writes)

    def act(self, out, in_, func, reads, writes, bias=None, scale=1.0, accum_out=None):
        kw = {}
        if bias is not None:
            kw["bias"] = bias
        if accum_out is not None:
            kw["accum_out"] = accum_out
        self.kb.op("act", lambda e: e.activation(out=out, in_=in_, func=func, scale=scale, **kw),
                   reads=reads, writes=writes)

    def tt(self, out, in0, in1, op, reads, writes, eng="dve"):
        self.kb.op(eng, lambda e: e.tensor_tensor(out=out, in0=in0, in1=in1, op=op), reads=reads, writes=writes)

    def ts(self, out, in0, s1, s2, op0, op1, reads, writes, eng="dve", accum_out=None):
        if s2 is None:
            self.kb.op(eng, lambda e: e.tensor_scalar(out=out, in0=in0, scalar1=s1, scalar2=None, op0=op0),
                       reads=reads, writes=writes)
        else:
            self.kb.op(eng, lambda e: e.tensor_scalar(out=out, in0=in0, scalar1=s1, scalar2=s2, op0=op0, op1=op1),
                       reads=reads, writes=writes)

    def stt(self, out, in0, scalar, in1, op0, op1, reads, writes):
        self.kb.op("dve", lambda e: e.scalar_tensor_tensor(out=out, in0=in0, scalar=scalar, in1=in1, op0=op0, op1=op1),
                   reads=reads, writes=writes)

    def cp(self, eng, out, in_, reads, writes):
        if eng == "act":
            self.kb.op("act", lambda e: e.copy(out=out, in_=in_), reads=reads, writes=writes)
        else:
            self.kb.op(eng, lambda e: e.tensor_copy(out=out, in_=in_), reads=reads, writes=writes)

    def load(self, out, in_, res, sem="d_ld", q="sp", **kw):
        self.kb.dma(q, [(out, in_)], sem, writes=[res], **kw)

    def loadw(self, out, in_, res, sem):
        n = out.shape[-1]
        pairs = []
        if n <= 1024:
            self.kb.dma("pool", [(out, in_)], sem, writes=[res])
            return
        for c0 in range(0, n, 1024):
            c1 = min(n, c0 + 1024)
            if len(out.shape) == 3:
                pairs.append((out[:, :, c0:c1], in_[:, :, c0:c1]))
            else:
                pairs.append((out[:, c0:c1], in_[:, c0:c1]))
        self.kb.dma("pool", pairs, sem, writes=[res])

    def rstd_from(self, out, in_, n, reads, writes, tmp, tmp_r):
        P = out.shape[0]
        self.act(tmp, in_, AF.Ln, reads=list(reads) + [self.c_r], writes=[tmp_r], bias=self.eps_t[0:P, :], scale=1.0 / n)
        self.act(out, tmp, AF.Exp, reads=[tmp_r], writes=writes, scale=-0.5)

    def build(self):
        nc, kb = self.nc, self.kb
        L = 4
        inp, outp = self.inp, self.outp
        xl_d = inp("xl", [128, KC, 2048])
        xc_d = inp("xc", [128, KC, 1024])
        cond_d = inp("cond", [128, KC, 2])
        ada_w_d = inp("ada_w", [L, D, 6 * D])
        ada_b_d = inp("ada_b", [128, L, 48])
        g1_d = inp("g1", [128, L, KC])
        g2_d = inp("g2", [128, L, KC])
        gf_d = inp("gf", [128, KC])
        cst_d = inp("cst", [128, 128 + 128 + 256])
        pool_w_d = inp("pool_w", [4, 256, 256])
        pool_s_d = inp("pool_s", [128, KC])
        pool_rc_d = inp("pool_rc", [128, 4, 16])
        mla_wd_d = inp("mla_wd", [D, 704])
        mla_gq_d = inp("mla_gq", [128, 384])
        mla_wuq_d = inp("mla_wuq", [384, 2560])
        mla_gkv_d = inp("mla_gkv", [128, 256])
        mla_wukv_d = inp("mla_wukv", [256, 2048])
        mla_wo_d = inp("mla_wo", [D, D])
        mla_cckv_d = inp("mla_cckv", [512, 256])
        mla_ckr_d = inp("mla_ckr", [512, 64])
        mla_rtok_d = inp("mla_rtok", [128, 2, 16, 32])
        mla_rA_d = inp("mla_rA", [64, 2, 32])
        mla_rB_d = inp("mla_rB", [64, 2, 64])
        cv_w1_d = inp("cv_w1", [D, 2 * D])
        cv_b1_d = inp("cv_b1", [128, 16])
        cv_wdw_d = inp("cv_wdw", [128, KC, 31])
        cv_vec_d = inp("cv_vec", [128, 4, KC])
        cv_w2_d = inp("cv_w2", [D, D])
        gq_w_d = inp("gq_w", [D, 1536])
        gq_g_d = inp("gq_g", [128, 2, 128])
        gq_wo_d = inp("gq_wo", [D, D])
        gq_ck_d = inp("gq_ck", [512, 256])
        gq_cv_d = inp("gq_cv", [512, 256])
        gq_rtok_d = inp("gq_rtok", [128, 2, 16, 64])
        rw_d = inp("rw", [128, L, KC, NE])
        wg_d = inp("moe_wg", [L, NE, D, FF])
        wu_d = inp("moe_wu", [L, NE, D, FF])
        wd_d = inp("moe_wd", [L, NE, FF, D])
        wscr_d = self.nc.dram_tensor("wscr", [L, NE, 128, 12288], BF16, kind="Internal").ap()
        self.wscr_rs = [Res(f"wscr{i}") for i in range(L)]
        yl_d = outp("yl", [128, KC, 2048])
        yc_d = outp("yc", [128, KC, 1024])
        st_ckv_d = outp("st_ckv", [1024, 256])
        st_kr_d = outp("st_kr", [1024, 64])
        st_k_d = outp("st_k", [1024, 256])
        st_v_d = outp("st_v", [1024, 256])
        self.__dict__.update(locals())

        self.X = kb.sbuf("X", [128, KC, 2048], F32)
        self.Xr = [Res(f"X{b}") for b in range(4)]
        self.cst = kb.sbuf("cstt", [128, 512], F32)
        self.c_r = Res("cst")
        self.ident = self.cst[:, 0:128]
        self.onesf = self.cst[:, 128:256]
        self.iota = self.cst[:, 256:512]
        self.cb = kb.sbuf("cb", [128, 640], BF16)
        self.identb = self.cb[:, 0:128]
        self.onesb_s = self.cb[:, 128:256]
        self.onesb = self.cb[:, 256:384]
        self.iotab = self.cb[:, 384:640]
        self.eps_t = kb.sbuf("eps", [128, 1], F32)
        self.mods = kb.sbuf("mods", [128, L, 48, 2], F32)
        self.mod_rs = [Res(f"mods{i}") for i in range(L)]
        self.mod_r = self.mod_rs[0]
        self.cnd = kb.sbuf("cnd", [128, KC, 2], F32)[:, :, :]
        self.cndb = kb.sbuf("cndb", [128, KC, 2], BF16)[:, :, :]
        self.adab = kb.sbuf("adab", [128, L, 48], F32)[:, :, :]
        self.cnd_r = Res("cnd")
        self.gs = kb.sbuf("gs", [128, L, 2, KC, 2], F32)
        self.vecs = kb.sbuf("vecs", [128, 3 * L * KC + KC + KC], F32)
        self.g1 = self.vecs[:, 0:L * KC].rearrange("p (l m) -> p l m", l=L)
        self.g2 = self.vecs[:, L * KC:2 * L * KC].rearrange("p (l m) -> p l m", l=L)
        self.gf = self.vecs[:, 3 * L * KC:3 * L * KC + KC]
        self.ps = [kb.psum(f"ps{i}", [128, 512], F32) for i in range(8)]
        self.psr = [Res(f"ps{i}", excl=True) for i in range(8)]
        self.ps_i = 0
        self.ps_pinned = set()
        self.ar = Arena(kb, 136 * 1024)

        kb.dma("sp", [(self.cst[:], cst_d[:, :])], "d_c", writes=[self.c_r])
        kb.dma("sp", [(self.g1, g1_d[:, :, :]), (self.g2, g2_d[:, :, :]), (self.gf, gf_d[:, :])], "d_c", writes=[self.c_r])
        kb.op("dve", lambda e: e.memset(self.eps_t[:], EPS), writes=[self.c_r])
        kb.op("dve", lambda e: e.tensor_copy(out=self.identb, in_=self.ident), reads=[self.c_r], writes=[self.c_r])
        kb.op("dve", lambda e: e.tensor_copy(out=self.onesb_s, in_=self.onesf), reads=[self.c_r], writes=[self.c_r])
        kb.op("dve", lambda e: e.memset(self.onesb, 1.0), writes=[self.c_r])
        kb.op("dve", lambda e: e.tensor_copy(out=self.iotab, in_=self.iota), reads=[self.c_r], writes=[self.c_r])

        self.adaln_setup()
        for grp in (Grp("L", 2048, 1, 2048, 0), Grp("C", 1024, 4, 256, 1)):
            if grp.name in getattr(self, "groups", "LC"):
                self.run_group(grp)
        kb.barrier()

    def adaln_setup(self):
        self.load(self.cnd, self.cond_d[:, :, :], self.cnd_r)
        self.load(self.adab, self.ada_b_d[:, :, :], self.cnd_r)
        self.act(self.cndb, self.cnd, AF.Silu, reads=[self.cnd_r], writes=[self.cnd_r])

    def adaln_issue(self, l, wb):
        ps, psr = self.next_ps(pin=True)
        for jb in range(6):
            w, w_r = wb[jb % 2]
            self.loadw(w, self.ada_w_d[l, :, jb * 1024:(jb + 1) * 1024].rearrange("(k p) n -> p k n", p=128), w_r, f"d_a{jb % 2}")
            for jj in range(8):
                j = jb * 8 + jj
                for k in range(KC):
                    self.mm(ps[:, 2 * j:2 * j + 2], w[:, k, jj * 128:(jj + 1) * 128], self.cndb[:, k, :],
                            k == 0, k == KC - 1, reads=[w_r, self.cnd_r], writes=[psr])
        return ps, psr

    def adaln_finish(self, l, ps, psr):
        mr = self.mod_rs[l]
        self.tt(self.mods[:, l, :, :], ps[:, 0:96].rearrange("p (j c) -> p j c", c=2),
                self.adab[:, l, :].unsqueeze(2).broadcast_to([128, 48, 2]), ALU.add,
                reads=[psr, self.cnd_r], writes=[mr])
        self.unpin(psr)
        for which, g in ((0, self.g1), (1, self.g2)):
            j0 = 8 + 24 * which
            self.stt(self.gs[:, l, which, :, :], self.mods[:, l, j0:j0 + 8, :], 1.0,
                     g[:, l, :].unsqueeze(2).broadcast_to([128, KC, 2]), ALU.add, ALU.mult,
                     reads=[mr, self.c_r], writes=[mr])

    def shift(self, l, which, m, ci):
        return self.mods[:, l, 24 * which + m, ci:ci + 1]

    def gate(self, l, which, m, ci):
        return self.mods[:, l, 16 + 24 * which + m, ci:ci + 1]

    def norm_block(self, b, gs_fn, sh_fn, out_fn, out_r, tmps, precise=False):
        X, Xr = self.X, self.Xr
        sq, sq_r, lnv, lnv_r, rstd, rstd_r, tm, tm_r = tmps
        tb = slice(b * 512, (b + 1) * 512)
        ps, psr = self.next_ps()
        for m in range(KC):
            if precise:
                s_ = tm[m % 2]
                s_r = tm_r[m % 2]
                self.tt(s_, X[:, m, tb], X[:, m, tb], ALU.mult, reads=[Xr[b]], writes=[s_r])
                self.mm(ps[:, :], self.onesf, s_, m == 0, m == KC - 1, reads=[s_r, self.c_r], writes=[psr])
            else:
                self.tt(sq[m % 2], X[:, m, tb], X[:, m, tb], ALU.mult, reads=[Xr[b]], writes=[sq_r[m % 2]])
                self.mm(ps[:, :], self.onesb_s, sq[m % 2], m == 0, m == KC - 1, reads=[sq_r[m % 2], self.c_r], writes=[psr])
        self.act(lnv, ps[:, :], AF.Ln, reads=[psr, self.c_r], writes=[lnv_r], bias=self.eps_t[:, :])
        self.act(rstd, lnv, AF.Exp, reads=[lnv_r], writes=[rstd_r], scale=-0.5)
        self.tt(lnv, rstd, rstd, ALU.mult, reads=[rstd_r], writes=[lnv_r])
        self.stt(lnv, ps[:, :], EPS, lnv, ALU.add, ALU.mult, reads=[psr, lnv_r], writes=[lnv_r])
        self.ts(lnv, lnv, -0.5, 1.5, ALU.mult, ALU.add, reads=[lnv_r], writes=[lnv_r])
        self.tt(rstd, rstd, lnv, ALU.mult, reads=[rstd_r, lnv_r], writes=[rstd_r])
        for m in range(KC):
            self.tt(tm[m % 2], X[:, m, tb], rstd, ALU.mult, reads=[Xr[b], rstd_r], writes=[tm_r[m % 2]])
            if gs_fn is None:
                self.cp("act", out_fn(m), tm[m % 2], reads=[tm_r[m % 2]], writes=[out_r])
            else:
                self.act(out_fn(m), tm[m % 2], AF.Identity, reads=[tm_r[m % 2], self.mod_r], writes=[out_r],
                         bias=sh_fn(m), scale=gs_fn(m))

    def norm_tmps(self):
        ar = self.ar
        sq0, r0 = ar.alloc("sq0", [512], BF16)
        sq1, r1 = ar.alloc("sq1", [512], BF16)
        lnv, lr = ar.alloc("lnv", [512], F32)
        rstd, rr = ar.alloc("rstd", [512], F32)
        t0, tr0 = ar.alloc("tm0", [512], F32)
        t1, tr1 = ar.alloc("tm1", [512], F32)
        return ([sq0, sq1], [r0, r1], lnv, lr, rstd, rr, [t0, t1], [tr0, tr1])

    def norm1(self, g, l):
        ar = self.ar
        hT, hT_r = ar.alloc("hT", [KC, g.T], BF16)
        mk = ar.mark()
        tmps = self.norm_tmps()
        for b in range(g.NB):
            self.norm_block(b, lambda m: self.gs[:, l, 0, m, g.ci:g.ci + 1], lambda m: self.shift(l, 0, m, g.ci),
                            lambda m: hT[:, m, b * 512:(b + 1) * 512], hT_r, tmps)
        ar.release(mk)
        return hT, hT_r

    def resid(self, b, m, cols, ps_ap, psr, gate_ap, extra_reads=()):
        self.stt(self.X[:, m, cols], ps_ap, gate_ap, self.X[:, m, cols], ALU.mult, ALU.add,
                 reads=[psr, self.Xr[b], self.mod_r] + list(extra_reads), writes=[self.Xr[b]])

    def run_group(self, g):
        kb, ar = self.kb, self.ar
        ar.reset()
        src = self.xl_d if g.lat else self.xc_d
        for b in range(g.NB):
            kb.dma("sp", [(self.X[:, :, b * 512:(b + 1) * 512], src[:, :, b * 512:(b + 1) * 512])], "d_x", writes=[self.Xr[b]])
        for l in range(self.depth):
            ar.reset()
            self.mod_r = self.mod_rs[l]
            if g.lat and l == 0:
                wb = [ar.alloc(f"adaw{i}", [KC, 1024], BF16) for i in range(2)]
                self.adaln_finish(0, *self.adaln_issue(0, wb))
                ar.reset()
            kind = l % 4
            if not self.do_mixer:
                pass
            elif kind == 0:
                self.pool_mixer(g, l)
            elif kind == 1:
                self.mla(g, l)
            elif kind == 2:
                self.conv(g, l)
            else:
                self.gqa(g, l)
            ar.reset()
            if self.do_moe:
                self.moe(g, l)
        ar.reset()
        tmps = self.norm_tmps()
        yb = [ar.alloc(f"yb{i}", [KC, 512], F32) for i in range(2)]
        dst = self.yl_d if g.lat else self.yc_d
        for b in range(g.NB):
            y, y_r = yb[b % 2]
            self.norm_block(b, lambda m: self.gf[:, m:m + 1], lambda m: 0.0, lambda m: y[:, m, :], y_r, tmps)
            kb.dma("sp", [(dst[:, :, b * 512:(b + 1) * 512], y[:, :, :])], "d_y", reads=[y_r])

    def pool_mixer(self, g, l):
        kb, ar = self.kb, self.ar
        hT, hT_r = self.norm1(g, l)
        nseq, S = g.nseq, g.S
        W = S + 16
        pw, pw_r = ar.alloc("pw", [4, 2, 256], BF16)
        psc, psc_r = ar.alloc("psc", [KC], F32)
        prc, prc_r = ar.alloc("prc", [4, 16], F32)
        gsc, gsc_r = ar.alloc("gsc", [KC], F32)
        self.loadw(pw, self.pool_w_d.rearrange("g (k p) n -> p g k n", p=128), pw_r, "d_w0")
        self.load(psc, self.pool_s_d[:, :], psc_r)
        self.load(prc, self.pool_rc_d[:, :, :], prc_r)
        self.tt(gsc, self.mods[:, l, 16:24, g.ci], psc, ALU.mult, reads=[self.mod_r, psc_r], writes=[gsc_r])
        lv = [ar.alloc(f"lv{i}", [nseq, W], F32) for i in range(5)]
        pooled, pooled_r = ar.alloc("pooled", [2, nseq, S], BF16)
        tmpb, tmpb_r = ar.alloc("tmpb", [nseq, 16], F32)
        kb.op("dve", lambda e: e.memset(lv[0][0].rearrange("p a b -> p (a b)"), 0.0), writes=[lv[0][1]])
        for gi in range(4):
            win = 2 << gi
            for kk in range(2):
                m = 2 * gi + kk
                hv = hT[:, m, :].rearrange("p (s t) -> p s t", s=nseq)
                P0, P0r = lv[0]
                self.cp("act", P0[:, :, 8:8 + S], hv, reads=[hT_r], writes=[P0r])
                A, Ar = lv[1]
                self.tt(A[:, :, 1:W], P0[:, :, 1:W], P0[:, :, 0:W - 1], ALU.add, reads=[P0r], writes=[Ar])
                sh = 1
                lo, hi = 1, W
                for level in range(2, gi + 2):
                    Bv, Br = lv[level]
                    lo2, hi2 = lo + sh, hi - sh
                    self.tt(Bv[:, :, lo2:hi2], A[:, :, lo2 - sh:hi2 - sh], A[:, :, lo2 + sh:hi2 + sh], ALU.add,
                            reads=[Ar], writes=[Br])
                    A, Ar = Bv, Br
                    lo, hi = lo2, hi2
                    sh *= 2
                pv = pooled[:, kk, :, :]
                self.stt(pv, A[:, :, 8:8 + S], 1.0 / win, hv, ALU.mult, ALU.subtract, reads=[Ar, hT_r], writes=[pooled_r])
                for side, c0 in ((0, 0), (1, S - 8)):
                    self.tt(tmpb[:, :, 0:8], A[:, :, 8 + c0:16 + c0],
                            prc[:, gi, side * 8:side * 8 + 8].unsqueeze(1).broadcast_to([128, nseq, 8]), ALU.mult,
                            reads=[Ar, prc_r], writes=[tmpb_r])
                    self.tt(pv[:, :, c0:c0 + 8], tmpb[:, :, 0:8], hv[:, :, c0:c0 + 8], ALU.subtract,
                            reads=[tmpb_r, hT_r], writes=[pooled_r])
            pf = pooled.rearrange("p k s t -> p k (s t)")
            for b in range(g.NB):
                tb = slice(b * 512, (b + 1) * 512)
                for mo in range(2):
                    m = 2 * gi + mo
                    ps, psr = self.next_ps()
                    for k in range(2):
                        self.mm(ps[:, :], pw[:, gi, k, mo * 128:(mo + 1) * 128], pf[:, k, tb], k == 0, k == 1,
                                reads=[pw_r, pooled_r], writes=[psr])
                    self.resid(b, m, tb, ps[:, :], psr, gsc[:, m:m + 1], extra_reads=[gsc_r])

    def attn_jobs(self, jobs, scale, reads, ebuf, LA=2):
        E, E_r = ebuf
        nE = len(E)
        sps = [self.next_ps(pin=True) for _ in range(nE)]
        od = [(self.next_ps(pin=True), self.next_ps(pin=True)) for _ in range(2)]
        flat = [(j, i) for j, job in enumerate(jobs) for i in range(len(job[1]))]

        def issue_s(n):
            j, i = flat[n]
            NQ, chunks = jobs[j][0], jobs[j][1]
            xr = list(jobs[j][4]) if len(jobs[j]) > 4 else []
            ps, psr = sps[n % nE]
            kparts, _ = chunks[i]
            for pi, (kT, qT) in enumerate(kparts):
                self.mm(ps[:, 0:NQ], kT, qT, pi == 0, pi == len(kparts) - 1, reads=reads + xr, writes=[psr])

        for n in range(min(LA, len(flat))):
            issue_s(n)
        rd, rd_r = self.rden
        for n, (j, i) in enumerate(flat):
            if n + LA < len(flat):
                issue_s(n + LA)
            NQ, chunks, out_ap, out_r = jobs[j][0:4]
            nch = len(chunks)
            ps, psr = sps[n % nE]
            (ops, opr), (dps, dpr) = od[j % 2]
            self.act(E[n % nE][:, 0:NQ], ps[:, 0:NQ], AF.Exp, reads=[psr], writes=[E_r[n % nE]], scale=scale)
            _, v = chunks[i]
            self.mm(ops[:, 0:NQ], v, E[n % nE][:, 0:NQ], i == 0, i == nch - 1, reads=reads + [E_r[n % nE]], writes=[opr])
            self.mm(dps[:, 0:NQ], self.onesb, E[n % nE][:, 0:NQ], i == 0, i == nch - 1, reads=[E_r[n % nE], self.c_r], writes=[dpr])
            if i == nch - 1:
                self.kb.op("dve", lambda e: e.reciprocal(out=rd[:, 0:NQ], in_=dps[:, 0:NQ]), reads=[dpr], writes=[rd_r])
                self.tt(out_ap, ops[:, 0:NQ], rd[:, 0:NQ], ALU.mult, reads=[opr, rd_r], writes=[out_r])
        for (_, r) in sps:
            self.unpin(r)
        for (a, b) in od:
            self.unpin(a[1])
            self.unpin(b[1])

    def mla(self, g, l):
        kb, ar = self.kb, self.ar
        T, NT = g.T, g.NT
        hT, hT_r = self.norm1(g, l)
        NKC = 512 if g.lat else 0
        NK = NKC + T
        cqnT, cqnT_r = ar.alloc("cqnT", [3, T], BF16)
        ckvT, ckvT_r = ar.alloc("ckvT", [2, NK], BF16)
        krT, krT_r = ar.alloc("krT", [NK], BF16)
        kb.op("dve", lambda e: e.memset(krT, 0.0), writes=[krT_r])
        wuq, wuq_r = ar.alloc("wuq", [3, 2560], BF16)
        wukv, wukv_r = ar.alloc("wukv", [2, 2048], BF16)
        self.loadw(wuq, self.mla_wuq_d.rearrange("(k p) n -> p k n", p=128), wuq_r, "d_w1")
        self.loadw(wukv, self.mla_wukv_d.rearrange("(k p) n -> p k n", p=128), wukv_r, "d_w1")
        mkA = ar.mark()
        wd, wd_r = ar.alloc("wd", [KC, 704], BF16)
        gq, gq_r = ar.alloc("gq", [384], F32)
        gkv, gkv_r = ar.alloc("gkv", [256], F32)
        self.loadw(wd, self.mla_wd_d.rearrange("(k p) n -> p k n", p=128), wd_r, "d_w0")
        self.load(gq, self.mla_gq_d[:, :], gq_r)
        self.load(gkv, self.mla_gkv_d[:, :], gkv_r)
        _jk = [ar.alloc(f"junk{i}", [384], F32) for i in range(2)]
        _st = [ar.alloc(f"st{i}", [8], F32) for i in range(2)]
        cqn = [ar.alloc(f"cqn{i}", [384], BF16) for i in range(2)]
        ckn = [ar.alloc(f"ckn{i}", [256], F32) for i in range(2)]
        cknb = [ar.alloc(f"cknb{i}", [384], BF16) for i in range(2)]
        for i in range(2):
            kb.op("dve", lambda e: e.memset(cknb[i][0], 0.0), writes=[cknb[i][1]])
        krf = [ar.alloc(f"krf{i}", [64], F32) for i in range(2)]
        if self.stop == "A00":
            return
        if g.lat:
            rt, rt_r = ar.alloc("rt", [2, 16, 32], F32)
            self.load(rt, self.mla_rtok_d[:, :, :, :], rt_r)
            _rt2 = [ar.alloc(f"rtmp{i}", [4, 32], F32) for i in range(2)]
            cc, cc_r = ar.alloc("cc", [4, 384], BF16)
            kb.op("dve", lambda e: e.memset(cc.rearrange("p a b -> p (a b)"), 0.0), writes=[cc_r])
            self.loadw(cc[:, :, 0:256], self.mla_cckv_d.rearrange("(j p) c -> p j c", p=128), cc_r, "d_w2")
            self.loadw(cc[:, :, 256:320], self.mla_ckr_d.rearrange("(j p) c -> p j c", p=128), cc_r, "d_w2")
            if self.stop == "A01":
                return
            for j in range(4):
                ps, psr = self.next_ps()
                pb = ps[:, :].bitcast(BF16)
                for k in range(2):
                    self.tr(pb[:, k * 128:(k + 1) * 128], cc[:, j, k * 128:(k + 1) * 128], self.identb, reads=[cc_r, self.c_r], writes=[psr])
                self.tr(pb[:, 256:384], cc[:, j, 256:384], self.identb, reads=[cc_r, self.c_r], writes=[psr])
                self.cp("act", ckvT[:, :, j * 128:(j + 1) * 128], pb[:, 0:256].rearrange("p (k t) -> p k t", k=2), reads=[psr], writes=[ckvT_r])
                self.cp("act", krT[0:64, j * 128:(j + 1) * 128], pb[0:64, 256:384], reads=[psr], writes=[krT_r])
        if self.stop == "A0":
            return
        def tile_gen(tt_):
            tsl = slice(tt_ * 128, (tt_ + 1) * 128)
            junk, junk_r = _jk[tt_ % 2]
            st, st_r = _st[tt_ % 2]
            if g.lat:
                rtmp, rtmp_r = _rt2[tt_ % 2]
            p1, p1r = self.next_ps()
            p2, p2r = self.next_ps()
            for k in range(KC):
                self.mm(p1[:, 0:384], hT[:, k, tsl], wd[:, k, 0:384], k == 0, k == KC - 1, reads=[hT_r, wd_r], writes=[p1r])
            for k in range(KC):
                self.mm(p2[:, 0:320], hT[:, k, tsl], wd[:, k, 384:704], k == 0, k == KC - 1, reads=[hT_r, wd_r], writes=[p2r])
            self.act(junk[:, 0:384], p1[:, 0:384], AF.Square, reads=[p1r], writes=[junk_r, st_r], accum_out=st[:, 0:1])
            self.act(junk[:, 0:256], p2[:, 0:256], AF.Square, reads=[p2r], writes=[junk_r, st_r], accum_out=st[:, 1:2])
            self.act(st[:, 2:3], st[:, 0:1], AF.Ln, reads=[st_r, self.c_r], writes=[st_r], bias=self.eps_t[:, :], scale=1.0 / 384)
            self.act(st[:, 3:4], st[:, 1:2], AF.Ln, reads=[st_r, self.c_r], writes=[st_r], bias=self.eps_t[:, :], scale=1.0 / 256)
            self.act(st[:, 4:6], st[:, 2:4], AF.Exp, reads=[st_r], writes=[st_r], scale=-0.5)
            if self.stop == "A1":
                return
            cq, cq_r = cqn[tt_ % 2]
            ck, ck_r = ckn[tt_ % 2]
            ckb, ckb_r = cknb[tt_ % 2]
            kf, kf_r = krf[tt_ % 2]
            self.stt(cq, p1[:, 0:384], st[:, 4:5], gq, ALU.mult, ALU.mult, reads=[p1r, st_r, gq_r], writes=[cq_r])
            self.stt(ck, p2[:, 0:256], st[:, 5:6], gkv, ALU.mult, ALU.mult, reads=[p2r, st_r, gkv_r], writes=[ck_r])
            self.cp("act", ckb[:, 0:256], ck, reads=[ck_r], writes=[ckb_r])
            self.cp("act", kf, p2[:, 256:320], reads=[p2r], writes=[kf_r])
            if g.lat:
                c_, s_ = rt[:, 0, tt_, :], rt[:, 1, tt_, :]
                x1, x2 = kf[:, 0:32], kf[:, 32:64]
                self.tt(rtmp[:, 0, :], x1, c_, ALU.mult, reads=[kf_r, rt_r], writes=[rtmp_r])
                self.tt(rtmp[:, 1, :], x2, s_, ALU.mult, reads=[kf_r, rt_r], writes=[rtmp_r])
                self.tt(rtmp[:, 2, :], x1, s_, ALU.mult, reads=[kf_r, rt_r], writes=[rtmp_r])
                self.tt(rtmp[:, 3, :], x2, c_, ALU.mult, reads=[kf_r, rt_r], writes=[rtmp_r])
                self.tt(ckb[:, 256:288], rtmp[:, 0, :], rtmp[:, 1, :], ALU.subtract, reads=[rtmp_r], writes=[ckb_r])
                self.tt(ckb[:, 288:320], rtmp[:, 2, :], rtmp[:, 3, :], ALU.add, reads=[rtmp_r], writes=[ckb_r])
            else:
                self.cp("dve", ckb[:, 256:320], kf, reads=[kf_r], writes=[ckb_r])
                kb.dma("sp", [(self.st_ckv_d[tsl, :], ck)], "d_st", reads=[ck_r])
                kb.dma("sp", [(self.st_kr_d[tsl, :], kf)], "d_st", reads=[kf_r])
            yield
            if self.stop == "A2":
                return
            ps, psr = self.next_ps()
            pb = ps[:, :].bitcast(BF16)
            for k in range(3):
                self.tr(pb[:, k * 128:(k + 1) * 128], cq[:, k * 128:(k + 1) * 128], self.identb, reads=[cq_r, self.c_r], writes=[psr])
            for k in range(2):
                self.tr(pb[:, (3 + k) * 128:(4 + k) * 128], ckb[:, k * 128:(k + 1) * 128], self.identb, reads=[ckb_r, self.c_r], writes=[psr])
            self.tr(pb[:, 640:768], ckb[:, 256:384], self.identb, reads=[ckb_r, self.c_r], writes=[psr])
            ksl = slice(NKC + tt_ * 128, NKC + (tt_ + 1) * 128)
            self.cp("act", cqnT[:, :, tsl], pb[:, 0:384].rearrange("p (k t) -> p k t", k=3), reads=[psr], writes=[cqnT_r])
            self.cp("act", ckvT[:, :, ksl], pb[:, 384:640].rearrange("p (k t) -> p k t", k=2), reads=[psr], writes=[ckvT_r])
            self.cp("act", krT[0:64, ksl], pb[0:64, 640:768], reads=[psr], writes=[krT_r])
        _cur = tile_gen(0)
        next(_cur)
        for tt_ in range(NT):
            _nxt = None
            if tt_ + 1 < NT:
                _nxt = tile_gen(tt_ + 1)
                next(_nxt)
            for _ in _cur:
                pass
            _cur = _nxt
        if self.stop == "A":
            return
        ar.release(mkA)
        attnT, attnT_r = hT, hT_r
        wo, wo_r = ar.alloc("wo", [KC, D], BF16)
        self.loadw(wo, self.mla_wo_d.rearrange("(k p) n -> p k n", p=128), wo_r, "d_w0")
        knT, knT_r = ar.alloc("knT", [NK], BF16)
        vh, vh_r = ar.alloc("vh", [NK // 128, 128], BF16)
        qn, qn_r = ar.alloc("qn", [T], BF16)
        qr, qr_r = ar.alloc("qr", [T], BF16)
        _eb = [ar.alloc(f"E{i}", [512], BF16) for i in range(3)]
        EB, EBr = [x[0] for x in _eb], [x[1] for x in _eb]
        self.rden = ar.alloc("rden", [512], F32)
        if g.lat:
            qrot, qrot_r = ar.alloc("qrot", [T], BF16)
            kb.op("dve", lambda e: e.memset(qrot, 0.0), writes=[qrot_r])
            rA, rA_r = ar.alloc("rA", [2, 32], F32)
            rB, rB_r = ar.alloc("rB", [2, 64], F32)
            self.load(rA[0:64], self.mla_rA_d[:, :, :], rA_r)
            self.load(rB[0:64], self.mla_rB_d[:, :, :], rB_r)
            t1, t1_r = ar.alloc("t1", [512], F32)
            t2, t2_r = ar.alloc("t2", [512], F32)
        scale = 192 ** -0.5
        qres = [[Res(f"qn{b}"), Res(f"qr{b}"), Res(f"qo{b}")] for b in range(g.NB)]
        for h in range(8):
            c0 = h * 256
            q0 = h * 320
            for kb0 in range(0, NK, 512):
                ps, psr = self.next_ps()
                for k in range(2):
                    self.mm(ps[:, :], wukv[:, k, c0:c0 + 128], ckvT[:, k, kb0:kb0 + 512], k == 0, k == 1, reads=[wukv_r, ckvT_r], writes=[psr])
                self.cp("act", knT[:, kb0:kb0 + 512], ps[:, :], reads=[psr], writes=[knT_r])
            for kc0 in range(0, NK // 128, 4):
                ps, psr = self.next_ps()
                for kk in range(4):
                    kc = kc0 + kk
                    for k in range(2):
                        self.mm(ps[:, kk * 128:(kk + 1) * 128], ckvT[:, k, kc * 128:(kc + 1) * 128], wukv[:, k, c0 + 128:c0 + 256],
                                k == 0, k == 1, reads=[wukv_r, ckvT_r], writes=[psr])
                self.cp("dve", vh[:, kc0:kc0 + 4, :], ps[:, :].rearrange("p (a b) -> p a b", a=4), reads=[psr], writes=[vh_r])
            if self.stop == "B0a":
                continue
            for b in range(g.NB):
                tb = slice(b * 512, (b + 1) * 512)
                ps, psr = self.next_ps()
                for k in range(3):
                    self.mm(ps[:, :], wuq[:, k, q0:q0 + 128], cqnT[:, k, tb], k == 0, k == 2, reads=[wuq_r, cqnT_r], writes=[psr])
                self.cp("act", qn[:, tb], ps[:, :], reads=[psr], writes=[qres[b][0]])
                ps, psr = self.next_ps()
                for k in range(3):
                    self.mm(ps[:, :], wuq[:, k, q0 + 128:q0 + 256], cqnT[:, k, tb], k == 0, k == 2, reads=[wuq_r, cqnT_r], writes=[psr])
                self.cp("act", qr[:, tb], ps[:, :], reads=[psr], writes=[qres[b][1]])
                if g.lat and self.stop != "B0b":
                    ps2, ps2r = self.next_ps()
                    for k in range(3):
                        self.mm(ps2[:, :], wuq[:, k, q0 + 192:q0 + 320], cqnT[:, k, tb], k == 0, k == 2, reads=[wuq_r, cqnT_r], writes=[ps2r])
                    r0 = b * 8
                    for rr in range(8):
                        sg_ = slice(rr * 64, (rr + 1) * 64)
                        self.stt(t1[0:64, sg_], ps[0:64, sg_], rA[0:64, 0, r0 + rr:r0 + rr + 1], rB[0:64, 0, :], ALU.mult, ALU.mult,
                                 reads=[psr, rA_r, rB_r], writes=[t1_r])
                        self.stt(t2[0:64, sg_], ps2[0:64, sg_], rA[0:64, 1, r0 + rr:r0 + rr + 1], rB[0:64, 1, :], ALU.mult, ALU.mult,
                                 reads=[ps2r, rA_r, rB_r], writes=[t2_r])
                    self.tt(qrot[0:64, tb], t1[0:64, :], t2[0:64, :], ALU.add, reads=[t1_r, t2_r, qrot_r], writes=[qres[b][2]])
            if self.stop in ("B0", "B0b"):
                continue
            rds = [knT_r, vh_r, krT_r]
            jobs = []
            if g.lat:
                for b in range(4):
                    qs = slice(b * 512, (b + 1) * 512)
                    chunks = []
                    for kc in range(NK // 128):
                        ks = slice(kc * 128, (kc + 1) * 128)
                        qrp = qr if kc < 4 else qrot
                        chunks.append(([(knT[:, ks], qn[:, qs]), (krT[:, ks], qrp[:, qs])], vh[:, kc, :]))
                    jobs.append((512, chunks, attnT[:, h, qs], attnT_r, qres[b]))
            else:
                for s_ in range(4):
                    qs = slice(s_ * 256, (s_ + 1) * 256)
                    chunks = []
                    for kc in range(2 * s_, 2 * s_ + 2):
                        ks = slice(kc * 128, (kc + 1) * 128)
                        chunks.append(([(knT[:, ks], qn[:, qs]), (krT[:, ks], qr[:, qs])], vh[:, kc, :]))
                    jobs.append((256, chunks, attnT[:, h, qs], attnT_r, qres[s_ // 2][0:2]))
            self.attn_jobs(jobs, scale, rds, (EB, EBr))
        if self.stop in ("B0", "B", "B0a", "B0b"):
            return
        for b in range(g.NB):
            tb = slice(b * 512, (b + 1) * 512)
            for m in range(KC):
                ps, psr = self.next_ps()
                for h in range(8):
                    self.mm(ps[:, :], wo[:, h, m * 128:(m + 1) * 128], attnT[:, h, tb], h == 0, h == 7, reads=[wo_r, attnT_r], writes=[psr])
                self.resid(b, m, tb, ps[:, :], psr, self.gate(l, 0, m, g.ci))

    def conv(self, g, l):
        kb, ar = self.kb, self.ar
        T, nseq, S = g.T, g.nseq, g.S
        W = S + 30
        glu, glu_r = ar.alloc("glu", [KC, nseq, W], BF16, top=True)
        vec, vec_r = ar.alloc("cvvec", [4, KC], F32)
        b1, b1_r = ar.alloc("cvb1", [16], F32)
        wdw, wdw_r = ar.alloc("wdw", [KC, 31], F32)
        self.load(vec, self.cv_vec_d[:, :, :], vec_r)
        self.load(b1, self.cv_b1_d[:, :], b1_r)
        self.load(wdw, self.cv_wdw_d[:, :, :], wdw_r)
        mk0 = ar.mark()
        hT, hT_r = self.norm1(g, l)
        w1, w1_r = ar.alloc("w1", [KC, 2048], BF16)
        self.loadw(w1[:, :, 0:1024], self.cv_w1_d[:, 0:1024].rearrange("(k p) n -> p k n", p=128), w1_r, "d_w0")
        self.loadw(w1[:, :, 1024:2048], self.cv_w1_d[:, 1024:2048].rearrange("(k p) n -> p k n", p=128), w1_r, "d_w0")
        sig, sig_r = ar.alloc("sig", [512], F32)
        kb.op("dve", lambda e: e.memset(glu.rearrange("p a b c -> p (a b c)"), 0.0), writes=[glu_r])
        NQ = 512
        spb = NQ // S if S < NQ else 1
        for b in range(g.NB):
            tb = slice(b * 512, (b + 1) * 512)
            for m in range(KC):
                pa, par = self.next_ps()
                pg, pgr = self.next_ps()
                for k in range(KC):
                    self.mm(pa[:, :], w1[:, k, m * 128:(m + 1) * 128], hT[:, k, tb], k == 0, k == KC - 1, reads=[w1_r, hT_r], writes=[par])
                for k in range(KC):
                    self.mm(pg[:, :], w1[:, k, 1024 + m * 128:1024 + (m + 1) * 128], hT[:, k, tb], k == 0, k == KC - 1, reads=[w1_r, hT_r], writes=[pgr])
                self.act(sig, pg[:, :], AF.Sigmoid, reads=[pgr, b1_r], writes=[sig_r], bias=b1[:, 8 + m:9 + m])
                if g.lat:
                    dst = glu[:, m, 0, 15 + b * 512:15 + (b + 1) * 512]
                    self.stt(dst, pa[:, :], b1[:, m:m + 1], sig, ALU.add, ALU.mult, reads=[par, b1_r, sig_r], writes=[glu_r])
                else:
                    dst = glu[:, m, 2 * b:2 * b + 2, 15:15 + S]
                    self.stt(dst, pa[:, :].rearrange("p (s t) -> p s t", s=2), b1[:, m:m + 1],
                             sig.rearrange("p (s t) -> p s t", s=2), ALU.add, ALU.mult, reads=[par, b1_r, sig_r], writes=[glu_r])
        ar.release(mk0)
        cv, cv_r = ar.alloc("cv", [KC, T], F32)
        mkd = ar.mark()
        dg = [ar.alloc(f"dg{i}", [31, 128], BF16) for i in range(2)]
        for m in range(KC):
            dgt, dg_r = dg[m % 2]
            for j in range(31):
                self.ts(dgt[:, j, :], self.identb, wdw[:, m, j:j + 1], None, ALU.mult, None, reads=[wdw_r, self.c_r], writes=[dg_r])
            for b in range(g.NB):
                ps, psr = self.next_ps()
                for j in range(31):
                    if g.lat:
                        rhs = glu[:, m, 0, b * 512 + j:b * 512 + j + 512]
                        out = ps[:, :]
                    else:
                        rhs = glu[:, m, 2 * b:2 * b + 2, j:j + S]
                        out = ps[:, :].rearrange("p (s t) -> p s t", s=2)
                    self.mm(out, dgt[:, j, :], rhs, j == 0, j == 30, reads=[dg_r, glu_r], writes=[psr])
                self.act(cv[:, m, b * 512:(b + 1) * 512], ps[:, :], AF.Identity, reads=[psr, vec_r], writes=[cv_r], bias=vec[:, 0, m:m + 1])
        ar.release(mkd)
        ar.release_top()
        w2, w2_r = ar.alloc("w2", [KC, D], BF16)
        self.loadw(w2, self.cv_w2_d.rearrange("(k p) n -> p k n", p=128), w2_r, "d_w1")
        sqf = [ar.alloc(f"sqf{i}", [512], F32) for i in range(2)]
        mean, mean_r = ar.alloc("mean", [512], F32)
        var, var_r = ar.alloc("var", [512], F32)
        lnv, lnv_r = ar.alloc("lnv", [512], F32)
        rstd, rstd_r = ar.alloc("rstd", [512], F32)
        tmf = [ar.alloc(f"tmf{i}", [512], F32) for i in range(2)]
        sb, sb_r = ar.alloc("sb", [KC, 512], BF16)
        tm2, tm2_r = ar.alloc("tm2", [512], F32)
        for b in range(g.NB):
            tb = slice(b * 512, (b + 1) * 512)
            pm, pmr = self.next_ps()
            pq, pqr = self.next_ps()
            for m in range(KC):
                s_, s_r = sqf[m % 2]
                self.act(s_, cv[:, m, tb], AF.Square, reads=[cv_r], writes=[s_r])
                self.mm(pm[:, :], self.onesf, cv[:, m, tb], m == 0, m == KC - 1, reads=[cv_r, self.c_r], writes=[pmr])
                self.mm(pq[:, :], self.onesf, s_, m == 0, m == KC - 1, reads=[s_r, self.c_r], writes=[pqr])
            self.cp("act", mean, pm[:, :], reads=[pmr], writes=[mean_r])
            self.tt(var, mean, mean, ALU.mult, reads=[mean_r], writes=[var_r])
            self.tt(var, pq[:, :], var, ALU.subtract, reads=[pqr, var_r], writes=[var_r])
            self.act(lnv, var, AF.Ln, reads=[var_r, self.c_r], writes=[lnv_r], bias=self.eps_t[:, :])
            self.act(rstd, lnv, AF.Exp, reads=[lnv_r], writes=[rstd_r], scale=-0.5)
            for m in range(KC):
                t_, t_r = tmf[m % 2]
                self.tt(t_, cv[:, m, tb], mean, ALU.subtract, reads=[cv_r, mean_r], writes=[t_r])
                self.tt(t_, t_, rstd, ALU.mult, reads=[t_r, rstd_r], writes=[t_r])
                self.act(sb[:, m, :], t_, AF.Silu, reads=[t_r, vec_r], writes=[sb_r], bias=vec[:, 2, m:m + 1], scale=vec[:, 1, m:m + 1])
            for m in range(KC):
                ps, psr = self.next_ps()
                for k in range(KC):
                    self.mm(ps[:, :], w2[:, k, m * 128:(m + 1) * 128], sb[:, k, :], k == 0, k == KC - 1, reads=[w2_r, sb_r], writes=[psr])
                self.ts(tm2, ps[:, :], vec[:, 3, m:m + 1], self.gate(l, 0, m, g.ci), ALU.add, ALU.mult,
                        reads=[psr, vec_r, self.mod_r], writes=[tm2_r])
                self.tt(self.X[:, m, tb], self.X[:, m, tb], tm2, ALU.add, reads=[tm2_r, self.Xr[b]], writes=[self.Xr[b]])

    def gqa(self, g, l):
        kb, ar = self.kb, self.ar
        T, NT = g.T, g.NT
        hT, hT_r = self.norm1(g, l)
        NKC = 512 if g.lat else 0
        NK = NKC + T
        gg, gg_r = ar.alloc("gg", [2, 128], F32)
        self.load(gg, self.gq_g_d[:, :, :], gg_r)
        if g.lat:
            rtb = [ar.alloc(f"grt{i}", [2, 64], F32) for i in range(2)]

            def load_rt(t):
                kb.dma("sp", [(rtb[t % 2][0], self.gq_rtok_d[:, :, t, :])], f"d_rt{t % 2}", writes=[rtb[t % 2][1]])
        attnT, attnT_r = ar.alloc("attnT", [4, T], BF16)
        QrT, QrT_r = ar.alloc("QrT", [4, T], BF16)
        if g.lat:
            QuT, QuT_r = ar.alloc("QuT", [4, T], BF16)
        KT, KT_r = ar.alloc("KT", [NK], BF16)
        Vt, Vt_r = ar.alloc("Vt", [NK // 128, 128], BF16)
        wq, wq_r = ar.alloc("wq", [KC, 768], BF16)
        wo = wq.rearrange("p a b -> p (a b)")[:, 0:4 * D].rearrange("p (a b) -> p a b", a=4)
        wo_r = wq_r
        _gsq = [ar.alloc(f"gsq{i}", [6, 128], F32) for i in range(2)]
        _gst = [ar.alloc(f"gst{i}", [24], F32) for i in range(2)]
        qf = [ar.alloc(f"qf{i}", [6, 128], F32) for i in range(2)]
        qb = [ar.alloc(f"qb{i}", [11, 128], BF16) for i in range(2)]
        _grt = [ar.alloc(f"grtmp{i}", [2, 5, 64], F32) for i in range(2)]
        _eb = [ar.alloc(f"E{i}", [512], BF16) for i in range(3)]
        EB, EBr = [x[0] for x in _eb], [x[1] for x in _eb]
        self.rden = ar.alloc("rden", [512], F32)
        if g.lat:
            cc, cc_r = ar.alloc("gcc", [4, 2, 128], BF16)
        scale = 128 ** -0.5
        for kvh in range(2):
            wsrc = self.gq_w_d.rearrange("(k p) n -> p k n", p=128)
            self.loadw(wq[:, :, 0:512], wsrc[:, :, kvh * 512:(kvh + 1) * 512], wq_r, "d_w0")
            self.loadw(wq[:, :, 512:640], wsrc[:, :, 1024 + kvh * 128:1024 + (kvh + 1) * 128], wq_r, "d_w0")
            self.loadw(wq[:, :, 640:768], wsrc[:, :, 1280 + kvh * 128:1280 + (kvh + 1) * 128], wq_r, "d_w0")
            if g.lat:
                self.loadw(cc[:, :, 0, :], self.gq_ck_d[:, kvh * 128:(kvh + 1) * 128].rearrange("(j p) c -> p j c", p=128), cc_r, "d_w2")
                self.loadw(cc[:, :, 1, :], self.gq_cv_d[:, kvh * 128:(kvh + 1) * 128].rearrange("(j p) c -> p j c", p=128), cc_r, "d_w2")
                ps, psr = self.next_ps()
                pb = ps[:, :].bitcast(BF16)
                for j in range(4):
                    self.tr(pb[:, j * 128:(j + 1) * 128], cc[:, j, 0, :], self.identb, reads=[cc_r, self.c_r], writes=[psr])
                self.cp("act", KT[:, 0:512], pb[:, 0:512], reads=[psr], writes=[KT_r])
                self.cp("dve", Vt[:, 0:4, :], cc[:, :, 1, :], reads=[cc_r], writes=[Vt_r])
            if g.lat:
                load_rt(0)
            def tile_gen(tt_):
                tsl = slice(tt_ * 128, (tt_ + 1) * 128)
                if g.lat:
                    if tt_ + 1 < NT:
                        load_rt(tt_ + 1)
                    rt, rt_r = rtb[tt_ % 2]
                st, st_r = _gst[tt_ % 2]
                sq, sq_r = _gsq[tt_ % 2]
                rtmp, rtmp_r = _grt[tt_ % 2]
                p1, p1r = self.next_ps()
                p2, p2r = self.next_ps()
                for k in range(KC):
                    self.mm(p1[:, :], hT[:, k, tsl], wq[:, k, 0:512], k == 0, k == KC - 1, reads=[hT_r, wq_r], writes=[p1r])
                for k in range(KC):
                    self.mm(p2[:, 0:256], hT[:, k, tsl], wq[:, k, 512:768], k == 0, k == KC - 1, reads=[hT_r, wq_r], writes=[p2r])
                self.act(sq[:, 0:4, :], p1[:, :].rearrange("p (h d) -> p h d", h=4), AF.Square, reads=[p1r], writes=[sq_r])
                self.act(sq[:, 4, :], p2[:, 0:128], AF.Square, reads=[p2r], writes=[sq_r])
                kb.op("dve", lambda e: e.tensor_reduce(out=st[:, 0:5], in_=sq[:, 0:5, :], axis=AX.X, op=ALU.add), reads=[sq_r], writes=[st_r])
                self.act(st[:, 8:13], st[:, 0:5], AF.Ln, reads=[st_r, self.c_r], writes=[st_r], bias=self.eps_t[:, :], scale=1.0 / 128)
                self.act(st[:, 16:21], st[:, 8:13], AF.Exp, reads=[st_r], writes=[st_r], scale=-0.5)
                q_, q_r = qf[tt_ % 2]
                o_, o_r = qb[tt_ % 2]
                self.tt(q_[:, 0:4, :], p1[:, :].rearrange("p (h d) -> p h d", h=4), st[:, 16:20].unsqueeze(2).broadcast_to([128, 4, 128]),
                        ALU.mult, reads=[p1r, st_r], writes=[q_r])
                self.tt(q_[:, 0:4, :], q_[:, 0:4, :], gg[:, 0, :].unsqueeze(1).broadcast_to([128, 4, 128]), ALU.mult, reads=[q_r, gg_r], writes=[q_r])
                self.stt(q_[:, 4, :], p2[:, 0:128], st[:, 20:21], gg[:, 1, :], ALU.mult, ALU.mult, reads=[p2r, st_r, gg_r], writes=[q_r])
                self.cp("act", q_[:, 5, :], p2[:, 128:256], reads=[p2r], writes=[q_r])
                self.cp("act", o_[:, 9, :], p2[:, 128:256], reads=[p2r], writes=[o_r])
                if g.lat:
                    c_ = rt[:, 0, :].unsqueeze(1).broadcast_to([128, 5, 64])
                    s_ = rt[:, 1, :].unsqueeze(1).broadcast_to([128, 5, 64])
                    x1, x2 = q_[:, 0:5, 0:64], q_[:, 0:5, 64:128]
                    self.tt(rtmp[:, 0, :, :], x1, c_, ALU.mult, reads=[q_r, rt_r], writes=[rtmp_r])
                    self.tt(rtmp[:, 1, :, :], x2, s_, ALU.mult, reads=[q_r, rt_r], writes=[rtmp_r])
                    self.tt(o_[:, 0:5, 0:64], rtmp[:, 0, :, :], rtmp[:, 1, :, :], ALU.subtract, reads=[rtmp_r], writes=[o_r])
                    self.tt(rtmp[:, 0, :, :], x1, s_, ALU.mult, reads=[q_r, rt_r], writes=[rtmp_r])
                    self.tt(rtmp[:, 1, :, :], x2, c_, ALU.mult, reads=[q_r, rt_r], writes=[rtmp_r])
                    self.tt(o_[:, 0:5, 64:128], rtmp[:, 0, :, :], rtmp[:, 1, :, :], ALU.add, reads=[rtmp_r], writes=[o_r])
                    self.cp("act", o_[:, 5:9, :], q_[:, 0:4, :], reads=[q_r], writes=[o_r])
                    ntr = 9
                else:
                    self.cp("act", o_[:, 0:5, :], q_[:, 0:5, :], reads=[q_r], writes=[o_r])
                    ntr = 5
                    kb.dma("sp", [(self.st_k_d[tsl, kvh * 128:(kvh + 1) * 128], q_[:, 4, :])], "d_st", reads=[q_r])
                    kb.dma("sp", [(self.st_v_d[tsl, kvh * 128:(kvh + 1) * 128], q_[:, 5, :])], "d_st", reads=[q_r])
                yield
                pa, par = self.next_ps()
                pba = pa[:, :].bitcast(BF16)
                for i in range(5):
                    self.tr(pba[:, i * 128:(i + 1) * 128], o_[:, i, :], self.identb, reads=[o_r, self.c_r], writes=[par])
                ksl = slice(NKC + tt_ * 128, NKC + (tt_ + 1) * 128)
                self.cp("act", QrT[:, :, tsl], pba[:, 0:512].rearrange("p (h t) -> p h t", h=4), reads=[par], writes=[QrT_r])
                self.cp("act", KT[:, ksl], pba[:, 512:640], reads=[par], writes=[KT_r])
                self.cp("dve", Vt[:, NKC // 128 + tt_, :], o_[:, 9, :], reads=[o_r], writes=[Vt_r])
                if g.lat:
                    pc, pcr = self.next_ps()
                    pbc = pc[:, :].bitcast(BF16)
                    for i in range(4):
                        self.tr(pbc[:, i * 128:(i + 1) * 128], o_[:, 5 + i, :], self.identb, reads=[o_r, self.c_r], writes=[pcr])
                    self.cp("act", QuT[:, :, tsl], pbc[:, 0:512].rearrange("p (h t) -> p h t", h=4), reads=[pcr], writes=[QuT_r])
            _cur = tile_gen(0)
            next(_cur)
            for tt_ in range(NT):
                _nxt = None
                if tt_ + 1 < NT:
                    _nxt = tile_gen(tt_ + 1)
                    next(_nxt)
                for _ in _cur:
                    pass
                _cur = _nxt
            rds = [KT_r, Vt_r, QrT_r] + ([QuT_r] if g.lat else [])
            for hh in range(4):
                jobs = []
                if g.lat:
                    for b in range(4):
                        qs = slice(b * 512, (b + 1) * 512)
                        chunks = []
                        for kc in range(NK // 128):
                            ks = slice(kc * 128, (kc + 1) * 128)
                            qsrc = QuT if kc < 4 else QrT
                            chunks.append(([(KT[:, ks], qsrc[:, hh, qs])], Vt[:, kc, :]))
                        jobs.append((512, chunks, attnT[:, hh, qs], attnT_r))
                else:
                    for s_ in range(4):
                        qs = slice(s_ * 256, (s_ + 1) * 256)
                        chunks = []
                        for kc in range(2 * s_, 2 * s_ + 2):
                            ks = slice(kc * 128, (kc + 1) * 128)
                            chunks.append(([(KT[:, ks], QrT[:, hh, qs])], Vt[:, kc, :]))
                        jobs.append((256, chunks, attnT[:, hh, qs], attnT_r))
                self.attn_jobs(jobs, scale, rds, (EB, EBr))
            self.loadw(wo, self.gq_wo_d[kvh * 512:(kvh + 1) * 512, :].rearrange("(k p) n -> p k n", p=128), wo_r, "d_w0")
            for b in range(g.NB):
                tb = slice(b * 512, (b + 1) * 512)
                for m in range(KC):
                    ps, psr = self.next_ps()
                    for hh in range(4):
                        self.mm(ps[:, :], wo[:, hh, m * 128:(m + 1) * 128], attnT[:, hh, tb], hh == 0, hh == 3, reads=[wo_r, attnT_r], writes=[psr])
                    self.resid(b, m, tb, ps[:, :], psr, self.gate(l, 0, m, g.ci))

    def moe(self, g, l):
        kb, ar = self.kb, self.ar
        T, NT, NB, C, NS, NCC = g.T, g.NT, g.NB, g.C, g.NSLOT, g.NCC
        ci = g.ci
        h2tok, h2tok_r = ar.alloc("h2tok", [NT, D], BF16)
        aff, aff_r = ar.alloc("aff", [NT, NE], F32)
        affhl, affhl_r = ar.alloc("affhl", [NT, NE, 2], BF16)
        posg, posg_r = ar.alloc("posg", [NT, NE], F32)
        mk0 = ar.mark()
        affT, affT_r = ar.alloc("affT", [T], F32)
        mk1 = ar.mark()
        rw, rw_r = ar.alloc("rw", [KC, NE], F32)
        self.load(rw, self.rw_d[:, l, :, :], rw_r)
        tmps = self.norm_tmps()
        h2f = [ar.alloc(f"h2f{i}", [KC, 512], F32) for i in range(2)]
        sm, sm_r = ar.alloc("sm", [16], F32)
        ex, ex_r = ar.alloc("ex", [4, NE], F32)
        def norm_b(b):
            hf, hf_r = h2f[b % 2]
            self.norm_block(b, lambda m: self.gs[:, l, 1, m, ci:ci + 1], lambda m: self.shift(l, 1, m, ci),
                            lambda m: hf[:, m, :], hf_r, tmps, precise=True)

        def route_b(b):
            hf, hf_r = h2f[b % 2]
            pT, pTr = self.next_ps(pin=True)
            pl, plr = self.next_ps(pin=True)
            t4 = slice(b * 4, b * 4 + 4)
            for q in range(4):
                qs = slice(q * 128, (q + 1) * 128)
                for k in range(KC):
                    self.mm(pl[:, q * NE:(q + 1) * NE], hf[:, k, qs], rw[:, k, :], k == 0, k == KC - 1, reads=[hf_r, rw_r], writes=[plr])
            plv = pl[:, 0:4 * NE].rearrange("p (q e) -> p q e", q=4)
            kb.op("dve", lambda e: e.tensor_reduce(out=sm[:, 0:4], in_=plv, axis=AX.X, op=ALU.max), reads=[plr], writes=[sm_r])
            self.tt(ex, plv, sm[:, 0:4].unsqueeze(2).broadcast_to([128, 4, NE]), ALU.subtract, reads=[plr, sm_r], writes=[ex_r])
            self.act(ex, ex, AF.Exp, reads=[ex_r], writes=[ex_r])
            kb.op("dve", lambda e: e.tensor_reduce(out=sm[:, 4:8], in_=ex, axis=AX.X, op=ALU.add), reads=[ex_r], writes=[sm_r])
            kb.op("dve", lambda e: e.reciprocal(out=sm[:, 8:12], in_=sm[:, 4:8]), reads=[sm_r], writes=[sm_r])
            self.tt(aff[:, t4, :], ex, sm[:, 8:12].unsqueeze(2).broadcast_to([128, 4, NE]), ALU.mult, reads=[ex_r, sm_r], writes=[aff_r])
            self.unpin(plr)
            self.cp("dve", affhl[:, t4, :, 0], aff[:, t4, :], reads=[aff_r], writes=[affhl_r])
            self.tt(ex, aff[:, t4, :], affhl[:, t4, :, 0], ALU.subtract, reads=[aff_r, affhl_r], writes=[ex_r])
            self.cp("dve", affhl[:, t4, :, 1], ex, reads=[ex_r], writes=[affhl_r])
            for q in range(4):
                tt_ = b * 4 + q
                qs = slice(q * 128, (q + 1) * 128)
                self.tr(pT[0:NE, qs], aff[:, tt_, :], self.ident, reads=[aff_r, self.c_r], writes=[pTr])
                for half in range(2):
                    ph, phr = self.next_ps()
                    for mm_ in range(4):
                        m = half * 4 + mm_
                        self.tr(ph[:, mm_ * 128:(mm_ + 1) * 128], hf[:, m, qs], self.ident, reads=[hf_r, self.c_r], writes=[phr])
                    self.cp("act" if half == 0 else "dve", h2tok[:, tt_, half * 512:(half + 1) * 512], ph[:, :], reads=[phr], writes=[h2tok_r])
            self.cp("act", affT[0:NE, b * 512:(b + 1) * 512], pT[0:NE, :], reads=[pTr], writes=[affT_r])
            self.unpin(pTr)

        norm_b(0)
        for b in range(NB):
            if b + 1 < NB:
                norm_b(b + 1)
            route_b(b)
        ar.release(mk1)
        S = g.S
        work, work_r = ar.alloc("work", [S], F32)
        vals, vals_r = ar.alloc("vals", [C], F32)
        mask, mask_r = ar.alloc("mask", [S], F32)
        cum, cum_r = ar.alloc("cum", [S], F32)
        zer, zer_r = ar.alloc("zer", [S], F32)
        pgT, pgT_r = ar.alloc("pgT", [T], F32)
        kb.op("dve", lambda e: e.memset(zer[0:NE, :], 0.0), writes=[zer_r])
        ada_next = None
        if g.lat and l + 1 < self.depth:
            wb = [ar.alloc(f"adaw{i}", [KC, 1024], BF16) for i in range(2)]
            ada_next = self.adaln_issue(l + 1, wb)
        for s in range(g.nseq):
            ss = slice(s * S, (s + 1) * S)
            self.cp("dve", work[0:NE, :], affT[0:NE, ss], reads=[affT_r], writes=[work_r])
            for r in range(C // 8):
                kb.op("dve", lambda e: e.max(out=vals[0:NE, r * 8:(r + 1) * 8], in_=work[0:NE, :]), reads=[work_r], writes=[vals_r])
                if r < C // 8 - 1:
                    kb.op("dve", lambda e: e.match_replace(out=work[0:NE, :], in_to_replace=vals[0:NE, r * 8:(r + 1) * 8],
                                                           in_values=work[0:NE, :], imm_value=-1.0), reads=[work_r, vals_r], writes=[work_r])
            self.ts(mask[0:NE, :], affT[0:NE, ss], vals[0:NE, C - 1:C], None, ALU.is_ge, None, reads=[affT_r, vals_r], writes=[mask_r])
            kb.op("dve", lambda e: e.tensor_tensor_scan(out=cum[0:NE, :], data0=mask[0:NE, :], data1=zer[0:NE, :], initial=0.0,
                                                        op0=ALU.add, op1=ALU.add), reads=[mask_r, zer_r], writes=[cum_r])
            self.stt(mask[0:NE, :], cum[0:NE, :], float(C), mask[0:NE, :], ALU.is_le, ALU.mult, reads=[cum_r, mask_r], writes=[mask_r])
            self.ts(cum[0:NE, :], cum[0:NE, :], float(s * C - 1), None, ALU.add, None, reads=[cum_r], writes=[cum_r])
            self.tt(cum[0:NE, :], cum[0:NE, :], mask[0:NE, :], ALU.mult, reads=[cum_r, mask_r], writes=[cum_r])
            self.ts(mask[0:NE, :], mask[0:NE, :], 4096.0, -4096.0, ALU.mult, ALU.add, reads=[mask_r], writes=[mask_r])
            self.tt(pgT[0:NE, ss], cum[0:NE, :], mask[0:NE, :], ALU.add, reads=[cum_r, mask_r], writes=[pgT_r])
        pp, ppr = self.next_ps()
        for tt_ in range(NT):
            self.tr(pp[:, tt_ * NE:(tt_ + 1) * NE], pgT[0:NE, tt_ * 128:(tt_ + 1) * 128], self.ident[0:NE, 0:NE], reads=[pgT_r, self.c_r], writes=[ppr])
        self.cp("dve", posg, pp[:, 0:NT * NE].rearrange("p (t e) -> p t e", e=NE), reads=[ppr], writes=[posg_r])
        if ada_next is not None:
            self.adaln_finish(l + 1, *ada_next)
        self.dbg("aff", aff, aff_r)
        self.dbg("posg", posg, posg_r)
        self.dbg("h2tok", h2tok, h2tok_r)
        self.dbg("affT", affT[0:NE, :], affT_r)
        self.dbg("pgT", pgT[0:NE, :], pgT_r)
        self.dbg("vals", vals[0:NE, :], vals_r)
        ar.release(mk0)
        wslots = []
        NWS = 2 if (g.lat or "L" in self.groups) else 4
        CG = 1 if g.lat else 4
        NBUF = 2 if g.lat else 8
        for i in range(NWS):
            a = ar.alloc(f"wg{i}", [KC, FF], BF16)
            b_ = ar.alloc(f"wu{i}", [KC, FF], BF16)
            c_ = ar.alloc(f"wd{i}", [4, D], BF16)
            wslots.append((a, b_, c_))
        Sel = [ar.alloc(f"Sel{i}", [NT, NS], BF16) for i in range(2)]
        SelT = [ar.alloc(f"SelT{i}", [NCC, T], BF16) for i in range(NBUF)]
        XG = [ar.alloc(f"xg{i}", [KC, NS], BF16) for i in range(2)]
        sg, sg_r = ar.alloc("sg", [NS], F32)
        hid, hid_r = ar.alloc("hid", [4, NS], BF16)
        YO = [ar.alloc(f"yo{i}", [NCC, D], BF16) for i in range(NBUF)]
        GSL = [ar.alloc(f"gsl{i}", [4], F32) for i in range(2)]

        use_scr = (not g.lat) and ("L" in self.groups)

        def load_w(e):
            (wg, wg_r), (wu, wu_r), (wd, wd_r) = wslots[e % NWS]
            if use_scr:
                sc = self.wscr_d[l, e]
                kb.dma("sp", [(wg.rearrange("p a b -> p (a b)"), sc[:, 0:4096])], f"d_m{e % NWS}a", reads=[self.wscr_rs[l]], writes=[wg_r])
                kb.dma("sp", [(wu.rearrange("p a b -> p (a b)"), sc[:, 4096:8192])], f"d_m{e % NWS}b", reads=[self.wscr_rs[l]], writes=[wu_r])
                kb.dma("sp", [(wd.rearrange("p a b -> p (a b)"), sc[:, 8192:12288])], f"d_m{e % NWS}c", reads=[self.wscr_rs[l]], writes=[wd_r])
                return
            self.loadw(wg, self.wg_d[l, e].rearrange("(k p) n -> p k n", p=128), wg_r, f"d_m{e % NWS}a")
            self.loadw(wu, self.wu_d[l, e].rearrange("(k p) n -> p k n", p=128), wu_r, f"d_m{e % NWS}b")
            self.loadw(wd, self.wd_d[l, e].rearrange("(k p) n -> p k n", p=128), wd_r, f"d_m{e % NWS}c")
            if g.lat:
                sc = self.wscr_d[l, e]
                kb.dma("sp", [(sc[:, 0:4096], wg.rearrange("p a b -> p (a b)")),
                              (sc[:, 4096:8192], wu.rearrange("p a b -> p (a b)")),
                              (sc[:, 8192:12288], wd.rearrange("p a b -> p (a b)"))], "d_ws",
                       reads=[wg_r, wu_r, wd_r], writes=[self.wscr_rs[l]])

        def s1_sel(e):
            sel, sel_r = Sel[e % 2]
            for tt_ in range(NT):
                self.ts(sel[:, tt_, :], self.iotab[:, 0:NS], posg[:, tt_, e:e + 1], None, ALU.is_equal, None,
                        reads=[posg_r, self.c_r], writes=[sel_r])

        def s1_rest(e, comb=None):
            sel, sel_r = Sel[e % 2]
            selT, selT_r = SelT[e % NBUF]
            xg, xg_r = XG[e % 2]
            gsl, gsl_r = GSL[e % 2]
            for m in range(KC):
                if m % 2 == 0:
                    pgm, pgmr = self.next_ps(pin=True)
                o = pgm[:, (m % 2) * 256:(m % 2) * 256 + NS]
                for tt_ in range(NT):
                    self.mm(o, h2tok[:, tt_, m * 128:(m + 1) * 128], sel[:, tt_, :], tt_ == 0, tt_ == NT - 1, reads=[h2tok_r, sel_r], writes=[pgmr])
                if comb is not None:
                    for _ in range((NB * KC + KC - 1) // KC):
                        next(comb, None)
                if m % 2 == 1:
                    self.cp("act", xg[:, m - 1:m + 1, :],
                            pgm[:, :].rearrange("p (a b) -> p a b", a=2)[:, :, 0:NS], reads=[pgmr], writes=[xg_r])
                    self.unpin(pgmr)
            if comb is not None:
                for _ in comb:
                    pass
            pg_, pg_r = self.next_ps()
            for cc in range(NCC):
                for tt_ in range(NT):
                    self.mm(pg_[:, 2 * cc:2 * cc + 2], sel[:, tt_, cc * 128:(cc + 1) * 128], affhl[:, tt_, e, :], tt_ == 0, tt_ == NT - 1,
                            reads=[sel_r, affhl_r], writes=[pg_r])
            for cc in range(NCC):
                kb.op("dve", lambda en: en.tensor_reduce(out=gsl[:, cc:cc + 1], in_=pg_[:, 2 * cc:2 * cc + 2], axis=AX.X, op=ALU.add), reads=[pg_r], writes=[gsl_r])
            for cc in range(NCC):
                for t0 in range(0, NT, 8):
                    pt, ptr = self.next_ps()
                    pbt = pt[:, :].bitcast(BF16)
                    for q in range(8):
                        self.tr(pbt[:, q * 128:(q + 1) * 128], sel[:, t0 + q, cc * 128:(cc + 1) * 128], self.identb, reads=[sel_r, self.c_r], writes=[ptr])
                    self.cp("act", selT[:, cc, t0 * 128:(t0 + 8) * 128], pbt[:, :], reads=[ptr], writes=[selT_r])

        def s2a(e):
            (wg, wg_r), (wu, wu_r), (wd, wd_r) = wslots[e % NWS]
            xg, xg_r = XG[e % 2]
            for f in range(4):
                pf, pfr = self.next_ps()
                for k in range(KC):
                    self.mm(pf[:, 0:NS], wg[:, k, f * 128:(f + 1) * 128], xg[:, k, :], k == 0, k == KC - 1, reads=[wg_r, xg_r], writes=[pfr])
                for k in range(KC):
                    self.mm(pf[:, 256:256 + NS], wu[:, k, f * 128:(f + 1) * 128], xg[:, k, :], k == 0, k == KC - 1, reads=[wu_r, xg_r], writes=[pfr])
                self.act(sg, pf[:, 0:NS], AF.Silu, reads=[pfr], writes=[sg_r])
                self.tt(hid[:, f, :], sg, pf[:, 256:256 + NS], ALU.mult, reads=[sg_r, pfr], writes=[hid_r])

        def s2b(e):
            (wg, wg_r), (wu, wu_r), (wd, wd_r) = wslots[e % NWS]
            yo, yo_r = YO[e % NBUF]
            gsl, gsl_r = GSL[e % 2]
            for cc in range(NCC):
                for db in range(2):
                    py, pyr = self.next_ps()
                    for f in range(4):
                        self.mm(py[:, :], hid[:, f, cc * 128:(cc + 1) * 128], wd[:, f, db * 512:(db + 1) * 512], f == 0, f == 3, reads=[hid_r, wd_r], writes=[pyr])
                    self.act(yo[:, cc, db * 512:(db + 1) * 512], py[:, :], AF.Copy, reads=[pyr, gsl_r], writes=[yo_r], scale=gsl[:, cc:cc + 1])

        def s3(es):
            for b in range(NB):
                tb = slice(b * 512, (b + 1) * 512)
                for m in range(KC):
                    pc, pcr = self.next_ps()
                    n = len(es) * NCC
                    i_ = 0
                    for e in es:
                        yo, yo_r = YO[e % NBUF]
                        selT, selT_r = SelT[e % NBUF]
                        for cc in range(NCC):
                            self.mm(pc[:, :], yo[:, cc, m * 128:(m + 1) * 128], selT[:, cc, tb], i_ == 0, i_ == n - 1, reads=[yo_r, selT_r], writes=[pcr])
                            i_ += 1
                    self.resid(b, m, tb, pc[:, :], pcr, self.gate(l, 1, m, ci))
                    yield

        load_w(0)
        s1_sel(0)
        s1_rest(0)
        for i in range(1, min(NWS - 1, NE)):
            load_w(i)
        for i in range(NE):
            if i + NWS - 1 < NE:
                load_w(i + NWS - 1)
            if i + 1 < NE:
                s1_sel(i + 1)
            s2a(i)
            comb = s3(list(range(i - CG, i))) if (i >= CG and i % CG == 0) else None
            if i + 1 < NE:
                s1_rest(i + 1, comb)
            elif comb is not None:
                for _ in comb:
                    pass
            s2b(i)
        for _ in s3(list(range(NE - CG, NE))):
            pass


def _fm(v):
    v = np.asarray(v, np.float32)
    lead = v.shape[:-1]
    n = v.shape[-1] // 128
    return np.ascontiguousarray(np.moveaxis(v.reshape(*lead, n, 128), -1, 0))


def _rope_tables(n_tokens, rot_dim, grid_w=64, theta=10000.0):
    rows = n_tokens // grid_w
    row = np.repeat(np.arange(rows), grid_w).astype(np.float32)
    col = np.tile(np.arange(grid_w), rows).astype(np.float32)
    axis_dim = rot_dim // 2
    inv = (np.float32(theta) ** (-np.arange(0, axis_dim, 2, dtype=np.float32) / np.float32(axis_dim))).astype(np.float32)
    ang = np.concatenate([row[:, None] * inv, col[:, None] * inv], axis=-1).astype(np.float32)
    return np.cos(ang).astype(np.float32), np.sin(ang).astype(np.float32), inv


def _consts():
    c = np.zeros((128, 512), np.float32)
    c[:, 0:128] = np.eye(128, dtype=np.float32)
    c[:, 128:256] = 1.0 / 1024
    c[:, 256:512] = np.arange(256, dtype=np.float32)[None, :]
    return c


def _pool_rc(S):
    out = np.zeros((128, 4, 16), np.float32)
    for gi, win in enumerate((2, 4, 8, 16)):
        t = np.concatenate([np.arange(8), np.arange(S - 8, S)])
        lo = np.clip(t - win // 2, 0, S)
        hi = np.clip(t + win // 2, 0, S)
        out[:, gi, :] = (1.0 / (hi - lo).astype(np.float32))[None, :]
    return out


_PROG_CACHE = {}


def _get_prog(depth=4, **kw):
    if depth not in _PROG_CACHE:
        p = Prog(depth, **kw)
        p.build()
        _PROG_CACHE[depth] = p
    return _PROG_CACHE[depth]


def make_in_maps(inp, cores=range(NCORES)):
    f32 = lambda a: np.ascontiguousarray(np.asarray(a, np.float32))
    L = 4
    shared = {}
    shared["ada_w"] = f32(inp["ada_w"])
    shared["ada_b"] = f32(np.moveaxis(np.asarray(inp["ada_b"], np.float32).reshape(L, 48, 128), -1, 0))
    shared["g1"] = _fm(inp["norm1_g"])
    shared["g2"] = _fm(inp["norm2_g"])
    shared["gf"] = _fm(inp["final_g"])
    shared["cst"] = _consts()
    shared["pool_w"] = f32(inp["pool_w"][0])
    shared["pool_s"] = _fm(inp["pool_scale"][0])
    shared["mla_wd"] = f32(inp["mla_w_down"][0])
    shared["mla_gq"] = f32(np.broadcast_to(np.asarray(inp["mla_g_q"][0], np.float32)[None, :], (128, 384)))
    wuq = np.asarray(inp["mla_w_uq"][0], np.float32).reshape(384, 8, 192)
    rope = wuq[:, :, 128:192]
    swapped = np.concatenate([rope[:, :, 32:64], rope[:, :, 0:32]], axis=-1)
    shared["mla_wuq"] = f32(np.concatenate([wuq[:, :, 0:128], rope, swapped, rope], axis=-1).reshape(384, 2560))
    shared["mla_gkv"] = f32(np.broadcast_to(np.asarray(inp["mla_g_kv"][0], np.float32)[None, :], (128, 256)))
    shared["mla_wukv"] = f32(inp["mla_w_ukv"][0])
    shared["mla_wo"] = f32(inp["mla_w_o"][0])
    cos, sin, _ = _rope_tables(2048, 64)
    rtok = np.stack([cos, sin], 0).reshape(2, 16, 128, 32).transpose(2, 0, 1, 3)
    shared["mla_rtok"] = f32(rtok)
    rows = np.arange(32, dtype=np.float32)
    cols = np.arange(64, dtype=np.float32)
    inv = _rope_tables(2048, 64)[2]
    rA = np.ones((64, 2, 32), np.float32)
    rB = np.ones((64, 2, 64), np.float32)
    for i in range(64):
        ii = i % 32
        sgn = -1.0 if i < 32 else 1.0
        if ii < 16:
            a = (rows * inv[ii]).astype(np.float32)
            rA[i, 0] = np.cos(a)
            rA[i, 1] = sgn * np.sin(a)
        else:
            a = (cols * inv[ii - 16]).astype(np.float32)
            rB[i, 0] = np.cos(a)
            rB[i, 1] = sgn * np.sin(a)
    shared["mla_rA"] = rA
    shared["mla_rB"] = rB
    shared["cv_w1"] = f32(inp["conv_w_pw1"][0])
    shared["cv_b1"] = _fm(inp["conv_b_pw1"][0])
    shared["cv_wdw"] = f32(np.asarray(inp["conv_w_dw"][0], np.float32).T.reshape(8, 128, 31).transpose(1, 0, 2))
    shared["cv_vec"] = f32(np.stack([_fm(inp["conv_b_dw"][0]), _fm(inp["conv_ln_g"][0]), _fm(inp["conv_ln_b"][0]),
                                     _fm(inp["conv_b_pw2"][0])], axis=1))
    shared["cv_w2"] = f32(inp["conv_w_pw2"][0])
    shared["gq_w"] = f32(inp["gqa_w_qkv"][0])
    shared["gq_g"] = f32(np.broadcast_to(np.stack([np.asarray(inp["gqa_g_q"][0], np.float32),
                                                    np.asarray(inp["gqa_g_k"][0], np.float32)], 0)[None], (128, 2, 128)))
    shared["gq_wo"] = f32(inp["gqa_w_o"][0])
    cos, sin, _ = _rope_tables(2048, 128)
    shared["gq_rtok"] = f32(np.stack([cos, sin], 0).reshape(2, 16, 128, 64).transpose(2, 0, 1, 3))
    shared["rw"] = f32(np.asarray(inp["router_w"], np.float32).reshape(L, 8, 128, NE).transpose(2, 0, 1, 3))
    shared["moe_wg"] = f32(inp["moe_w_gate"])
    shared["moe_wu"] = f32(inp["moe_w_up"])
    shared["moe_wd"] = f32(inp["moe_w_down"])
    shared["pool_rc"] = _pool_rc(2048)
    maps = []
    xs = np.asarray(inp["x_sample"], np.float32)
    xp = np.asarray(inp["x_prompt"], np.float32)
    cc = np.asarray(inp["c"], np.float32)
    cctx = np.asarray(inp["c_ctx"], np.float32)
    for i in cores:
        m = dict(shared)
        m["xl"] = f32(xs[i].T.reshape(8, 128, 2048).transpose(1, 0, 2))
        m["xc"] = f32(xp[4 * i:4 * i + 4].reshape(1024, 1024).T.reshape(8, 128, 1024).transpose(1, 0, 2))
        m["cond"] = f32(np.stack([_fm(cc[i]), _fm(cctx)], axis=-1))
        m["mla_cckv"] = f32(inp["cache_mla_ckv"][i, 0])
        m["mla_ckr"] = f32(inp["cache_mla_krope"][i, 0])
        m["gq_ck"] = f32(np.asarray(inp["cache_gqa_k"][i, 0]).reshape(512, 256))
        m["gq_cv"] = f32(np.asarray(inp["cache_gqa_v"][i, 0]).reshape(512, 256))
        maps.append(m)
    return maps


def kernel(**inputs):
    prog = _get_prog(4)
    maps = make_in_maps(inputs)
    res = run_bass_kernel_spmd(prog.nc, maps, core_ids=list(range(NCORES)))
    return assemble(res.results)


def assemble(results):
    n = len(results)
    y_prompt = np.zeros((4 * n, 256, D), np.float32)
    y_sample = np.zeros((n, 2048, D), np.float32)
    s_ckv = np.zeros((4 * n, 1, 256, 256), np.float32)
    s_kr = np.zeros((4 * n, 1, 256, 64), np.float32)
    s_k = np.zeros((4 * n, 1, 256, 2, 128), np.float32)
    s_v = np.zeros((4 * n, 1, 256, 2, 128), np.float32)
    for i, r in enumerate(results):
        y_sample[i] = r["yl"].transpose(1, 0, 2).reshape(D, 2048).T
        y_prompt[4 * i:4 * i + 4] = r["yc"].transpose(1, 0, 2).reshape(D, 1024).T.reshape(4, 256, D)
        s_ckv[4 * i:4 * i + 4, 0] = r["st_ckv"].reshape(4, 256, 256)
        s_kr[4 * i:4 * i + 4, 0] = r["st_kr"].reshape(4, 256, 64)
        s_k[4 * i:4 * i + 4, 0] = r["st_k"].reshape(4, 256, 2, 128)
        s_v[4 * i:4 * i + 4, 0] = r["st_v"].reshape(4, 256, 2, 128)
    return (y_prompt, y_sample, s_ckv, s_kr, s_k, s_v)
```

```python
import contextlib
import numpy as np
import concourse.bass as bass
import concourse.mybir as mybir
from concourse.bass_utils import run_bass_kernel_spmd

F32 = mybir.dt.float32
BF16 = mybir.dt.bfloat16
AF = mybir.ActivationFunctionType
ALU = mybir.AluOpType
AX = mybir.AxisListType

D = 1024
KC = 8
NE = 16
FF = 512
EPS = 1e-6
NCORES = 8


class Res:
    __slots__ = ("name", "w", "r", "excl")

    def __init__(self, name, excl=False):
        self.name = name
        self.w = None
        self.r = {}
        self.excl = excl


class Eng:
    def __init__(self, name, eng, sem):
        self.name = name
        self.eng = eng
        self.sem = sem
        self.count = 0
        self.waited = {}


class KB:
    def __init__(self, nc):
        self.nc = nc
        self.stack = contextlib.ExitStack()
        self.sems = {}
        self.dma_tot = {}
        self.E = {}
        for name, e in (("pe", nc.tensor), ("act", nc.scalar), ("dve", nc.vector),
                        ("pool", nc.gpsimd), ("sp", nc.sync)):
            s = self.stack.enter_context(nc.semaphore("s_" + name))
            self.sems["s_" + name] = s
            self.E[name] = Eng(name, e, "s_" + name)
        self.n_wait = 0
        self.n_ins = 0
        self.snaps = {}

    def sbuf(self, name, shape, dtype):
        return self.stack.enter_context(self.nc.sbuf_tensor(name, list(shape), dtype))

    def psum(self, name, shape, dtype=F32):
        return self.stack.enter_context(self.nc.psum_tensor(name, list(shape), dtype))

    def dsem(self, key):
        if key not in self.sems:
            self.sems[key] = self.stack.enter_context(self.nc.semaphore(key))
            self.dma_tot[key] = 0
        return key

    def _wait(self, E, key, val):
        if key in self.dma_tot:
            val = max(val, self.dma_tot[key])
        if E.waited.get(key, 0) >= val:
            return
        E.eng.wait_ge(self.sems[key], val)
        E.waited[key] = val
        self.n_wait += 1
        snap = self.snaps.get((key, val))
        if snap:
            for k2, v2 in snap.items():
                if E.waited.get(k2, 0) < v2:
                    E.waited[k2] = v2

    def _dep(self, E, tok):
        key, val = tok
        if E.name == "pe" and key == "s_pe":
            return
        self._wait(E, key, val)

    def _sync(self, E, reads, writes):
        for res in reads:
            if res.w is not None:
                self._dep(E, res.w)
            if res.excl:
                for k, v in res.r.items():
                    if k != E.sem:
                        self._dep(E, (k, v))
        for res in writes:
            if res.w is not None and res.w[0] != E.sem:
                self._dep(E, res.w)
            for k, v in res.r.items():
                if k != E.sem:
                    self._dep(E, (k, v))

    def _mark(self, tok, reads, writes):
        key, val = tok
        for res in reads:
            if res.r.get(key, 0) < val:
                res.r[key] = val
        for res in writes:
            res.w = tok
            res.r = {}

    def op(self, ename, fn, reads=(), writes=()):
        E = self.E[ename]
        self._sync(E, reads, writes)
        ins = fn(E.eng)
        E.count += 1
        ins.then_inc(self.sems[E.sem], 1)
        self.snaps[(E.sem, E.count)] = dict(E.waited)
        self._mark((E.sem, E.count), reads, writes)
        self.n_ins += 1
        return ins

    def dma(self, qname, pairs, sem_key, reads=(), writes=(), **kw):
        E = self.E[qname]
        self.dsem(sem_key)
        self._sync(E, reads, writes)
        for (o, i) in pairs:
            ins = E.eng.dma_start(out=o, in_=i, **kw)
            ins.then_inc(self.sems[sem_key], 16)
            self.dma_tot[sem_key] += 16
            self.n_ins += 1
        tok = (sem_key, self.dma_tot[sem_key])
        self._mark(tok, reads, writes)
        return tok

    def barrier(self):
        for E in self.E.values():
            for F in self.E.values():
                if F is not E and F.count:
                    self._wait(E, F.sem, F.count)
            for k, tot in self.dma_tot.items():
                if tot:
                    self._wait(E, k, tot)

    def close(self):
        self.stack.close()


class Arena:
    def __init__(self, kb, nbytes):
        self.kb = kb
        self.n = nbytes
        self.t = kb.sbuf("arena", [128, nbytes // 4], F32)
        self.off = 0
        self.top = nbytes

    def reset(self):
        self.kb.barrier()
        self.off = 0
        self.top = self.n

    def release_top(self):
        self.kb.barrier()
        self.top = self.n

    def mark(self):
        return self.off

    def release(self, mark):
        self.kb.barrier()
        self.off = mark

    def alloc(self, name, shape, dtype, parts=128, top=False):
        esz = 2 if dtype == BF16 else 4
        n = int(np.prod(shape))
        nb = (n * esz + 31) // 32 * 32
        assert self.off + nb <= self.top, f"arena overflow at {name}: {self.off}+{nb}>{self.top}"
        if top:
            self.top -= nb
            start = self.top
        else:
            start = self.off
            self.off += nb
        v = self.t[0:parts, start // 4:(start + nb) // 4]
        if dtype != F32:
            v = v.bitcast(dtype)
        v = v[:, 0:n]
        if len(shape) == 2:
            v = v.rearrange("p (a b) -> p a b", a=shape[0])
        elif len(shape) == 3:
            v = v.rearrange("p (a b c) -> p a b c", a=shape[0], b=shape[1])
        return v, Res(name)


class Grp:
    def __init__(self, name, T, nseq, S, ci):
        self.name = name
        self.T = T
        self.nseq = nseq
        self.S = S
        self.ci = ci
        self.NT = T // 128
        self.NB = T // 512
        self.C = S // 8
        self.NSLOT = nseq * self.C
        self.NCC = self.NSLOT // 128
        self.lat = (ci == 0)


class Prog:
    def __init__(self, depth=4, do_moe=True, do_mixer=True, debug=False, groups="LC", stop=""):
        self.depth = depth
        self.groups = groups
        self.stop = stop
        self.debug = debug
        self.do_moe = do_moe
        self.do_mixer = do_mixer
        nc = self.nc = bass.Bass("TRN2", target_bir_lowering=False)
        self.kb = kb = KB(nc)
        self.din = {}
        self.dout = {}

    def inp(self, name, shape, dt=F32):
        ap = self.nc.dram_tensor(name, list(shape), dt, kind="ExternalInput").ap()
        self.din[name] = ap
        return ap

    def outp(self, name, shape):
        ap = self.nc.dram_tensor(name, list(shape), F32, kind="ExternalOutput").ap()
        self.dout[name] = ap
        return ap

    def dbg(self, name, ap, res):
        if not getattr(self, "debug", False) or name in self.dout:
            return
        shp = list(ap.shape)
        d = self.nc.dram_tensor("dbg_" + name, shp, ap.dtype, kind="ExternalOutput").ap()
        self.dout[name] = d
        self.kb.dma("sp", [(d, ap)], "d_dbg", reads=[res])

    def next_ps(self, pin=False):
        i = self.ps_i
        while i in self.ps_pinned:
            i = (i + 1) % 8
        self.ps_i = (i + 1) % 8
        if pin:
            self.ps_pinned.add(i)
        return self.ps[i], self.psr[i]

    def unpin(self, psr):
        self.ps_pinned.discard(self.psr.index(psr))

    def mm(self, out, lhsT, rhs, start, stop, reads, writes):
        self.kb.op("pe", lambda e: e.matmul(out, lhsT, rhs, start=start, stop=stop), reads=reads, writes=writes)

    def tr(self, out, in_, ident, reads, writes):
        self.kb.op("pe", lambda e: e.transpose(out, in_, ident), reads=reads, writes=writes)

    def act(self, out, in_, func, reads, writes, bias=None, scale=1.0, accum_out=None):
        kw = {}
        if bias is not None:
            kw["bias"] = bias
        if accum_out is not None:
            kw["accum_out"] = accum_out
        self.kb.op("act", lambda e: e.activation(out=out, in_=in_, func=func, scale=scale, **kw),
                   reads=reads, writes=writes)

    def tt(self, out, in0, in1, op, reads, writes, eng="dve"):
        self.kb.op(eng, lambda e: e.tensor_tensor(out=out, in0=in0, in1=in1, op=op), reads=reads, writes=writes)

    def ts(self, out, in0, s1, s2, op0, op1, reads, writes, eng="dve", accum_out=None):
        if s2 is None:
            self.kb.op(eng, lambda e: e.tensor_scalar(out=out, in0=in0, scalar1=s1, scalar2=None, op0=op0),
                       reads=reads, writes=writes)
        else:
            self.kb.op(eng, lambda e: e.tensor_scalar(out=out, in0=in0, scalar1=s1, scalar2=s2, op0=op0, op1=op1),
                       reads=reads, writes=writes)

    def stt(self, out, in0, scalar, in1, op0, op1, reads, writes):
        self.kb.op("dve", lambda e: e.scalar_tensor_tensor(out=out, in0=in0, scalar=scalar, in1=in1, op0=op0, op1=op1),
                   reads=reads, writes=writes)

    def cp(self, eng, out, in_, reads, writes):
        if eng == "act":
            self.kb.op("act", lambda e: e.copy(out=out, in_=in_), reads=reads, writes=writes)
        else:
            self.kb.op(eng, lambda e: e.tensor_copy(out=out, in_=in_), reads=reads, writes=writes)

    def load(self, out, in_, res, sem="d_ld", q="sp", **kw):
        self.kb.dma(q, [(out, in_)], sem, writes=[res], **kw)

    def loadw(self, out, in_, res, sem):
        n = out.shape[-1]
        pairs = []
        if n <= 1024:
            self.kb.dma("pool", [(out, in_)], sem, writes=[res])
            return
        for c0 in range(0, n, 1024):
            c1 = min(n, c0 + 1024)
            if len(out.shape) == 3:
                pairs.append((out[:, :, c0:c1], in_[:, :, c0:c1]))
            else:
                pairs.append((out[:, c0:c1], in_[:, c0:c1]))
        self.kb.dma("pool", pairs, sem, writes=[res])

    def rstd_from(self, out, in_, n, reads, writes, tmp, tmp_r):
        P = out.shape[0]
        self.act(tmp, in_, AF.Ln, reads=list(reads) + [self.c_r], writes=[tmp_r], bias=self.eps_t[0:P, :], scale=1.0 / n)
        self.act(out, tmp, AF.Exp, reads=[tmp_r], writes=writes, scale=-0.5)

    def build(self):
        nc, kb = self.nc, self.kb
        L = 4
        inp, outp = self.inp, self.outp
        xl_d = inp("xl", [128, KC, 2048])
        xc_d = inp("xc", [128, KC, 1024])
        cond_d = inp("cond", [128, KC, 2])
        ada_w_d = inp("ada_w", [L, D, 6 * D])
        ada_b_d = inp("ada_b", [128, L, 48])
        g1_d = inp("g1", [128, L, KC])
        g2_d = inp("g2", [128, L, KC])
        gf_d = inp("gf", [128, KC])
        cst_d = inp("cst", [128, 128 + 128 + 256])
        pool_w_d = inp("pool_w", [4, 256, 256])
        pool_s_d = inp("pool_s", [128, KC])
        pool_rc_d = inp("pool_rc", [128, 4, 16])
        mla_wd_d = inp("mla_wd", [D, 704])
        mla_gq_d = inp("mla_gq", [128, 384])
        mla_wuq_d = inp("mla_wuq", [384, 2560])
        mla_gkv_d = inp("mla_gkv", [128, 256])
        mla_wukv_d = inp("mla_wukv", [256, 2048])
        mla_wo_d = inp("mla_wo", [D, D])
        mla_cckv_d = inp("mla_cckv", [512, 256])
        mla_ckr_d = inp("mla_ckr", [512, 64])
        mla_rtok_d = inp("mla_rtok", [128, 2, 16, 32])
        mla_rA_d = inp("mla_rA", [64, 2, 32])
        mla_rB_d = inp("mla_rB", [64, 2, 64])
        cv_w1_d = inp("cv_w1", [D, 2 * D])
        cv_b1_d = inp("cv_b1", [128, 16])
        cv_wdw_d = inp("cv_wdw", [128, KC, 31])
        cv_vec_d = inp("cv_vec", [128, 4, KC])
        cv_w2_d = inp("cv_w2", [D, D])
        gq_w_d = inp("gq_w", [D, 1536])
        gq_g_d = inp("gq_g", [128, 2, 128])
        gq_wo_d = inp("gq_wo", [D, D])
        gq_ck_d = inp("gq_ck", [512, 256])
        gq_cv_d = inp("gq_cv", [512, 256])
        gq_rtok_d = inp("gq_rtok", [128, 2, 16, 64])
        rw_d = inp("rw", [128, L, KC, NE])
        wg_d = inp("moe_wg", [L, NE, D, FF])
        wu_d = inp("moe_wu", [L, NE, D, FF])
        wd_d = inp("moe_wd", [L, NE, FF, D])
        wscr_d = self.nc.dram_tensor("wscr", [L, NE, 128, 12288], BF16, kind="Internal").ap()
        self.wscr_rs = [Res(f"wscr{i}") for i in range(L)]
        yl_d = outp("yl", [128, KC, 2048])
        yc_d = outp("yc", [128, KC, 1024])
        st_ckv_d = outp("st_ckv", [1024, 256])
        st_kr_d = outp("st_kr", [1024, 64])
        st_k_d = outp("st_k", [1024, 256])
        st_v_d = outp("st_v", [1024, 256])
        self.__dict__.update(locals())

        self.X = kb.sbuf("X", [128, KC, 2048], F32)
        self.Xr = [Res(f"X{b}") for b in range(4)]
        self.cst = kb.sbuf("cstt", [128, 512], F32)
        self.c_r = Res("cst")
        self.ident = self.cst[:, 0:128]
        self.onesf = self.cst[:, 128:256]
        self.iota = self.cst[:, 256:512]
        self.cb = kb.sbuf("cb", [128, 640], BF16)
        self.identb = self.cb[:, 0:128]
        self.onesb_s = self.cb[:, 128:256]
        self.onesb = self.cb[:, 256:384]
        self.iotab = self.cb[:, 384:640]
        self.eps_t = kb.sbuf("eps", [128, 1], F32)
        self.mods = kb.sbuf("mods", [128, L, 48, 2], F32)
        self.mod_rs = [Res(f"mods{i}") for i in range(L)]
        self.mod_r = self.mod_rs[0]
        self.cnd = kb.sbuf("cnd", [128, KC, 2], F32)[:, :, :]
        self.cndb = kb.sbuf("cndb", [128, KC, 2], BF16)[:, :, :]
        self.adab = kb.sbuf("adab", [128, L, 48], F32)[:, :, :]
        self.cnd_r = Res("cnd")
        self.gs = kb.sbuf("gs", [128, L, 2, KC, 2], F32)
        self.vecs = kb.sbuf("vecs", [128, 3 * L * KC + KC + KC], F32)
        self.g1 = self.vecs[:, 0:L * KC].rearrange("p (l m) -> p l m", l=L)
        self.g2 = self.vecs[:, L * KC:2 * L * KC].rearrange("p (l m) -> p l m", l=L)
        self.gf = self.vecs[:, 3 * L * KC:3 * L * KC + KC]
        self.ps = [kb.psum(f"ps{i}", [128, 512], F32) for i in range(8)]
        self.psr = [Res(f"ps{i}", excl=True) for i in range(8)]
        self.ps_i = 0
        self.ps_pinned = set()
        self.ar = Arena(kb, 136 * 1024)

        kb.dma("sp", [(self.cst[:], cst_d[:, :])], "d_c", writes=[self.c_r])
        kb.dma("sp", [(self.g1, g1_d[:, :, :]), (self.g2, g2_d[:, :, :]), (self.gf, gf_d[:, :])], "d_c", writes=[self.c_r])
        kb.op("dve", lambda e: e.memset(self.eps_t[:], EPS), writes=[self.c_r])
        kb.op("dve", lambda e: e.tensor_copy(out=self.identb, in_=self.ident), reads=[self.c_r], writes=[self.c_r])
        kb.op("dve", lambda e: e.tensor_copy(out=self.onesb_s, in_=self.onesf), reads=[self.c_r], writes=[self.c_r])
        kb.op("dve", lambda e: e.memset(self.onesb, 1.0), writes=[self.c_r])
        kb.op("dve", lambda e: e.tensor_copy(out=self.iotab, in_=self.iota), reads=[self.c_r], writes=[self.c_r])

        self.adaln_setup()
        for grp in (Grp("L", 2048, 1, 2048, 0), Grp("C", 1024, 4, 256, 1)):
            if grp.name in getattr(self, "groups", "LC"):
                self.run_group(grp)
        kb.barrier()

    def adaln_setup(self):
        self.load(self.cnd, self.cond_d[:, :, :], self.cnd_r)
        self.load(self.adab, self.ada_b_d[:, :, :], self.cnd_r)
        self.act(self.cndb, self.cnd, AF.Silu, reads=[self.cnd_r], writes=[self.cnd_r])

    def adaln_issue(self, l, wb):
        ps, psr = self.next_ps(pin=True)
        for jb in range(6):
            w, w_r = wb[jb % 2]
            self.loadw(w, self.ada_w_d[l, :, jb * 1024:(jb + 1) * 1024].rearrange("(k p) n -> p k n", p=128), w_r, f"d_a{jb % 2}")
            for jj in range(8):
                j = jb * 8 + jj
                for k in range(KC):
                    self.mm(ps[:, 2 * j:2 * j + 2], w[:, k, jj * 128:(jj + 1) * 128], self.cndb[:, k, :],
                            k == 0, k == KC - 1, reads=[w_r, self.cnd_r], writes=[psr])
        return ps, psr

    def adaln_finish(self, l, ps, psr):
        mr = self.mod_rs[l]
        self.tt(self.mods[:, l, :, :], ps[:, 0:96].rearrange("p (j c) -> p j c", c=2),
                self.adab[:, l, :].unsqueeze(2).broadcast_to([128, 48, 2]), ALU.add,
                reads=[psr, self.cnd_r], writes=[mr])
        self.unpin(psr)
        for which, g in ((0, self.g1), (1, self.g2)):
            j0 = 8 + 24 * which
            self.stt(self.gs[:, l, which, :, :], self.mods[:, l, j0:j0 + 8, :], 1.0,
                     g[:, l, :].unsqueeze(2).broadcast_to([128, KC, 2]), ALU.add, ALU.mult,
                     reads=[mr, self.c_r], writes=[mr])

    def shift(self, l, which, m, ci):
        return self.mods[:, l, 24 * which + m, ci:ci + 1]

    def gate(self, l, which, m, ci):
        return self.mods[:, l, 16 + 24 * which + m, ci:ci + 1]

    def norm_block(self, b, gs_fn, sh_fn, out_fn, out_r, tmps, precise=False):
        X, Xr = self.X, self.Xr
        sq, sq_r, lnv, lnv_r, rstd, rstd_r, tm, tm_r = tmps
        tb = slice(b * 512, (b + 1) * 512)
        ps, psr = self.next_ps()
        for m in range(KC):
            if precise:
                s_ = tm[m % 2]
                s_r = tm_r[m % 2]
                self.tt(s_, X[:, m, tb], X[:, m, tb], ALU.mult, reads=[Xr[b]], writes=[s_r])
                self.mm(ps[:, :], self.onesf, s_, m == 0, m == KC - 1, reads=[s_r, self.c_r], writes=[psr])
            else:
                self.tt(sq[m % 2], X[:, m, tb], X[:, m, tb], ALU.mult, reads=[Xr[b]], writes=[sq_r[m % 2]])
                self.mm(ps[:, :], self.onesb_s, sq[m % 2], m == 0, m == KC - 1, reads=[sq_r[m % 2], self.c_r], writes=[psr])
        self.act(lnv, ps[:, :], AF.Ln, reads=[psr, self.c_r], writes=[lnv_r], bias=self.eps_t[:, :])
        self.act(rstd, lnv, AF.Exp, reads=[lnv_r], writes=[rstd_r], scale=-0.5)
        self.tt(lnv, rstd, rstd, ALU.mult, reads=[rstd_r], writes=[lnv_r])
        self.stt(lnv, ps[:, :], EPS, lnv, ALU.add, ALU.mult, reads=[psr, lnv_r], writes=[lnv_r])
        self.ts(lnv, lnv, -0.5, 1.5, ALU.mult, ALU.add, reads=[lnv_r], writes=[lnv_r])
        self.tt(rstd, rstd, lnv, ALU.mult, reads=[rstd_r, lnv_r], writes=[rstd_r])
        for m in range(KC):
            self.tt(tm[m % 2], X[:, m, tb], rstd, ALU.mult, reads=[Xr[b], rstd_r], writes=[tm_r[m % 2]])
            if gs_fn is None:
                self.cp("act", out_fn(m), tm[m % 2], reads=[tm_r[m % 2]], writes=[out_r])
            else:
                self.act(out_fn(m), tm[m % 2], AF.Identity, reads=[tm_r[m % 2], self.mod_r], writes=[out_r],
                         bias=sh_fn(m), scale=gs_fn(m))

    def norm_tmps(self):
        ar = self.ar
        sq0, r0 = ar.alloc("sq0", [512], BF16)
        sq1, r1 = ar.alloc("sq1", [512], BF16)
        lnv, lr = ar.alloc("lnv", [512], F32)
        rstd, rr = ar.alloc("rstd", [512], F32)
        t0, tr0 = ar.alloc("tm0", [512], F32)
        t1, tr1 = ar.alloc("tm1", [512], F32)
        return ([sq0, sq1], [r0, r1], lnv, lr, rstd, rr, [t0, t1], [tr0, tr1])

    def norm1(self, g, l):
        ar = self.ar
        hT, hT_r = ar.alloc("hT", [KC, g.T], BF16)
        mk = ar.mark()
        tmps = self.norm_tmps()
        for b in range(g.NB):
            self.norm_block(b, lambda m: self.gs[:, l, 0, m, g.ci:g.ci + 1], lambda m: self.shift(l, 0, m, g.ci),
                            lambda m: hT[:, m, b * 512:(b + 1) * 512], hT_r, tmps)
        ar.release(mk)
        return hT, hT_r

    def resid(self, b, m, cols, ps_ap, psr, gate_ap, extra_reads=()):
        self.stt(self.X[:, m, cols], ps_ap, gate_ap, self.X[:, m, cols], ALU.mult, ALU.add,
                 reads=[psr, self.Xr[b], self.mod_r] + list(extra_reads), writes=[self.Xr[b]])

    def run_group(self, g):
        kb, ar = self.kb, self.ar
        ar.reset()
        src = self.xl_d if g.lat else self.xc_d
        for b in range(g.NB):
            kb.dma("sp", [(self.X[:, :, b * 512:(b + 1) * 512], src[:, :, b * 512:(b + 1) * 512])], "d_x", writes=[self.Xr[b]])
        for l in range(self.depth):
            ar.reset()
            self.mod_r = self.mod_rs[l]
            if g.lat and l == 0:
                wb = [ar.alloc(f"adaw{i}", [KC, 1024], BF16) for i in range(2)]
                self.adaln_finish(0, *self.adaln_issue(0, wb))
                ar.reset()
            kind = l % 4
            if not self.do_mixer:
                pass
            elif kind == 0:
                self.pool_mixer(g, l)
            elif kind == 1:
                self.mla(g, l)
            elif kind == 2:
                self.conv(g, l)
            else:
                self.gqa(g, l)
            ar.reset()
            if self.do_moe:
                self.moe(g, l)
        ar.reset()
        tmps = self.norm_tmps()
        yb = [ar.alloc(f"yb{i}", [KC, 512], F32) for i in range(2)]
        dst = self.yl_d if g.lat else self.yc_d
        for b in range(g.NB):
            y, y_r = yb[b % 2]
            self.norm_block(b, lambda m: self.gf[:, m:m + 1], lambda m: 0.0, lambda m: y[:, m, :], y_r, tmps)
            kb.dma("sp", [(dst[:, :, b * 512:(b + 1) * 512], y[:, :, :])], "d_y", reads=[y_r])

    def pool_mixer(self, g, l):
        kb, ar = self.kb, self.ar
        hT, hT_r = self.norm1(g, l)
        nseq, S = g.nseq, g.S
        W = S + 16
        pw, pw_r = ar.alloc("pw", [4, 2, 256], BF16)
        psc, psc_r = ar.alloc("psc", [KC], F32)
        prc, prc_r = ar.alloc("prc", [4, 16], F32)
        gsc, gsc_r = ar.alloc("gsc", [KC], F32)
        self.loadw(pw, self.pool_w_d.rearrange("g (k p) n -> p g k n", p=128), pw_r, "d_w0")
        self.load(psc, self.pool_s_d[:, :], psc_r)
        self.load(prc, self.pool_rc_d[:, :, :], prc_r)
        self.tt(gsc, self.mods[:, l, 16:24, g.ci], psc, ALU.mult, reads=[self.mod_r, psc_r], writes=[gsc_r])
        lv = [ar.alloc(f"lv{i}", [nseq, W], F32) for i in range(5)]
        pooled, pooled_r = ar.alloc("pooled", [2, nseq, S], BF16)
        tmpb, tmpb_r = ar.alloc("tmpb", [nseq, 16], F32)
        kb.op("dve", lambda e: e.memset(lv[0][0].rearrange("p a b -> p (a b)"), 0.0), writes=[lv[0][1]])
        for gi in range(4):
            win = 2 << gi
            for kk in range(2):
                m = 2 * gi + kk
                hv = hT[:, m, :].rearrange("p (s t) -> p s t", s=nseq)
                P0, P0r = lv[0]
                self.cp("act", P0[:, :, 8:8 + S], hv, reads=[hT_r], writes=[P0r])
                A, Ar = lv[1]
                self.tt(A[:, :, 1:W], P0[:, :, 1:W], P0[:, :, 0:W - 1], ALU.add, reads=[P0r], writes=[Ar])
                sh = 1
                lo, hi = 1, W
                for level in range(2, gi + 2):
                    Bv, Br = lv[level]
                    lo2, hi2 = lo + sh, hi - sh
                    self.tt(Bv[:, :, lo2:hi2], A[:, :, lo2 - sh:hi2 - sh], A[:, :, lo2 + sh:hi2 + sh], ALU.add,
                            reads=[Ar], writes=[Br])
                    A, Ar = Bv, Br
                    lo, hi = lo2, hi2
                    sh *= 2
                pv = pooled[:, kk, :, :]
                self.stt(pv, A[:, :, 8:8 + S], 1.0 / win, hv, ALU.mult, ALU.subtract, reads=[Ar, hT_r], writes=[pooled_r])
                for side, c0 in ((0, 0), (1, S - 8)):
                    self.tt(tmpb[:, :, 0:8], A[:, :, 8 + c0:16 + c0],
                            prc[:, gi, side * 8:side * 8 + 8].unsqueeze(1).broadcast_to([128, nseq, 8]), ALU.mult,
                            reads=[Ar, prc_r], writes=[tmpb_r])
                    self.tt(pv[:, :, c0:c0 + 8], tmpb[:, :, 0:8], hv[:, :, c0:c0 + 8], ALU.subtract,
                            reads=[tmpb_r, hT_r], writes=[pooled_r])
            pf = pooled.rearrange("p k s t -> p k (s t)")
            for b in range(g.NB):
                tb = slice(b * 512, (b + 1) * 512)
                for mo in range(2):
                    m = 2 * gi + mo
                    ps, psr = self.next_ps()
                    for k in range(2):
                        self.mm(ps[:, :], pw[:, gi, k, mo * 128:(mo + 1) * 128], pf[:, k, tb], k == 0, k == 1,
                                reads=[pw_r, pooled_r], writes=[psr])
                    self.resid(b, m, tb, ps[:, :], psr, gsc[:, m:m + 1], extra_reads=[gsc_r])

    def attn_jobs(self, jobs, scale, reads, ebuf, LA=2):
        E, E_r = ebuf
        nE = len(E)
        sps = [self.next_ps(pin=True) for _ in range(nE)]
        od = [(self.next_ps(pin=True), self.next_ps(pin=True)) for _ in range(2)]
        flat = [(j, i) for j, job in enumerate(jobs) for i in range(len(job[1]))]

        def issue_s(n):
            j, i = flat[n]
            NQ, chunks = jobs[j][0], jobs[j][1]
            xr = list(jobs[j][4]) if len(jobs[j]) > 4 else []
            ps, psr = sps[n % nE]
            kparts, _ = chunks[i]
            for pi, (kT, qT) in enumerate(kparts):
                self.mm(ps[:, 0:NQ], kT, qT, pi == 0, pi == len(kparts) - 1, reads=reads + xr, writes=[psr])

        for n in range(min(LA, len(flat))):
            issue_s(n)
        rd, rd_r = self.rden
        for n, (j, i) in enumerate(flat):
            if n + LA < len(flat):
                issue_s(n + LA)
            NQ, chunks, out_ap, out_r = jobs[j][0:4]
            nch = len(chunks)
            ps, psr = sps[n % nE]
            (ops, opr), (dps, dpr) = od[j % 2]
            self.act(E[n % nE][:, 0:NQ], ps[:, 0:NQ], AF.Exp, reads=[psr], writes=[E_r[n % nE]], scale=scale)
            _, v = chunks[i]
            self.mm(ops[:, 0:NQ], v, E[n % nE][:, 0:NQ], i == 0, i == nch - 1, reads=reads + [E_r[n % nE]], writes=[opr])
            self.mm(dps[:, 0:NQ], self.onesb, E[n % nE][:, 0:NQ], i == 0, i == nch - 1, reads=[E_r[n % nE], self.c_r], writes=[dpr])
            if i == nch - 1:
                self.kb.op("dve", lambda e: e.reciprocal(out=rd[:, 0:NQ], in_=dps[:, 0:NQ]), reads=[dpr], writes=[rd_r])
                self.tt(out_ap, ops[:, 0:NQ], rd[:, 0:NQ], ALU.mult, reads=[opr, rd_r], writes=[out_r])
        for (_, r) in sps:
            self.unpin(r)
        for (a, b) in od:
            self.unpin(a[1])
            self.unpin(b[1])

    def mla(self, g, l):
        kb, ar = self.kb, self.ar
        T, NT = g.T, g.NT
        hT, hT_r = self.norm1(g, l)
        NKC = 512 if g.lat else 0
        NK = NKC + T
        cqnT, cqnT_r = ar.alloc("cqnT", [3, T], BF16)
        ckvT, ckvT_r = ar.alloc("ckvT", [2, NK], BF16)
        krT, krT_r = ar.alloc("krT", [NK], BF16)
        kb.op("dve", lambda e: e.memset(krT, 0.0), writes=[krT_r])
        wuq, wuq_r = ar.alloc("wuq", [3, 2560], BF16)
        wukv, wukv_r = ar.alloc("wukv", [2, 2048], BF16)
        self.loadw(wuq, self.mla_wuq_d.rearrange("(k p) n -> p k n", p=128), wuq_r, "d_w1")
        self.loadw(wukv, self.mla_wukv_d.rearrange("(k p) n -> p k n", p=128), wukv_r, "d_w1")
        mkA = ar.mark()
        wd, wd_r = ar.alloc("wd", [KC, 704], BF16)
        gq, gq_r = ar.alloc("gq", [384], F32)
        gkv, gkv_r = ar.alloc("gkv", [256], F32)
        self.loadw(wd, self.mla_wd_d.rearrange("(k p) n -> p k n", p=128), wd_r, "d_w0")
        self.load(gq, self.mla_gq_d[:, :], gq_r)
        self.load(gkv, self.mla_gkv_d[:, :], gkv_r)
        _jk = [ar.alloc(f"junk{i}", [384], F32) for i in range(2)]
        _st = [ar.alloc(f"st{i}", [8], F32) for i in range(2)]
        cqn = [ar.alloc(f"cqn{i}", [384], BF16) for i in range(2)]
        ckn = [ar.alloc(f"ckn{i}", [256], F32) for i in range(2)]
        cknb = [ar.alloc(f"cknb{i}", [384], BF16) for i in range(2)]
        for i in range(2):
            kb.op("dve", lambda e: e.memset(cknb[i][0], 0.0), writes=[cknb[i][1]])
        krf = [ar.alloc(f"krf{i}", [64], F32) for i in range(2)]
        if self.stop == "A00":
            return
        if g.lat:
            rt, rt_r = ar.alloc("rt", [2, 16, 32], F32)
            self.load(rt, self.mla_rtok_d[:, :, :, :], rt_r)
            _rt2 = [ar.alloc(f"rtmp{i}", [4, 32], F32) for i in range(2)]
            cc, cc_r = ar.alloc("cc", [4, 384], BF16)
            kb.op("dve", lambda e: e.memset(cc.rearrange("p a b -> p (a b)"), 0.0), writes=[cc_r])
            self.loadw(cc[:, :, 0:256], self.mla_cckv_d.rearrange("(j p) c -> p j c", p=128), cc_r, "d_w2")
            self.loadw(cc[:, :, 256:320], self.mla_ckr_d.rearrange("(j p) c -> p j c", p=128), cc_r, "d_w2")
            if self.stop == "A01":
                return
            for j in range(4):
                ps, psr = self.next_ps()
                pb = ps[:, :].bitcast(BF16)
                for k in range(2):
                    self.tr(pb[:, k * 128:(k + 1) * 128], cc[:, j, k * 128:(k + 1) * 128], self.identb, reads=[cc_r, self.c_r], writes=[psr])
                self.tr(pb[:, 256:384], cc[:, j, 256:384], self.identb, reads=[cc_r, self.c_r], writes=[psr])
                self.cp("act", ckvT[:, :, j * 128:(j + 1) * 128], pb[:, 0:256].rearrange("p (k t) -> p k t", k=2), reads=[psr], writes=[ckvT_r])
                self.cp("act", krT[0:64, j * 128:(j + 1) * 128], pb[0:64, 256:384], reads=[psr], writes=[krT_r])
        if self.stop == "A0":
            return
        def tile_gen(tt_):
            tsl = slice(tt_ * 128, (tt_ + 1) * 128)
            junk, junk_r = _jk[tt_ % 2]
            st, st_r = _st[tt_ % 2]
            if g.lat:
                rtmp, rtmp_r = _rt2[tt_ % 2]
            p1, p1r = self.next_ps()
            p2, p2r = self.next_ps()
            for k in range(KC):
                self.mm(p1[:, 0:384], hT[:, k, tsl], wd[:, k, 0:384], k == 0, k == KC - 1, reads=[hT_r, wd_r], writes=[p1r])
            for k in range(KC):
                self.mm(p2[:, 0:320], hT[:, k, tsl], wd[:, k, 384:704], k == 0, k == KC - 1, reads=[hT_r, wd_r], writes=[p2r])
            self.act(junk[:, 0:384], p1[:, 0:384], AF.Square, reads=[p1r], writes=[junk_r, st_r], accum_out=st[:, 0:1])
            self.act(junk[:, 0:256], p2[:, 0:256], AF.Square, reads=[p2r], writes=[junk_r, st_r], accum_out=st[:, 1:2])
            self.act(st[:, 2:3], st[:, 0:1], AF.Ln, reads=[st_r, self.c_r], writes=[st_r], bias=self.eps_t[:, :], scale=1.0 / 384)
            self.act(st[:, 3:4], st[:, 1:2], AF.Ln, reads=[st_r, self.c_r], writes=[st_r], bias=self.eps_t[:, :], scale=1.0 / 256)
            self.act(st[:, 4:6], st[:, 2:4], AF.Exp, reads=[st_r], writes=[st_r], scale=-0.5)
            if self.stop == "A1":
                return
            cq, cq_r = cqn[tt_ % 2]
            ck, ck_r = ckn[tt_ % 2]
            ckb, ckb_r = cknb[tt_ % 2]
            kf, kf_r = krf[tt_ % 2]
            self.stt(cq, p1[:, 0:384], st[:, 4:5], gq, ALU.mult, ALU.mult, reads=[p1r, st_r, gq_r], writes=[cq_r])
            self.stt(ck, p2[:, 0:256], st[:, 5:6], gkv, ALU.mult, ALU.mult, reads=[p2r, st_r, gkv_r], writes=[ck_r])
            self.cp("act", ckb[:, 0:256], ck, reads=[ck_r], writes=[ckb_r])
            self.cp("act", kf, p2[:, 256:320], reads=[p2r], writes=[kf_r])
            if g.lat:
                c_, s_ = rt[:, 0, tt_, :], rt[:, 1, tt_, :]
                x1, x2 = kf[:, 0:32], kf[:, 32:64]
                self.tt(rtmp[:, 0, :], x1, c_, ALU.mult, reads=[kf_r, rt_r], writes=[rtmp_r])
                self.tt(rtmp[:, 1, :], x2, s_, ALU.mult, reads=[kf_r, rt_r], writes=[rtmp_r])
                self.tt(rtmp[:, 2, :], x1, s_, ALU.mult, reads=[kf_r, rt_r], writes=[rtmp_r])
                self.tt(rtmp[:, 3, :], x2, c_, ALU.mult, reads=[kf_r, rt_r], writes=[rtmp_r])
                self.tt(ckb[:, 256:288], rtmp[:, 0, :], rtmp[:, 1, :], ALU.subtract, reads=[rtmp_r], writes=[ckb_r])
                self.tt(ckb[:, 288:320], rtmp[:, 2, :], rtmp[:, 3, :], ALU.add, reads=[rtmp_r], writes=[ckb_r])
            else:
                self.cp("dve", ckb[:, 256:320], kf, reads=[kf_r], writes=[ckb_r])
                kb.dma("sp", [(self.st_ckv_d[tsl, :], ck)], "d_st", reads=[ck_r])
                kb.dma("sp", [(self.st_kr_d[tsl, :], kf)], "d_st", reads=[kf_r])
            yield
            if self.stop == "A2":
                return
            ps, psr = self.next_ps()
            pb = ps[:, :].bitcast(BF16)
            for k in range(3):
                self.tr(pb[:, k * 128:(k + 1) * 128], cq[:, k * 128:(k + 1) * 128], self.identb, reads=[cq_r, self.c_r], writes=[psr])
            for k in range(2):
                self.tr(pb[:, (3 + k) * 128:(4 + k) * 128], ckb[:, k * 128:(k + 1) * 128], self.identb, reads=[ckb_r, self.c_r], writes=[psr])
            self.tr(pb[:, 640:768], ckb[:, 256:384], self.identb, reads=[ckb_r, self.c_r], writes=[psr])
            ksl = slice(NKC + tt_ * 128, NKC + (tt_ + 1) * 128)
            self.cp("act", cqnT[:, :, tsl], pb[:, 0:384].rearrange("p (k t) -> p k t", k=3), reads=[psr], writes=[cqnT_r])
            self.cp("act", ckvT[:, :, ksl], pb[:, 384:640].rearrange("p (k t) -> p k t", k=2), reads=[psr], writes=[ckvT_r])
            self.cp("act", krT[0:64, ksl], pb[0:64, 640:768], reads=[psr], writes=[krT_r])
        _cur = tile_gen(0)
        next(_cur)
        for tt_ in range(NT):
            _nxt = None
            if tt_ + 1 < NT:
                _nxt = tile_gen(tt_ + 1)
                next(_nxt)
            for _ in _cur:
                pass
            _cur = _nxt
        if self.stop == "A":
            return
        ar.release(mkA)
        attnT, attnT_r = hT, hT_r
        wo, wo_r = ar.alloc("wo", [KC, D], BF16)
        self.loadw(wo, self.mla_wo_d.rearrange("(k p) n -> p k n", p=128), wo_r, "d_w0")
        knT, knT_r = ar.alloc("knT", [NK], BF16)
        vh, vh_r = ar.alloc("vh", [NK // 128, 128], BF16)
        qn, qn_r = ar.alloc("qn", [T], BF16)
        qr, qr_r = ar.alloc("qr", [T], BF16)
        _eb = [ar.alloc(f"E{i}", [512], BF16) for i in range(3)]
        EB, EBr = [x[0] for x in _eb], [x[1] for x in _eb]
        self.rden = ar.alloc("rden", [512], F32)
        if g.lat:
            qrot, qrot_r = ar.alloc("qrot", [T], BF16)
            kb.op("dve", lambda e: e.memset(qrot, 0.0), writes=[qrot_r])
            rA, rA_r = ar.alloc("rA", [2, 32], F32)
            rB, rB_r = ar.alloc("rB", [2, 64], F32)
            self.load(rA[0:64], self.mla_rA_d[:, :, :], rA_r)
            self.load(rB[0:64], self.mla_rB_d[:, :, :], rB_r)
            t1, t1_r = ar.alloc("t1", [512], F32)
            t2, t2_r = ar.alloc("t2", [512], F32)
        scale = 192 ** -0.5
        qres = [[Res(f"qn{b}"), Res(f"qr{b}"), Res(f"qo{b}")] for b in range(g.NB)]
        for h in range(8):
            c0 = h * 256
            q0 = h * 320
            for kb0 in range(0, NK, 512):
                ps, psr = self.next_ps()
                for k in range(2):
                    self.mm(ps[:, :], wukv[:, k, c0:c0 + 128], ckvT[:, k, kb0:kb0 + 512], k == 0, k == 1, reads=[wukv_r, ckvT_r], writes=[psr])
                self.cp("act", knT[:, kb0:kb0 + 512], ps[:, :], reads=[psr], writes=[knT_r])
            for kc0 in range(0, NK // 128, 4):
                ps, psr = self.next_ps()
                for kk in range(4):
                    kc = kc0 + kk
                    for k in range(2):
                        self.mm(ps[:, kk * 128:(kk + 1) * 128], ckvT[:, k, kc * 128:(kc + 1) * 128], wukv[:, k, c0 + 128:c0 + 256],
                                k == 0, k == 1, reads=[wukv_r, ckvT_r], writes=[psr])
                self.cp("dve", vh[:, kc0:kc0 + 4, :], ps[:, :].rearrange("p (a b) -> p a b", a=4), reads=[psr], writes=[vh_r])
            if self.stop == "B0a":
                continue
            for b in range(g.NB):
                tb = slice(b * 512, (b + 1) * 512)
                ps, psr = self.next_ps()
                for k in range(3):
                    self.mm(ps[:, :], wuq[:, k, q0:q0 + 128], cqnT[:, k, tb], k == 0, k == 2, reads=[wuq_r, cqnT_r], writes=[psr])
                self.cp("act", qn[:, tb], ps[:, :], reads=[psr], writes=[qres[b][0]])
                ps, psr = self.next_ps()
                for k in range(3):
                    self.mm(ps[:, :], wuq[:, k, q0 + 128:q0 + 256], cqnT[:, k, tb], k == 0, k == 2, reads=[wuq_r, cqnT_r], writes=[psr])
                self.cp("act", qr[:, tb], ps[:, :], reads=[psr], writes=[qres[b][1]])
                if g.lat and self.stop != "B0b":
                    ps2, ps2r = self.next_ps()
                    for k in range(3):
                        self.mm(ps2[:, :], wuq[:, k, q0 + 192:q0 + 320], cqnT[:, k, tb], k == 0, k == 2, reads=[wuq_r, cqnT_r], writes=[ps2r])
                    r0 = b * 8
                    for rr in range(8):
                        sg_ = slice(rr * 64, (rr + 1) * 64)
                        self.stt(t1[0:64, sg_], ps[0:64, sg_], rA[0:64, 0, r0 + rr:r0 + rr + 1], rB[0:64, 0, :], ALU.mult, ALU.mult,
                                 reads=[psr, rA_r, rB_r], writes=[t1_r])
                        self.stt(t2[0:64, sg_], ps2[0:64, sg_], rA[0:64, 1, r0 + rr:r0 + rr + 1], rB[0:64, 1, :], ALU.mult, ALU.mult,
                                 reads=[ps2r, rA_r, rB_r], writes=[t2_r])
                    self.tt(qrot[0:64, tb], t1[0:64, :], t2[0:64, :], ALU.add, reads=[t1_r, t2_r, qrot_r], writes=[qres[b][2]])
            if self.stop in ("B0", "B0b"):
                continue
            rds = [knT_r, vh_r, krT_r]
            jobs = []
            if g.lat:
                for b in range(4):
                    qs = slice(b * 512, (b + 1) * 512)
                    chunks = []
                    for kc in range(NK // 128):
                        ks = slice(kc * 128, (kc + 1) * 128)
                        qrp = qr if kc < 4 else qrot
                        chunks.append(([(knT[:, ks], qn[:, qs]), (krT[:, ks], qrp[:, qs])], vh[:, kc, :]))
                    jobs.append((512, chunks, attnT[:, h, qs], attnT_r, qres[b]))
            else:
                for s_ in range(4):
                    qs = slice(s_ * 256, (s_ + 1) * 256)
                    chunks = []
                    for kc in range(2 * s_, 2 * s_ + 2):
                        ks = slice(kc * 128, (kc + 1) * 128)
                        chunks.append(([(knT[:, ks], qn[:, qs]), (krT[:, ks], qr[:, qs])], vh[:, kc, :]))
                    jobs.append((256, chunks, attnT[:, h, qs], attnT_r, qres[s_ // 2][0:2]))
            self.attn_jobs(jobs, scale, rds, (EB, EBr))
        if self.stop in ("B0", "B", "B0a", "B0b"):
            return
        for b in range(g.NB):
            tb = slice(b * 512, (b + 1) * 512)
            for m in range(KC):
                ps, psr = self.next_ps()
                for h in range(8):
                    self.mm(ps[:, :], wo[:, h, m * 128:(m + 1) * 128], attnT[:, h, tb], h == 0, h == 7, reads=[wo_r, attnT_r], writes=[psr])
                self.resid(b, m, tb, ps[:, :], psr, self.gate(l, 0, m, g.ci))

    def conv(self, g, l):
        kb, ar = self.kb, self.ar
        T, nseq, S = g.T, g.nseq, g.S
        W = S + 30
        glu, glu_r = ar.alloc("glu", [KC, nseq, W], BF16, top=True)
        vec, vec_r = ar.alloc("cvvec", [4, KC], F32)
        b1, b1_r = ar.alloc("cvb1", [16], F32)
        wdw, wdw_r = ar.alloc("wdw", [KC, 31], F32)
        self.load(vec, self.cv_vec_d[:, :, :], vec_r)
        self.load(b1, self.cv_b1_d[:, :], b1_r)
        self.load(wdw, self.cv_wdw_d[:, :, :], wdw_r)
        mk0 = ar.mark()
        hT, hT_r = self.norm1(g, l)
        w1, w1_r = ar.alloc("w1", [KC, 2048], BF16)
        self.loadw(w1[:, :, 0:1024], self.cv_w1_d[:, 0:1024].rearrange("(k p) n -> p k n", p=128), w1_r, "d_w0")
        self.loadw(w1[:, :, 1024:2048], self.cv_w1_d[:, 1024:2048].rearrange("(k p) n -> p k n", p=128), w1_r, "d_w0")
        sig, sig_r = ar.alloc("sig", [512], F32)
        kb.op("dve", lambda e: e.memset(glu.rearrange("p a b c -> p (a b c)"), 0.0), writes=[glu_r])
        NQ = 512
        spb = NQ // S if S < NQ else 1
        for b in range(g.NB):
            tb = slice(b * 512, (b + 1) * 512)
            for m in range(KC):
                pa, par = self.next_ps()
                pg, pgr = self.next_ps()
                for k in range(KC):
                    self.mm(pa[:, :], w1[:, k, m * 128:(m + 1) * 128], hT[:, k, tb], k == 0, k == KC - 1, reads=[w1_r, hT_r], writes=[par])
                for k in range(KC):
                    self.mm(pg[:, :], w1[:, k, 1024 + m * 128:1024 + (m + 1) * 128], hT[:, k, tb], k == 0, k == KC - 1, reads=[w1_r, hT_r], writes=[pgr])
                self.act(sig, pg[:, :], AF.Sigmoid, reads=[pgr, b1_r], writes=[sig_r], bias=b1[:, 8 + m:9 + m])
                if g.lat:
                    dst = glu[:, m, 0, 15 + b * 512:15 + (b + 1) * 512]
                    self.stt(dst, pa[:, :], b1[:, m:m + 1], sig, ALU.add, ALU.mult, reads=[par, b1_r, sig_r], writes=[glu_r])
                else:
                    dst = glu[:, m, 2 * b:2 * b + 2, 15:15 + S]
                    self.stt(dst, pa[:, :].rearrange("p (s t) -> p s t", s=2), b1[:, m:m + 1],
                             sig.rearrange("p (s t) -> p s t", s=2), ALU.add, ALU.mult, reads=[par, b1_r, sig_r], writes=[glu_r])
        ar.release(mk0)
        cv, cv_r = ar.alloc("cv", [KC, T], F32)
        mkd = ar.mark()
        dg = [ar.alloc(f"dg{i}", [31, 128], BF16) for i in range(2)]
        for m in range(KC):
            dgt, dg_r = dg[m % 2]
            for j in range(31):
                self.ts(dgt[:, j, :], self.identb, wdw[:, m, j:j + 1], None, ALU.mult, None, reads=[wdw_r, self.c_r], writes=[dg_r])
            for b in range(g.NB):
                ps, psr = self.next_ps()
                for j in range(31):
                    if g.lat:
                        rhs = glu[:, m, 0, b * 512 + j:b * 512 + j + 512]
                        out = ps[:, :]
                    else:
                        rhs = glu[:, m, 2 * b:2 * b + 2, j:j + S]
                        out = ps[:, :].rearrange("p (s t) -> p s t", s=2)
                    self.mm(out, dgt[:, j, :], rhs, j == 0, j == 30, reads=[dg_r, glu_r], writes=[psr])
                self.act(cv[:, m, b * 512:(b + 1) * 512], ps[:, :], AF.Identity, reads=[psr, vec_r], writes=[cv_r], bias=vec[:, 0, m:m + 1])
        ar.release(mkd)
        ar.release_top()
        w2, w2_r = ar.alloc("w2", [KC, D], BF16)
        self.loadw(w2, self.cv_w2_d.rearrange("(k p) n -> p k n", p=128), w2_r, "d_w1")
        sqf = [ar.alloc(f"sqf{i}", [512], F32) for i in range(2)]
        mean, mean_r = ar.alloc("mean", [512], F32)
        var, var_r = ar.alloc("var", [512], F32)
        lnv, lnv_r = ar.alloc("lnv", [512], F32)
        rstd, rstd_r = ar.alloc("rstd", [512], F32)
        tmf = [ar.alloc(f"tmf{i}", [512], F32) for i in range(2)]
        sb, sb_r = ar.alloc("sb", [KC, 512], BF16)
        tm2, tm2_r = ar.alloc("tm2", [512], F32)
        def blk_gen(b):
            tb = slice(b * 512, (b + 1) * 512)
            pm, pmr = self.next_ps(pin=True)
            pq, pqr = self.next_ps(pin=True)
            for m in range(KC):
                s_, s_r = sqf[m % 2]
                self.act(s_, cv[:, m, tb], AF.Square, reads=[cv_r], writes=[s_r])
                self.mm(pm[:, :], self.onesf, cv[:, m, tb], m == 0, m == KC - 1, reads=[cv_r, self.c_r], writes=[pmr])
                self.mm(pq[:, :], self.onesf, s_, m == 0, m == KC - 1, reads=[s_r, self.c_r], writes=[pqr])
            yield
            self.cp("act", mean, pm[:, :], reads=[pmr], writes=[mean_r])
            self.tt(var, mean, mean, ALU.mult, reads=[mean_r], writes=[var_r])
            self.tt(var, pq[:, :], var, ALU.subtract, reads=[pqr, var_r], writes=[var_r])
            self.unpin(pmr)
            self.unpin(pqr)
            self.act(lnv, var, AF.Ln, reads=[var_r, self.c_r], writes=[lnv_r], bias=self.eps_t[:, :])
            self.act(rstd, lnv, AF.Exp, reads=[lnv_r], writes=[rstd_r], scale=-0.5)
            for m in range(KC):
                t_, t_r = tmf[m % 2]
                self.tt(t_, cv[:, m, tb], mean, ALU.subtract, reads=[cv_r, mean_r], writes=[t_r])
                self.tt(t_, t_, rstd, ALU.mult, reads=[t_r, rstd_r], writes=[t_r])
                self.act(sb[:, m, :], t_, AF.Silu, reads=[t_r, vec_r], writes=[sb_r], bias=vec[:, 2, m:m + 1], scale=vec[:, 1, m:m + 1])
            for m in range(KC):
                ps, psr = self.next_ps()
                for k in range(KC):
                    self.mm(ps[:, :], w2[:, k, m * 128:(m + 1) * 128], sb[:, k, :], k == 0, k == KC - 1, reads=[w2_r, sb_r], writes=[psr])
                self.ts(tm2, ps[:, :], vec[:, 3, m:m + 1], self.gate(l, 0, m, g.ci), ALU.add, ALU.mult,
                        reads=[psr, vec_r, self.mod_r], writes=[tm2_r])
                self.tt(self.X[:, m, tb], self.X[:, m, tb], tm2, ALU.add, reads=[tm2_r, self.Xr[b]], writes=[self.Xr[b]])
        _cur = blk_gen(0)
        next(_cur)
        for b in range(g.NB):
            _nxt = None
            if b + 1 < g.NB:
                _nxt = blk_gen(b + 1)
                next(_nxt)
            for _ in _cur:
                pass
            _cur = _nxt

    def gqa(self, g, l):
        kb, ar = self.kb, self.ar
        T, NT = g.T, g.NT
        hT, hT_r = self.norm1(g, l)
        NKC = 512 if g.lat else 0
        NK = NKC + T
        gg, gg_r = ar.alloc("gg", [2, 128], F32)
        self.load(gg, self.gq_g_d[:, :, :], gg_r)
        if g.lat:
            rtb = [ar.alloc(f"grt{i}", [2, 64], F32) for i in range(2)]

            def load_rt(t):
                kb.dma("sp", [(rtb[t % 2][0], self.gq_rtok_d[:, :, t, :])], f"d_rt{t % 2}", writes=[rtb[t % 2][1]])
        attnT, attnT_r = ar.alloc("attnT", [4, T], BF16)
        QrT, QrT_r = ar.alloc("QrT", [4, T], BF16)
        if g.lat:
            QuT, QuT_r = ar.alloc("QuT", [4, T], BF16)
        KT, KT_r = ar.alloc("KT", [NK], BF16)
        Vt, Vt_r = ar.alloc("Vt", [NK // 128, 128], BF16)
        wq, wq_r = ar.alloc("wq", [KC, 768], BF16)
        wo = wq.rearrange("p a b -> p (a b)")[:, 0:4 * D].rearrange("p (a b) -> p a b", a=4)
        wo_r = wq_r
        _gsq = [ar.alloc(f"gsq{i}", [6, 128], F32) for i in range(2)]
        _gst = [ar.alloc(f"gst{i}", [24], F32) for i in range(2)]
        qf = [ar.alloc(f"qf{i}", [6, 128], F32) for i in range(2)]
        qb = [ar.alloc(f"qb{i}", [11, 128], BF16) for i in range(2)]
        _grt = [ar.alloc(f"grtmp{i}", [2, 5, 64], F32) for i in range(2)]
        _eb = [ar.alloc(f"E{i}", [512], BF16) for i in range(3)]
        EB, EBr = [x[0] for x in _eb], [x[1] for x in _eb]
        self.rden = ar.alloc("rden", [512], F32)
        if g.lat:
            cc, cc_r = ar.alloc("gcc", [4, 2, 128], BF16)
        scale = 128 ** -0.5
        for kvh in range(2):
            wsrc = self.gq_w_d.rearrange("(k p) n -> p k n", p=128)
            self.loadw(wq[:, :, 0:512], wsrc[:, :, kvh * 512:(kvh + 1) * 512], wq_r, "d_w0")
            self.loadw(wq[:, :, 512:640], wsrc[:, :, 1024 + kvh * 128:1024 + (kvh + 1) * 128], wq_r, "d_w0")
            self.loadw(wq[:, :, 640:768], wsrc[:, :, 1280 + kvh * 128:1280 + (kvh + 1) * 128], wq_r, "d_w0")
            if g.lat:
                self.loadw(cc[:, :, 0, :], self.gq_ck_d[:, kvh * 128:(kvh + 1) * 128].rearrange("(j p) c -> p j c", p=128), cc_r, "d_w2")
                self.loadw(cc[:, :, 1, :], self.gq_cv_d[:, kvh * 128:(kvh + 1) * 128].rearrange("(j p) c -> p j c", p=128), cc_r, "d_w2")
                ps, psr = self.next_ps()
                pb = ps[:, :].bitcast(BF16)
                for j in range(4):
                    self.tr(pb[:, j * 128:(j + 1) * 128], cc[:, j, 0, :], self.identb, reads=[cc_r, self.c_r], writes=[psr])
                self.cp("act", KT[:, 0:512], pb[:, 0:512], reads=[psr], writes=[KT_r])
                self.cp("dve", Vt[:, 0:4, :], cc[:, :, 1, :], reads=[cc_r], writes=[Vt_r])
            if g.lat:
                load_rt(0)
            def tile_gen(tt_):
                tsl = slice(tt_ * 128, (tt_ + 1) * 128)
                if g.lat:
                    if tt_ + 1 < NT:
                        load_rt(tt_ + 1)
                    rt, rt_r = rtb[tt_ % 2]
                st, st_r = _gst[tt_ % 2]
                sq, sq_r = _gsq[tt_ % 2]
                rtmp, rtmp_r = _grt[tt_ % 2]
                p1, p1r = self.next_ps()
                p2, p2r = self.next_ps()
                for k in range(KC):
                    self.mm(p1[:, :], hT[:, k, tsl], wq[:, k, 0:512], k == 0, k == KC - 1, reads=[hT_r, wq_r], writes=[p1r])
                for k in range(KC):
                    self.mm(p2[:, 0:256], hT[:, k, tsl], wq[:, k, 512:768], k == 0, k == KC - 1, reads=[hT_r, wq_r], writes=[p2r])
                self.act(sq[:, 0:4, :], p1[:, :].rearrange("p (h d) -> p h d", h=4), AF.Square, reads=[p1r], writes=[sq_r])
                self.act(sq[:, 4, :], p2[:, 0:128], AF.Square, reads=[p2r], writes=[sq_r])
                kb.op("dve", lambda e: e.tensor_reduce(out=st[:, 0:5], in_=sq[:, 0:5, :], axis=AX.X, op=ALU.add), reads=[sq_r], writes=[st_r])
                self.act(st[:, 8:13], st[:, 0:5], AF.Ln, reads=[st_r, self.c_r], writes=[st_r], bias=self.eps_t[:, :], scale=1.0 / 128)
                self.act(st[:, 16:21], st[:, 8:13], AF.Exp, reads=[st_r], writes=[st_r], scale=-0.5)
                q_, q_r = qf[tt_ % 2]
                o_, o_r = qb[tt_ % 2]
                self.tt(q_[:, 0:4, :], p1[:, :].rearrange("p (h d) -> p h d", h=4), st[:, 16:20].unsqueeze(2).broadcast_to([128, 4, 128]),
                        ALU.mult, reads=[p1r, st_r], writes=[q_r])
                self.tt(q_[:, 0:4, :], q_[:, 0:4, :], gg[:, 0, :].unsqueeze(1).broadcast_to([128, 4, 128]), ALU.mult, reads=[q_r, gg_r], writes=[q_r])
                self.stt(q_[:, 4, :], p2[:, 0:128], st[:, 20:21], gg[:, 1, :], ALU.mult, ALU.mult, reads=[p2r, st_r, gg_r], writes=[q_r])
                self.cp("act", q_[:, 5, :], p2[:, 128:256], reads=[p2r], writes=[q_r])
                self.cp("act", o_[:, 9, :], p2[:, 128:256], reads=[p2r], writes=[o_r])
                if g.lat:
                    c_ = rt[:, 0, :].unsqueeze(1).broadcast_to([128, 5, 64])
                    s_ = rt[:, 1, :].unsqueeze(1).broadcast_to([128, 5, 64])
                    x1, x2 = q_[:, 0:5, 0:64], q_[:, 0:5, 64:128]
                    self.tt(rtmp[:, 0, :, :], x1, c_, ALU.mult, reads=[q_r, rt_r], writes=[rtmp_r])
                    self.tt(rtmp[:, 1, :, :], x2, s_, ALU.mult, reads=[q_r, rt_r], writes=[rtmp_r])
                    self.tt(o_[:, 0:5, 0:64], rtmp[:, 0, :, :], rtmp[:, 1, :, :], ALU.subtract, reads=[rtmp_r], writes=[o_r])
                    self.tt(rtmp[:, 0, :, :], x1, s_, ALU.mult, reads=[q_r, rt_r], writes=[rtmp_r])
                    self.tt(rtmp[:, 1, :, :], x2, c_, ALU.mult, reads=[q_r, rt_r], writes=[rtmp_r])
                    self.tt(o_[:, 0:5, 64:128], rtmp[:, 0, :, :], rtmp[:, 1, :, :], ALU.add, reads=[rtmp_r], writes=[o_r])
                    self.cp("act", o_[:, 5:9, :], q_[:, 0:4, :], reads=[q_r], writes=[o_r])
                    ntr = 9
                else:
                    self.cp("act", o_[:, 0:5, :], q_[:, 0:5, :], reads=[q_r], writes=[o_r])
                    ntr = 5
                    kb.dma("sp", [(self.st_k_d[tsl, kvh * 128:(kvh + 1) * 128], q_[:, 4, :])], "d_st", reads=[q_r])
                    kb.dma("sp", [(self.st_v_d[tsl, kvh * 128:(kvh + 1) * 128], q_[:, 5, :])], "d_st", reads=[q_r])
                yield
                pa, par = self.next_ps()
                pba = pa[:, :].bitcast(BF16)
                for i in range(5):
                    self.tr(pba[:, i * 128:(i + 1) * 128], o_[:, i, :], self.identb, reads=[o_r, self.c_r], writes=[par])
                ksl = slice(NKC + tt_ * 128, NKC + (tt_ + 1) * 128)
                self.cp("act", QrT[:, :, tsl], pba[:, 0:512].rearrange("p (h t) -> p h t", h=4), reads=[par], writes=[QrT_r])
                self.cp("act", KT[:, ksl], pba[:, 512:640], reads=[par], writes=[KT_r])
                self.cp("dve", Vt[:, NKC // 128 + tt_, :], o_[:, 9, :], reads=[o_r], writes=[Vt_r])
                if g.lat:
                    pc, pcr = self.next_ps()
                    pbc = pc[:, :].bitcast(BF16)
                    for i in range(4):
                        self.tr(pbc[:, i * 128:(i + 1) * 128], o_[:, 5 + i, :], self.identb, reads=[o_r, self.c_r], writes=[pcr])
                    self.cp("act", QuT[:, :, tsl], pbc[:, 0:512].rearrange("p (h t) -> p h t", h=4), reads=[pcr], writes=[QuT_r])
            _cur = tile_gen(0)
            next(_cur)
            for tt_ in range(NT):
                _nxt = None
                if tt_ + 1 < NT:
                    _nxt = tile_gen(tt_ + 1)
                    next(_nxt)
                for _ in _cur:
                    pass
                _cur = _nxt
            rds = [KT_r, Vt_r, QrT_r] + ([QuT_r] if g.lat else [])
            for hh in range(4):
                jobs = []
                if g.lat:
                    for b in range(4):
                        qs = slice(b * 512, (b + 1) * 512)
                        chunks = []
                        for kc in range(NK // 128):
                            ks = slice(kc * 128, (kc + 1) * 128)
                            qsrc = QuT if kc < 4 else QrT
                            chunks.append(([(KT[:, ks], qsrc[:, hh, qs])], Vt[:, kc, :]))
                        jobs.append((512, chunks, attnT[:, hh, qs], attnT_r))
                else:
                    for s_ in range(4):
                        qs = slice(s_ * 256, (s_ + 1) * 256)
                        chunks = []
                        for kc in range(2 * s_, 2 * s_ + 2):
                            ks = slice(kc * 128, (kc + 1) * 128)
                            chunks.append(([(KT[:, ks], QrT[:, hh, qs])], Vt[:, kc, :]))
                        jobs.append((256, chunks, attnT[:, hh, qs], attnT_r))
                self.attn_jobs(jobs, scale, rds, (EB, EBr))
            self.loadw(wo, self.gq_wo_d[kvh * 512:(kvh + 1) * 512, :].rearrange("(k p) n -> p k n", p=128), wo_r, "d_w0")
            for b in range(g.NB):
                tb = slice(b * 512, (b + 1) * 512)
                for m in range(KC):
                    ps, psr = self.next_ps()
                    for hh in range(4):
                        self.mm(ps[:, :], wo[:, hh, m * 128:(m + 1) * 128], attnT[:, hh, tb], hh == 0, hh == 3, reads=[wo_r, attnT_r], writes=[psr])
                    self.resid(b, m, tb, ps[:, :], psr, self.gate(l, 0, m, g.ci))

    def moe(self, g, l):
        kb, ar = self.kb, self.ar
        T, NT, NB, C, NS, NCC = g.T, g.NT, g.NB, g.C, g.NSLOT, g.NCC
        ci = g.ci
        h2tok, h2tok_r = ar.alloc("h2tok", [NT, D], BF16)
        aff, aff_r = ar.alloc("aff", [NT, NE], F32)
        affhl, affhl_r = ar.alloc("affhl", [NT, NE, 2], BF16)
        posg, posg_r = ar.alloc("posg", [NT, NE], F32)
        mk0 = ar.mark()
        affT, affT_r = ar.alloc("affT", [T], F32)
        mk1 = ar.mark()
        rw, rw_r = ar.alloc("rw", [KC, NE], F32)
        self.load(rw, self.rw_d[:, l, :, :], rw_r)
        tmps = self.norm_tmps()
        h2f = [ar.alloc(f"h2f{i}", [KC, 512], F32) for i in range(2)]
        sm, sm_r = ar.alloc("sm", [16], F32)
        ex, ex_r = ar.alloc("ex", [4, NE], F32)
        def norm_b(b):
            hf, hf_r = h2f[b % 2]
            self.norm_block(b, lambda m: self.gs[:, l, 1, m, ci:ci + 1], lambda m: self.shift(l, 1, m, ci),
                            lambda m: hf[:, m, :], hf_r, tmps, precise=True)

        def route_b(b):
            hf, hf_r = h2f[b % 2]
            pT, pTr = self.next_ps(pin=True)
            pl, plr = self.next_ps(pin=True)
            t4 = slice(b * 4, b * 4 + 4)
            for q in range(4):
                qs = slice(q * 128, (q + 1) * 128)
                for k in range(KC):
                    self.mm(pl[:, q * NE:(q + 1) * NE], hf[:, k, qs], rw[:, k, :], k == 0, k == KC - 1, reads=[hf_r, rw_r], writes=[plr])
            plv = pl[:, 0:4 * NE].rearrange("p (q e) -> p q e", q=4)
            kb.op("dve", lambda e: e.tensor_reduce(out=sm[:, 0:4], in_=plv, axis=AX.X, op=ALU.max), reads=[plr], writes=[sm_r])
            self.tt(ex, plv, sm[:, 0:4].unsqueeze(2).broadcast_to([128, 4, NE]), ALU.subtract, reads=[plr, sm_r], writes=[ex_r])
            self.act(ex, ex, AF.Exp, reads=[ex_r], writes=[ex_r])
            kb.op("dve", lambda e: e.tensor_reduce(out=sm[:, 4:8], in_=ex, axis=AX.X, op=ALU.add), reads=[ex_r], writes=[sm_r])
            kb.op("dve", lambda e: e.reciprocal(out=sm[:, 8:12], in_=sm[:, 4:8]), reads=[sm_r], writes=[sm_r])
            self.tt(aff[:, t4, :], ex, sm[:, 8:12].unsqueeze(2).broadcast_to([128, 4, NE]), ALU.mult, reads=[ex_r, sm_r], writes=[aff_r])
            self.unpin(plr)
            self.cp("dve", affhl[:, t4, :, 0], aff[:, t4, :], reads=[aff_r], writes=[affhl_r])
            self.tt(ex, aff[:, t4, :], affhl[:, t4, :, 0], ALU.subtract, reads=[aff_r, affhl_r], writes=[ex_r])
            self.cp("dve", affhl[:, t4, :, 1], ex, reads=[ex_r], writes=[affhl_r])
            for q in range(4):
                tt_ = b * 4 + q
                qs = slice(q * 128, (q + 1) * 128)
                self.tr(pT[0:NE, qs], aff[:, tt_, :], self.ident, reads=[aff_r, self.c_r], writes=[pTr])
                for half in range(2):
                    ph, phr = self.next_ps()
                    for mm_ in range(4):
                        m = half * 4 + mm_
                        self.tr(ph[:, mm_ * 128:(mm_ + 1) * 128], hf[:, m, qs], self.ident, reads=[hf_r, self.c_r], writes=[phr])
                    self.cp("act" if half == 0 else "dve", h2tok[:, tt_, half * 512:(half + 1) * 512], ph[:, :], reads=[phr], writes=[h2tok_r])
            self.cp("act", affT[0:NE, b * 512:(b + 1) * 512], pT[0:NE, :], reads=[pTr], writes=[affT_r])
            self.unpin(pTr)

        norm_b(0)
        for b in range(NB):
            if b + 1 < NB:
                norm_b(b + 1)
            route_b(b)
        ar.release(mk1)
        S = g.S
        work, work_r = ar.alloc("work", [S], F32)
        vals, vals_r = ar.alloc("vals", [C], F32)
        mask, mask_r = ar.alloc("mask", [S], F32)
        cum, cum_r = ar.alloc("cum", [S], F32)
        zer, zer_r = ar.alloc("zer", [S], F32)
        pgT, pgT_r = ar.alloc("pgT", [T], F32)
        kb.op("dve", lambda e: e.memset(zer[0:NE, :], 0.0), writes=[zer_r])
        ada_next = None
        if g.lat and l + 1 < self.depth:
            wb = [ar.alloc(f"adaw{i}", [KC, 1024], BF16) for i in range(2)]
            ada_next = self.adaln_issue(l + 1, wb)
        for s in range(g.nseq):
            ss = slice(s * S, (s + 1) * S)
            self.cp("dve", work[0:NE, :], affT[0:NE, ss], reads=[affT_r], writes=[work_r])
            for r in range(C // 8):
                kb.op("dve", lambda e: e.max(out=vals[0:NE, r * 8:(r + 1) * 8], in_=work[0:NE, :]), reads=[work_r], writes=[vals_r])
                if r < C // 8 - 1:
                    kb.op("dve", lambda e: e.match_replace(out=work[0:NE, :], in_to_replace=vals[0:NE, r * 8:(r + 1) * 8],
                                                           in_values=work[0:NE, :], imm_value=-1.0), reads=[work_r, vals_r], writes=[work_r])
            self.ts(mask[0:NE, :], affT[0:NE, ss], vals[0:NE, C - 1:C], None, ALU.is_ge, None, reads=[affT_r, vals_r], writes=[mask_r])
            kb.op("dve", lambda e: e.tensor_tensor_scan(out=cum[0:NE, :], data0=mask[0:NE, :], data1=zer[0:NE, :], initial=0.0,
                                                        op0=ALU.add, op1=ALU.add), reads=[mask_r, zer_r], writes=[cum_r])
            self.stt(mask[0:NE, :], cum[0:NE, :], float(C), mask[0:NE, :], ALU.is_le, ALU.mult, reads=[cum_r, mask_r], writes=[mask_r])
            self.ts(cum[0:NE, :], cum[0:NE, :], float(s * C - 1), None, ALU.add, None, reads=[cum_r], writes=[cum_r])
            self.tt(cum[0:NE, :], cum[0:NE, :], mask[0:NE, :], ALU.mult, reads=[cum_r, mask_r], writes=[cum_r])
            self.ts(mask[0:NE, :], mask[0:NE, :], 4096.0, -4096.0, ALU.mult, ALU.add, reads=[mask_r], writes=[mask_r])
            self.tt(pgT[0:NE, ss], cum[0:NE, :], mask[0:NE, :], ALU.add, reads=[cum_r, mask_r], writes=[pgT_r])
        pp, ppr = self.next_ps()
        for tt_ in range(NT):
            self.tr(pp[:, tt_ * NE:(tt_ + 1) * NE], pgT[0:NE, tt_ * 128:(tt_ + 1) * 128], self.ident[0:NE, 0:NE], reads=[pgT_r, self.c_r], writes=[ppr])
        self.cp("dve", posg, pp[:, 0:NT * NE].rearrange("p (t e) -> p t e", e=NE), reads=[ppr], writes=[posg_r])
        if ada_next is not None:
            self.adaln_finish(l + 1, *ada_next)
        self.dbg("aff", aff, aff_r)
        self.dbg("posg", posg, posg_r)
        self.dbg("h2tok", h2tok, h2tok_r)
        self.dbg("affT", affT[0:NE, :], affT_r)
        self.dbg("pgT", pgT[0:NE, :], pgT_r)
        self.dbg("vals", vals[0:NE, :], vals_r)
        ar.release(mk0)
        wslots = []
        NWS = 2 if (g.lat or "L" in self.groups) else 4
        CG = 1 if g.lat else 4
        NBUF = 2 if g.lat else 8
        for i in range(NWS):
            a = ar.alloc(f"wg{i}", [KC, FF], BF16)
            b_ = ar.alloc(f"wu{i}", [KC, FF], BF16)
            c_ = ar.alloc(f"wd{i}", [4, D], BF16)
            wslots.append((a, b_, c_))
        Sel = [ar.alloc(f"Sel{i}", [NT, NS], BF16) for i in range(2)]
        SelT = [ar.alloc(f"SelT{i}", [NCC, T], BF16) for i in range(NBUF)]
        XG = [ar.alloc(f"xg{i}", [KC, NS], BF16) for i in range(2)]
        sg, sg_r = ar.alloc("sg", [NS], F32)
        hid, hid_r = ar.alloc("hid", [4, NS], BF16)
        YO = [ar.alloc(f"yo{i}", [NCC, D], BF16) for i in range(NBUF)]
        GSL = [ar.alloc(f"gsl{i}", [4], F32) for i in range(2)]

        use_scr = (not g.lat) and ("L" in self.groups)

        def load_w(e):
            (wg, wg_r), (wu, wu_r), (wd, wd_r) = wslots[e % NWS]
            if use_scr:
                sc = self.wscr_d[l, e]
                kb.dma("sp", [(wg.rearrange("p a b -> p (a b)"), sc[:, 0:4096])], f"d_m{e % NWS}a", reads=[self.wscr_rs[l]], writes=[wg_r])
                kb.dma("sp", [(wu.rearrange("p a b -> p (a b)"), sc[:, 4096:8192])], f"d_m{e % NWS}b", reads=[self.wscr_rs[l]], writes=[wu_r])
                kb.dma("sp", [(wd.rearrange("p a b -> p (a b)"), sc[:, 8192:12288])], f"d_m{e % NWS}c", reads=[self.wscr_rs[l]], writes=[wd_r])
                return
            self.loadw(wg, self.wg_d[l, e].rearrange("(k p) n -> p k n", p=128), wg_r, f"d_m{e % NWS}a")
            self.loadw(wu, self.wu_d[l, e].rearrange("(k p) n -> p k n", p=128), wu_r, f"d_m{e % NWS}b")
            self.loadw(wd, self.wd_d[l, e].rearrange("(k p) n -> p k n", p=128), wd_r, f"d_m{e % NWS}c")
            if g.lat:
                sc = self.wscr_d[l, e]
                kb.dma("sp", [(sc[:, 0:4096], wg.rearrange("p a b -> p (a b)")),
                              (sc[:, 4096:8192], wu.rearrange("p a b -> p (a b)")),
                              (sc[:, 8192:12288], wd.rearrange("p a b -> p (a b)"))], "d_ws",
                       reads=[wg_r, wu_r, wd_r], writes=[self.wscr_rs[l]])

        def s1_sel(e):
            sel, sel_r = Sel[e % 2]
            for tt_ in range(NT):
                self.ts(sel[:, tt_, :], self.iotab[:, 0:NS], posg[:, tt_, e:e + 1], None, ALU.is_equal, None,
                        reads=[posg_r, self.c_r], writes=[sel_r])

        def s1_rest(e, comb=None):
            sel, sel_r = Sel[e % 2]
            selT, selT_r = SelT[e % NBUF]
            xg, xg_r = XG[e % 2]
            gsl, gsl_r = GSL[e % 2]
            for m in range(KC):
                if m % 2 == 0:
                    pgm, pgmr = self.next_ps(pin=True)
                o = pgm[:, (m % 2) * 256:(m % 2) * 256 + NS]
                for tt_ in range(NT):
                    self.mm(o, h2tok[:, tt_, m * 128:(m + 1) * 128], sel[:, tt_, :], tt_ == 0, tt_ == NT - 1, reads=[h2tok_r, sel_r], writes=[pgmr])
                if comb is not None:
                    for _ in range((NB * KC + KC - 1) // KC):
                        next(comb, None)
                if m % 2 == 1:
                    self.cp("act", xg[:, m - 1:m + 1, :],
                            pgm[:, :].rearrange("p (a b) -> p a b", a=2)[:, :, 0:NS], reads=[pgmr], writes=[xg_r])
                    self.unpin(pgmr)
            if comb is not None:
                for _ in comb:
                    pass
            pg_, pg_r = self.next_ps()
            for cc in range(NCC):
                for tt_ in range(NT):
                    self.mm(pg_[:, 2 * cc:2 * cc + 2], sel[:, tt_, cc * 128:(cc + 1) * 128], affhl[:, tt_, e, :], tt_ == 0, tt_ == NT - 1,
                            reads=[sel_r, affhl_r], writes=[pg_r])
            for cc in range(NCC):
                kb.op("dve", lambda en: en.tensor_reduce(out=gsl[:, cc:cc + 1], in_=pg_[:, 2 * cc:2 * cc + 2], axis=AX.X, op=ALU.add), reads=[pg_r], writes=[gsl_r])
            for cc in range(NCC):
                for t0 in range(0, NT, 8):
                    pt, ptr = self.next_ps()
                    pbt = pt[:, :].bitcast(BF16)
                    for q in range(8):
                        self.tr(pbt[:, q * 128:(q + 1) * 128], sel[:, t0 + q, cc * 128:(cc + 1) * 128], self.identb, reads=[sel_r, self.c_r], writes=[ptr])
                    self.cp("act", selT[:, cc, t0 * 128:(t0 + 8) * 128], pbt[:, :], reads=[ptr], writes=[selT_r])

        def s2a(e):
            (wg, wg_r), (wu, wu_r), (wd, wd_r) = wslots[e % NWS]
            xg, xg_r = XG[e % 2]
            for f in range(4):
                pf, pfr = self.next_ps()
                for k in range(KC):
                    self.mm(pf[:, 0:NS], wg[:, k, f * 128:(f + 1) * 128], xg[:, k, :], k == 0, k == KC - 1, reads=[wg_r, xg_r], writes=[pfr])
                for k in range(KC):
                    self.mm(pf[:, 256:256 + NS], wu[:, k, f * 128:(f + 1) * 128], xg[:, k, :], k == 0, k == KC - 1, reads=[wu_r, xg_r], writes=[pfr])
                self.act(sg, pf[:, 0:NS], AF.Silu, reads=[pfr], writes=[sg_r])
                self.tt(hid[:, f, :], sg, pf[:, 256:256 + NS], ALU.mult, reads=[sg_r, pfr], writes=[hid_r])

        def s2b(e):
            (wg, wg_r), (wu, wu_r), (wd, wd_r) = wslots[e % NWS]
            yo, yo_r = YO[e % NBUF]
            gsl, gsl_r = GSL[e % 2]
            for cc in range(NCC):
                for db in range(2):
                    py, pyr = self.next_ps()
                    for f in range(4):
                        self.mm(py[:, :], hid[:, f, cc * 128:(cc + 1) * 128], wd[:, f, db * 512:(db + 1) * 512], f == 0, f == 3, reads=[hid_r, wd_r], writes=[pyr])
                    self.act(yo[:, cc, db * 512:(db + 1) * 512], py[:, :], AF.Copy, reads=[pyr, gsl_r], writes=[yo_r], scale=gsl[:, cc:cc + 1])

        def s3(es):
            for b in range(NB):
                tb = slice(b * 512, (b + 1) * 512)
                for m in range(KC):
                    pc, pcr = self.next_ps()
                    n = len(es) * NCC
                    i_ = 0
                    for e in es:
                        yo, yo_r = YO[e % NBUF]
                        selT, selT_r = SelT[e % NBUF]
                        for cc in range(NCC):
                            self.mm(pc[:, :], yo[:, cc, m * 128:(m + 1) * 128], selT[:, cc, tb], i_ == 0, i_ == n - 1, reads=[yo_r, selT_r], writes=[pcr])
                            i_ += 1
                    self.resid(b, m, tb, pc[:, :], pcr, self.gate(l, 1, m, ci))
                    yield

        load_w(0)
        s1_sel(0)
        s1_rest(0)
        for i in range(1, min(NWS - 1, NE)):
            load_w(i)
        for i in range(NE):
            if i + NWS - 1 < NE:
                load_w(i + NWS - 1)
            if i + 1 < NE:
                s1_sel(i + 1)
            s2a(i)
            comb = s3(list(range(i - CG, i))) if (i >= CG and i % CG == 0) else None
            if i + 1 < NE:
                s1_rest(i + 1, comb)
            elif comb is not None:
                for _ in comb:
                    pass
            s2b(i)
        for _ in s3(list(range(NE - CG, NE))):
            pass


def _fm(v):
    v = np.asarray(v, np.float32)
    lead = v.shape[:-1]
    n = v.shape[-1] // 128
    return np.ascontiguousarray(np.moveaxis(v.reshape(*lead, n, 128), -1, 0))


def _rope_tables(n_tokens, rot_dim, grid_w=64, theta=10000.0):
    rows = n_tokens // grid_w
    row = np.repeat(np.arange(rows), grid_w).astype(np.float32)
    col = np.tile(np.arange(grid_w), rows).astype(np.float32)
    axis_dim = rot_dim // 2
    inv = (np.float32(theta) ** (-np.arange(0, axis_dim, 2, dtype=np.float32) / np.float32(axis_dim))).astype(np.float32)
    ang = np.concatenate([row[:, None] * inv, col[:, None] * inv], axis=-1).astype(np.float32)
    return np.cos(ang).astype(np.float32), np.sin(ang).astype(np.float32), inv


def _consts():
    c = np.zeros((128, 512), np.float32)
    c[:, 0:128] = np.eye(128, dtype=np.float32)
    c[:, 128:256] = 1.0 / 1024
    c[:, 256:512] = np.arange(256, dtype=np.float32)[None, :]
    return c


def _pool_rc(S):
    out = np.zeros((128, 4, 16), np.float32)
    for gi, win in enumerate((2, 4, 8, 16)):
        t = np.concatenate([np.arange(8), np.arange(S - 8, S)])
        lo = np.clip(t - win // 2, 0, S)
        hi = np.clip(t + win // 2, 0, S)
        out[:, gi, :] = (1.0 / (hi - lo).astype(np.float32))[None, :]
    return out


_PROG_CACHE = {}


def _get_prog(depth=4, **kw):
    if depth not in _PROG_CACHE:
        p = Prog(depth, **kw)
        p.build()
        _PROG_CACHE[depth] = p
    return _PROG_CACHE[depth]


def make_in_maps(inp, cores=range(NCORES)):
    f32 = lambda a: np.ascontiguousarray(np.asarray(a, np.float32))
    L = 4
    shared = {}
    shared["ada_w"] = f32(inp["ada_w"])
    shared["ada_b"] = f32(np.moveaxis(np.asarray(inp["ada_b"], np.float32).reshape(L, 48, 128), -1, 0))
    shared["g1"] = _fm(inp["norm1_g"])
    shared["g2"] = _fm(inp["norm2_g"])
    shared["gf"] = _fm(inp["final_g"])
    shared["cst"] = _consts()
    shared["pool_w"] = f32(inp["pool_w"][0])
    shared["pool_s"] = _fm(inp["pool_scale"][0])
    shared["mla_wd"] = f32(inp["mla_w_down"][0])
    shared["mla_gq"] = f32(np.broadcast_to(np.asarray(inp["mla_g_q"][0], np.float32)[None, :], (128, 384)))
    wuq = np.asarray(inp["mla_w_uq"][0], np.float32).reshape(384, 8, 192)
    rope = wuq[:, :, 128:192]
    swapped = np.concatenate([rope[:, :, 32:64], rope[:, :, 0:32]], axis=-1)
    shared["mla_wuq"] = f32(np.concatenate([wuq[:, :, 0:128], rope, swapped, rope], axis=-1).reshape(384, 2560))
    shared["mla_gkv"] = f32(np.broadcast_to(np.asarray(inp["mla_g_kv"][0], np.float32)[None, :], (128, 256)))
    shared["mla_wukv"] = f32(inp["mla_w_ukv"][0])
    shared["mla_wo"] = f32(inp["mla_w_o"][0])
    cos, sin, _ = _rope_tables(2048, 64)
    rtok = np.stack([cos, sin], 0).reshape(2, 16, 128, 32).transpose(2, 0, 1, 3)
    shared["mla_rtok"] = f32(rtok)
    rows = np.arange(32, dtype=np.float32)
    cols = np.arange(64, dtype=np.float32)
    inv = _rope_tables(2048, 64)[2]
    rA = np.ones((64, 2, 32), np.float32)
    rB = np.ones((64, 2, 64), np.float32)
    for i in range(64):
        ii = i % 32
        sgn = -1.0 if i < 32 else 1.0
        if ii < 16:
            a = (rows * inv[ii]).astype(np.float32)
            rA[i, 0] = np.cos(a)
            rA[i, 1] = sgn * np.sin(a)
        else:
            a = (cols * inv[ii - 16]).astype(np.float32)
            rB[i, 0] = np.cos(a)
            rB[i, 1] = sgn * np.sin(a)
    shared["mla_rA"] = rA
    shared["mla_rB"] = rB
    shared["cv_w1"] = f32(inp["conv_w_pw1"][0])
    shared["cv_b1"] = _fm(inp["conv_b_pw1"][0])
    shared["cv_wdw"] = f32(np.asarray(inp["conv_w_dw"][0], np.float32).T.reshape(8, 128, 31).transpose(1, 0, 2))
    shared["cv_vec"] = f32(np.stack([_fm(inp["conv_b_dw"][0]), _fm(inp["conv_ln_g"][0]), _fm(inp["conv_ln_b"][0]),
                                     _fm(inp["conv_b_pw2"][0])], axis=1))
    shared["cv_w2"] = f32(inp["conv_w_pw2"][0])
    shared["gq_w"] = f32(inp["gqa_w_qkv"][0])
    shared["gq_g"] = f32(np.broadcast_to(np.stack([np.asarray(inp["gqa_g_q"][0], np.float32),
                                                    np.asarray(inp["gqa_g_k"][0], np.float32)], 0)[None], (128, 2, 128)))
    shared["gq_wo"] = f32(inp["gqa_w_o"][0])
    cos, sin, _ = _rope_tables(2048, 128)
    shared["gq_rtok"] = f32(np.stack([cos, sin], 0).reshape(2, 16, 128, 64).transpose(2, 0, 1, 3))
    shared["rw"] = f32(np.asarray(inp["router_w"], np.float32).reshape(L, 8, 128, NE).transpose(2, 0, 1, 3))
    shared["moe_wg"] = f32(inp["moe_w_gate"])
    shared["moe_wu"] = f32(inp["moe_w_up"])
    shared["moe_wd"] = f32(inp["moe_w_down"])
    shared["pool_rc"] = _pool_rc(2048)
    maps = []
    xs = np.asarray(inp["x_sample"], np.float32)
    xp = np.asarray(inp["x_prompt"], np.float32)
    cc = np.asarray(inp["c"], np.float32)
    cctx = np.asarray(inp["c_ctx"], np.float32)
    for i in cores:
        m = dict(shared)
        m["xl"] = f32(xs[i].T.reshape(8, 128, 2048).transpose(1, 0, 2))
        m["xc"] = f32(xp[4 * i:4 * i + 4].reshape(1024, 1024).T.reshape(8, 128, 1024).transpose(1, 0, 2))
        m["cond"] = f32(np.stack([_fm(cc[i]), _fm(cctx)], axis=-1))
        m["mla_cckv"] = f32(inp["cache_mla_ckv"][i, 0])
        m["mla_ckr"] = f32(inp["cache_mla_krope"][i, 0])
        m["gq_ck"] = f32(np.asarray(inp["cache_gqa_k"][i, 0]).reshape(512, 256))
        m["gq_cv"] = f32(np.asarray(inp["cache_gqa_v"][i, 0]).reshape(512, 256))
        maps.append(m)
    return maps


def kernel(**inputs):
    prog = _get_prog(4)
    maps = make_in_maps(inputs)
    res = run_bass_kernel_spmd(prog.nc, maps, core_ids=list(range(NCORES)))
    return assemble(res.results)


def assemble(results):
    n = len(results)
    y_prompt = np.zeros((4 * n, 256, D), np.float32)
    y_sample = np.zeros((n, 2048, D), np.float32)
    s_ckv = np.zeros((4 * n, 1, 256, 256), np.float32)
    s_kr = np.zeros((4 * n, 1, 256, 64), np.float32)
    s_k = np.zeros((4 * n, 1, 256, 2, 128), np.float32)
    s_v = np.zeros((4 * n, 1, 256, 2, 128), np.float32)
    for i, r in enumerate(results):
        y_sample[i] = r["yl"].transpose(1, 0, 2).reshape(D, 2048).T
        y_prompt[4 * i:4 * i + 4] = r["yc"].transpose(1, 0, 2).reshape(D, 1024).T.reshape(4, 256, D)
        s_ckv[4 * i:4 * i + 4, 0] = r["st_ckv"].reshape(4, 256, 256)
        s_kr[4 * i:4 * i + 4, 0] = r["st_kr"].reshape(4, 256, 64)
        s_k[4 * i:4 * i + 4, 0] = r["st_k"].reshape(4, 256, 2, 128)
        s_v[4 * i:4 * i + 4, 0] = r["st_v"].reshape(4, 256, 2, 128)
    return (y_prompt, y_sample, s_ckv, s_kr, s_k, s_v)
```

```python
import contextlib
import numpy as np
import concourse.bass as bass
import concourse.mybir as mybir
from concourse.bass_utils import run_bass_kernel_spmd

F32 = mybir.dt.float32
BF16 = mybir.dt.bfloat16
AF = mybir.ActivationFunctionType
ALU = mybir.AluOpType
AX = mybir.AxisListType

D = 1024
KC = 8
NE = 16
FF = 512
EPS = 1e-6
NCORES = 8


class Res:
    __slots__ = ("name", "w", "r", "excl")

    def __init__(self, name, excl=False):
        self.name = name
        self.w = None
        self.r = {}
        self.excl = excl


class Eng:
    def __init__(self, name, eng, sem):
        self.name = name
        self.eng = eng
        self.sem = sem
        self.count = 0
        self.waited = {}


class KB:
    def __init__(self, nc):
        self.nc = nc
        self.stack = contextlib.ExitStack()
        self.sems = {}
        self.dma_tot = {}
        self.E = {}
        for name, e in (("pe", nc.tensor), ("act", nc.scalar), ("dve", nc.vector),
                        ("pool", nc.gpsimd), ("sp", nc.sync)):
            s = self.stack.enter_context(nc.semaphore("s_" + name))
            self.sems["s_" + name] = s
            self.E[name] = Eng(name, e, "s_" + name)
        self.n_wait = 0
        self.n_ins = 0
        self.snaps = {}

    def sbuf(self, name, shape, dtype):
        return self.stack.enter_context(self.nc.sbuf_tensor(name, list(shape), dtype))

    def psum(self, name, shape, dtype=F32):
        return self.stack.enter_context(self.nc.psum_tensor(name, list(shape), dtype))

    def dsem(self, key):
        if key not in self.sems:
            self.sems[key] = self.stack.enter_context(self.nc.semaphore(key))
            self.dma_tot[key] = 0
        return key

    def _wait(self, E, key, val):
        if key in self.dma_tot:
            val = max(val, self.dma_tot[key])
        if E.waited.get(key, 0) >= val:
            return
        E.eng.wait_ge(self.sems[key], val)
        E.waited[key] = val
        self.n_wait += 1
        snap = self.snaps.get((key, val))
        if snap:
            for k2, v2 in snap.items():
                if E.waited.get(k2, 0) < v2:
                    E.waited[k2] = v2

    def _dep(self, E, tok):
        key, val = tok
        if E.name == "pe" and key == "s_pe":
            return
        self._wait(E, key, val)

    def _sync(self, E, reads, writes):
        for res in reads:
            if res.w is not None:
                self._dep(E, res.w)
            if res.excl:
                for k, v in res.r.items():
                    if k != E.sem:
                        self._dep(E, (k, v))
        for res in writes:
            if res.w is not None and res.w[0] != E.sem:
                self._dep(E, res.w)
            for k, v in res.r.items():
                if k != E.sem:
                    self._dep(E, (k, v))

    def _mark(self, tok, reads, writes):
        key, val = tok
        for res in reads:
            if res.r.get(key, 0) < val:
                res.r[key] = val
        for res in writes:
            res.w = tok
            res.r = {}

    def op(self, ename, fn, reads=(), writes=()):
        E = self.E[ename]
        self._sync(E, reads, writes)
        ins = fn(E.eng)
        E.count += 1
        ins.then_inc(self.sems[E.sem], 1)
        self.snaps[(E.sem, E.count)] = dict(E.waited)
        self._mark((E.sem, E.count), reads, writes)
        self.n_ins += 1
        return ins

    def dma(self, qname, pairs, sem_key, reads=(), writes=(), **kw):
        E = self.E[qname]
        self.dsem(sem_key)
        self._sync(E, reads, writes)
        for (o, i) in pairs:
            ins = E.eng.dma_start(out=o, in_=i, **kw)
            ins.then_inc(self.sems[sem_key], 16)
            self.dma_tot[sem_key] += 16
            self.n_ins += 1
        tok = (sem_key, self.dma_tot[sem_key])
        self._mark(tok, reads, writes)
        return tok

    def barrier(self):
        for E in self.E.values():
            for F in self.E.values():
                if F is not E and F.count:
                    self._wait(E, F.sem, F.count)
            for k, tot in self.dma_tot.items():
                if tot:
                    self._wait(E, k, tot)

    def close(self):
        self.stack.close()


class Arena:
    def __init__(self, kb, nbytes):
        self.kb = kb
        self.n = nbytes
        self.t = kb.sbuf("arena", [128, nbytes // 4], F32)
        self.off = 0
        self.top = nbytes

    def reset(self):
        self.kb.barrier()
        self.off = 0
        self.top = self.n

    def release_top(self):
        self.kb.barrier()
        self.top = self.n

    def mark(self):
        return self.off

    def release(self, mark):
        self.kb.barrier()
        self.off = mark

    def alloc(self, name, shape, dtype, parts=128, top=False):
        esz = 2 if dtype == BF16 else 4
        n = int(np.prod(shape))
        nb = (n * esz + 31) // 32 * 32
        assert self.off + nb <= self.top, f"arena overflow at {name}: {self.off}+{nb}>{self.top}"
        if top:
            self.top -= nb
            start = self.top
        else:
            start = self.off
            self.off += nb
        v = self.t[0:parts, start // 4:(start + nb) // 4]
        if dtype != F32:
            v = v.bitcast(dtype)
        v = v[:, 0:n]
        if len(shape) == 2:
            v = v.rearrange("p (a b) -> p a b", a=shape[0])
        elif len(shape) == 3:
            v = v.rearrange("p (a b c) -> p a b c", a=shape[0], b=shape[1])
        return v, Res(name)


class Grp:
    def __init__(self, name, T, nseq, S, ci):
        self.name = name
        self.T = T
        self.nseq = nseq
        self.S = S
        self.ci = ci
        self.NT = T // 128
        self.NB = T // 512
        self.C = S // 8
        self.NSLOT = nseq * self.C
        self.NCC = self.NSLOT // 128
        self.lat = (ci == 0)


class Prog:
    def __init__(self, depth=4, do_moe=True, do_mixer=True, debug=False, groups="LC", stop=""):
        self.depth = depth
        self.groups = groups
        self.stop = stop
        self.debug = debug
        self.do_moe = do_moe
        self.do_mixer = do_mixer
        nc = self.nc = bass.Bass("TRN2", target_bir_lowering=False)
        self.kb = kb = KB(nc)
        self.din = {}
        self.dout = {}

    def inp(self, name, shape, dt=F32):
        ap = self.nc.dram_tensor(name, list(shape), dt, kind="ExternalInput").ap()
        self.din[name] = ap
        return ap

    def outp(self, name, shape):
        ap = self.nc.dram_tensor(name, list(shape), F32, kind="ExternalOutput").ap()
        self.dout[name] = ap
        return ap

    def dbg(self, name, ap, res):
        if not getattr(self, "debug", False) or name in self.dout:
            return
        shp = list(ap.shape)
        d = self.nc.dram_tensor("dbg_" + name, shp, ap.dtype, kind="ExternalOutput").ap()
        self.dout[name] = d
        self.kb.dma("sp", [(d, ap)], "d_dbg", reads=[res])

    def next_ps(self, pin=False):
        i = self.ps_i
        while i in self.ps_pinned:
            i = (i + 1) % 8
        self.ps_i = (i + 1) % 8
        if pin:
            self.ps_pinned.add(i)
        return self.ps[i], self.psr[i]

    def unpin(self, psr):
        self.ps_pinned.discard(self.psr.index(psr))

    def mm(self, out, lhsT, rhs, start, stop, reads, writes):
        self.kb.op("pe", lambda e: e.matmul(out, lhsT, rhs, start=start, stop=stop), reads=reads, writes=writes)

    def tr(self, out, in_, ident, reads, writes):
        self.kb.op("pe", lambda e: e.transpose(out, in_, ident), reads=reads, writes=writes)

    def act(self, out, in_, func, reads, writes, bias=None, scale=1.0, accum_out=None):
        kw = {}
        if bias is not None:
            kw["bias"] = bias
        if accum_out is not None:
            kw["accum_out"] = accum_out
        self.kb.op("act", lambda e: e.activation(out=out, in_=in_, func=func, scale=scale, **kw),
                   reads=reads, writes=writes)

    def tt(self, out, in0, in1, op, reads, writes, eng="dve"):
        self.kb.op(eng, lambda e: e.tensor_tensor(out=out, in0=in0, in1=in1, op=op), reads=reads, writes=writes)

    def ts(self, out, in0, s1, s2, op0, op1, reads, writes, eng="dve", accum_out=None):
        if s2 is None:
            self.kb.op(eng, lambda e: e.tensor_scalar(out=out, in0=in0, scalar1=s1, scalar2=None, op0=op0),
                       reads=reads, writes=writes)
        else:
            self.kb.op(eng, lambda e: e.tensor_scalar(out=out, in0=in0, scalar1=s1, scalar2=s2, op0=op0, op1=op1),
                       reads=reads, writes=writes)

    def stt(self, out, in0, scalar, in1, op0, op1, reads, writes):
        self.kb.op("dve", lambda e: e.scalar_tensor_tensor(out=out, in0=in0, scalar=scalar, in1=in1, op0=op0, op1=op1),
                   reads=reads, writes=writes)

    def cp(self, eng, out, in_, reads, writes):
        if eng == "act":
            self.kb.op("act", lambda e: e.copy(out=out, in_=in_), reads=reads, writes=writes)
        else:
            self.kb.op(eng, lambda e: e.tensor_copy(out=out, in_=in_), reads=reads, writes=writes)

    def load(self, out, in_, res, sem="d_ld", q="sp", **kw):
        self.kb.dma(q, [(out, in_)], sem, writes=[res], **kw)

    def loadw(self, out, in_, res, sem):
        n = out.shape[-1]
        pairs = []
        if n <= 1024:
            self.kb.dma("pool", [(out, in_)], sem, writes=[res])
            return
        for c0 in range(0, n, 1024):
            c1 = min(n, c0 + 1024)
            if len(out.shape) == 3:
                pairs.append((out[:, :, c0:c1], in_[:, :, c0:c1]))
            else:
                pairs.append((out[:, c0:c1], in_[:, c0:c1]))
        self.kb.dma("pool", pairs, sem, writes=[res])

    def rstd_from(self, out, in_, n, reads, writes, tmp, tmp_r):
        P = out.shape[0]
        self.act(tmp, in_, AF.Ln, reads=list(reads) + [self.c_r], writes=[tmp_r], bias=self.eps_t[0:P, :], scale=1.0 / n)
        self.act(out, tmp, AF.Exp, reads=[tmp_r], writes=writes, scale=-0.5)

    def build(self):
        nc, kb = self.nc, self.kb
        L = 4
        inp, outp = self.inp, self.outp
        xl_d = inp("xl", [128, KC, 2048])
        xc_d = inp("xc", [128, KC, 1024])
        cond_d = inp("cond", [128, KC, 2])
        ada_w_d = inp("ada_w", [L, D, 6 * D])
        ada_b_d = inp("ada_b", [128, L, 48])
        g1_d = inp("g1", [128, L, KC])
        g2_d = inp("g2", [128, L, KC])
        gf_d = inp("gf", [128, KC])
        cst_d = inp("cst", [128, 128 + 128 + 256])
        pool_w_d = inp("pool_w", [4, 256, 256])
        pool_s_d = inp("pool_s", [128, KC])
        pool_rc_d = inp("pool_rc", [128, 4, 16])
        mla_wd_d = inp("mla_wd", [D, 704])
        mla_gq_d = inp("mla_gq", [128, 384])
        mla_wuq_d = inp("mla_wuq", [384, 2560])
        mla_gkv_d = inp("mla_gkv", [128, 256])
        mla_wukv_d = inp("mla_wukv", [256, 2048])
        mla_wo_d = inp("mla_wo", [D, D])
        mla_cckv_d = inp("mla_cckv", [512, 256])
        mla_ckr_d = inp("mla_ckr", [512, 64])
        mla_rtok_d = inp("mla_rtok", [128, 2, 16, 32])
        mla_rA_d = inp("mla_rA", [64, 2, 32])
        mla_rB_d = inp("mla_rB", [64, 2, 64])
        cv_w1_d = inp("cv_w1", [D, 2 * D])
        cv_b1_d = inp("cv_b1", [128, 16])
        cv_wdw_d = inp("cv_wdw", [128, KC, 31])
        cv_vec_d = inp("cv_vec", [128, 4, KC])
        cv_w2_d = inp("cv_w2", [D, D])
        gq_w_d = inp("gq_w", [D, 1536])
        gq_g_d = inp("gq_g", [128, 2, 128])
        gq_wo_d = inp("gq_wo", [D, D])
        gq_ck_d = inp("gq_ck", [512, 256])
        gq_cv_d = inp("gq_cv", [512, 256])
        gq_rtok_d = inp("gq_rtok", [128, 2, 16, 64])
        rw_d = inp("rw", [128, L, KC, NE])
        wg_d = inp("moe_wg", [L, NE, D, FF])
        wu_d = inp("moe_wu", [L, NE, D, FF])
        wd_d = inp("moe_wd", [L, NE, FF, D])
        wscr_d = self.nc.dram_tensor("wscr", [L, NE, 128, 12288], BF16, kind="Internal").ap()
        self.wscr_rs = [Res(f"wscr{i}") for i in range(L)]
        yl_d = outp("yl", [128, KC, 2048])
        yc_d = outp("yc", [128, KC, 1024])
        st_ckv_d = outp("st_ckv", [1024, 256])
        st_kr_d = outp("st_kr", [1024, 64])
        st_k_d = outp("st_k", [1024, 256])
        st_v_d = outp("st_v", [1024, 256])
        self.__dict__.update(locals())

        self.X = kb.sbuf("X", [128, KC, 2048], F32)
        self.Xr = [Res(f"X{b}") for b in range(4)]
        self.cst = kb.sbuf("cstt", [128, 512], F32)
        self.c_r = Res("cst")
        self.ident = self.cst[:, 0:128]
        self.onesf = self.cst[:, 128:256]
        self.iota = self.cst[:, 256:512]
        self.cb = kb.sbuf("cb", [128, 640], BF16)
        self.identb = self.cb[:, 0:128]
        self.onesb_s = self.cb[:, 128:256]
        self.onesb = self.cb[:, 256:384]
        self.iotab = self.cb[:, 384:640]
        self.eps_t = kb.sbuf("eps", [128, 1], F32)
        self.mods = kb.sbuf("mods", [128, L, 48, 2], F32)
        self.mod_rs = [Res(f"mods{i}") for i in range(L)]
        self.mod_r = self.mod_rs[0]
        self.cnd = kb.sbuf("cnd", [128, KC, 2], F32)[:, :, :]
        self.cndb = kb.sbuf("cndb", [128, KC, 2], BF16)[:, :, :]
        self.adab = kb.sbuf("adab", [128, L, 48], F32)[:, :, :]
        self.cnd_r = Res("cnd")
        self.gs = kb.sbuf("gs", [128, L, 2, KC, 2], F32)
        self.vecs = kb.sbuf("vecs", [128, 3 * L * KC + KC + KC], F32)
        self.g1 = self.vecs[:, 0:L * KC].rearrange("p (l m) -> p l m", l=L)
        self.g2 = self.vecs[:, L * KC:2 * L * KC].rearrange("p (l m) -> p l m", l=L)
        self.gf = self.vecs[:, 3 * L * KC:3 * L * KC + KC]
        self.ps = [kb.psum(f"ps{i}", [128, 512], F32) for i in range(8)]
        self.psr = [Res(f"ps{i}", excl=True) for i in range(8)]
        self.ps_i = 0
        self.ps_pinned = set()
        self.ar = Arena(kb, 136 * 1024)

        kb.dma("sp", [(self.cst[:], cst_d[:, :])], "d_c", writes=[self.c_r])
        kb.dma("sp", [(self.g1, g1_d[:, :, :]), (self.g2, g2_d[:, :, :]), (self.gf, gf_d[:, :])], "d_c", writes=[self.c_r])
        kb.op("dve", lambda e: e.memset(self.eps_t[:], EPS), writes=[self.c_r])
        kb.op("dve", lambda e: e.tensor_copy(out=self.identb, in_=self.ident), reads=[self.c_r], writes=[self.c_r])
        kb.op("dve", lambda e: e.tensor_copy(out=self.onesb_s, in_=self.onesf), reads=[self.c_r], writes=[self.c_r])
        kb.op("dve", lambda e: e.memset(self.onesb, 1.0), writes=[self.c_r])
        kb.op("dve", lambda e: e.tensor_copy(out=self.iotab, in_=self.iota), reads=[self.c_r], writes=[self.c_r])

        self.adaln_setup()
        for grp in (Grp("L", 2048, 1, 2048, 0), Grp("C", 1024, 4, 256, 1)):
            if grp.name in getattr(self, "groups", "LC"):
                self.run_group(grp)
        kb.barrier()

    def adaln_setup(self):
        self.load(self.cnd, self.cond_d[:, :, :], self.cnd_r)
        self.load(self.adab, self.ada_b_d[:, :, :], self.cnd_r)
        self.act(self.cndb, self.cnd, AF.Silu, reads=[self.cnd_r], writes=[self.cnd_r])

    def adaln_issue(self, l, wb):
        ps, psr = self.next_ps(pin=True)
        for jb in range(6):
            w, w_r = wb[jb % 2]
            self.loadw(w, self.ada_w_d[l, :, jb * 1024:(jb + 1) * 1024].rearrange("(k p) n -> p k n", p=128), w_r, f"d_a{jb % 2}")
            for jj in range(8):
                j = jb * 8 + jj
                for k in range(KC):
                    self.mm(ps[:, 2 * j:2 * j + 2], w[:, k, jj * 128:(jj + 1) * 128], self.cndb[:, k, :],
                            k == 0, k == KC - 1, reads=[w_r, self.cnd_r], writes=[psr])
        return ps, psr

    def adaln_finish(self, l, ps, psr):
        mr = self.mod_rs[l]
        self.tt(self.mods[:, l, :, :], ps[:, 0:96].rearrange("p (j c) -> p j c", c=2),
                self.adab[:, l, :].unsqueeze(2).broadcast_to([128, 48, 2]), ALU.add,
                reads=[psr, self.cnd_r], writes=[mr])
        self.unpin(psr)
        for which, g in ((0, self.g1), (1, self.g2)):
            j0 = 8 + 24 * which
            self.stt(self.gs[:, l, which, :, :], self.mods[:, l, j0:j0 + 8, :], 1.0,
                     g[:, l, :].unsqueeze(2).broadcast_to([128, KC, 2]), ALU.add, ALU.mult,
                     reads=[mr, self.c_r], writes=[mr])

    def shift(self, l, which, m, ci):
        return self.mods[:, l, 24 * which + m, ci:ci + 1]

    def gate(self, l, which, m, ci):
        return self.mods[:, l, 16 + 24 * which + m, ci:ci + 1]

    def norm_block(self, b, gs_fn, sh_fn, out_fn, out_r, tmps, precise=False):
        X, Xr = self.X, self.Xr
        sq, sq_r, lnv, lnv_r, rstd, rstd_r, tm, tm_r = tmps
        tb = slice(b * 512, (b + 1) * 512)
        ps, psr = self.next_ps()
        for m in range(KC):
            if precise:
                s_ = tm[m % 2]
                s_r = tm_r[m % 2]
                self.tt(s_, X[:, m, tb], X[:, m, tb], ALU.mult, reads=[Xr[b]], writes=[s_r])
                self.mm(ps[:, :], self.onesf, s_, m == 0, m == KC - 1, reads=[s_r, self.c_r], writes=[psr])
            else:
                self.tt(sq[m % 2], X[:, m, tb], X[:, m, tb], ALU.mult, reads=[Xr[b]], writes=[sq_r[m % 2]])
                self.mm(ps[:, :], self.onesb_s, sq[m % 2], m == 0, m == KC - 1, reads=[sq_r[m % 2], self.c_r], writes=[psr])
        self.act(lnv, ps[:, :], AF.Ln, reads=[psr, self.c_r], writes=[lnv_r], bias=self.eps_t[:, :])
        self.act(rstd, lnv, AF.Exp, reads=[lnv_r], writes=[rstd_r], scale=-0.5)
        self.tt(lnv, rstd, rstd, ALU.mult, reads=[rstd_r], writes=[lnv_r])
        self.stt(lnv, ps[:, :], EPS, lnv, ALU.add, ALU.mult, reads=[psr, lnv_r], writes=[lnv_r])
        self.ts(lnv, lnv, -0.5, 1.5, ALU.mult, ALU.add, reads=[lnv_r], writes=[lnv_r])
        self.tt(rstd, rstd, lnv, ALU.mult, reads=[rstd_r, lnv_r], writes=[rstd_r])
        for m in range(KC):
            self.tt(tm[m % 2], X[:, m, tb], rstd, ALU.mult, reads=[Xr[b], rstd_r], writes=[tm_r[m % 2]])
            if gs_fn is None:
                self.cp("act", out_fn(m), tm[m % 2], reads=[tm_r[m % 2]], writes=[out_r])
            else:
                self.act(out_fn(m), tm[m % 2], AF.Identity, reads=[tm_r[m % 2], self.mod_r], writes=[out_r],
                         bias=sh_fn(m), scale=gs_fn(m))

    def norm_tmps(self):
        ar = self.ar
        sq0, r0 = ar.alloc("sq0", [512], BF16)
        sq1, r1 = ar.alloc("sq1", [512], BF16)
        lnv, lr = ar.alloc("lnv", [512], F32)
        rstd, rr = ar.alloc("rstd", [512], F32)
        t0, tr0 = ar.alloc("tm0", [512], F32)
        t1, tr1 = ar.alloc("tm1", [512], F32)
        return ([sq0, sq1], [r0, r1], lnv, lr, rstd, rr, [t0, t1], [tr0, tr1])

    def norm1(self, g, l):
        ar = self.ar
        hT, hT_r = ar.alloc("hT", [KC, g.T], BF16)
        mk = ar.mark()
        tmps = self.norm_tmps()
        for b in range(g.NB):
            self.norm_block(b, lambda m: self.gs[:, l, 0, m, g.ci:g.ci + 1], lambda m: self.shift(l, 0, m, g.ci),
                            lambda m: hT[:, m, b * 512:(b + 1) * 512], hT_r, tmps)
        ar.release(mk)
        return hT, hT_r

    def resid(self, b, m, cols, ps_ap, psr, gate_ap, extra_reads=()):
        self.stt(self.X[:, m, cols], ps_ap, gate_ap, self.X[:, m, cols], ALU.mult, ALU.add,
                 reads=[psr, self.Xr[b], self.mod_r] + list(extra_reads), writes=[self.Xr[b]])

    def run_group(self, g):
        kb, ar = self.kb, self.ar
        ar.reset()
        src = self.xl_d if g.lat else self.xc_d
        for b in range(g.NB):
            kb.dma("sp", [(self.X[:, :, b * 512:(b + 1) * 512], src[:, :, b * 512:(b + 1) * 512])], "d_x", writes=[self.Xr[b]])
        for l in range(self.depth):
            ar.reset()
            self.mod_r = self.mod_rs[l]
            if g.lat and l == 0:
                wb = [ar.alloc(f"adaw{i}", [KC, 1024], BF16) for i in range(2)]
                self.adaln_finish(0, *self.adaln_issue(0, wb))
                ar.reset()
            kind = l % 4
            if not self.do_mixer:
                pass
            elif kind == 0:
                self.pool_mixer(g, l)
            elif kind == 1:
                self.mla(g, l)
            elif kind == 2:
                self.conv(g, l)
            else:
                self.gqa(g, l)
            ar.reset()
            if self.do_moe:
                self.moe(g, l)
        ar.reset()
        tmps = self.norm_tmps()
        yb = [ar.alloc(f"yb{i}", [KC, 512], F32) for i in range(2)]
        dst = self.yl_d if g.lat else self.yc_d
        for b in range(g.NB):
            y, y_r = yb[b % 2]
            self.norm_block(b, lambda m: self.gf[:, m:m + 1], lambda m: 0.0, lambda m: y[:, m, :], y_r, tmps)
            kb.dma("sp", [(dst[:, :, b * 512:(b + 1) * 512], y[:, :, :])], "d_y", reads=[y_r])

    def pool_mixer(self, g, l):
        kb, ar = self.kb, self.ar
        hT, hT_r = self.norm1(g, l)
        nseq, S = g.nseq, g.S
        W = S + 16
        pw, pw_r = ar.alloc("pw", [4, 2, 256], BF16)
        psc, psc_r = ar.alloc("psc", [KC], F32)
        prc, prc_r = ar.alloc("prc", [4, 16], F32)
        gsc, gsc_r = ar.alloc("gsc", [KC], F32)
        self.loadw(pw, self.pool_w_d.rearrange("g (k p) n -> p g k n", p=128), pw_r, "d_w0")
        self.load(psc, self.pool_s_d[:, :], psc_r)
        self.load(prc, self.pool_rc_d[:, :, :], prc_r)
        self.tt(gsc, self.mods[:, l, 16:24, g.ci], psc, ALU.mult, reads=[self.mod_r, psc_r], writes=[gsc_r])
        lv = [ar.alloc(f"lv{i}", [nseq, W], F32) for i in range(5)]
        pooled, pooled_r = ar.alloc("pooled", [2, nseq, S], BF16)
        tmpb, tmpb_r = ar.alloc("tmpb", [nseq, 16], F32)
        kb.op("dve", lambda e: e.memset(lv[0][0].rearrange("p a b -> p (a b)"), 0.0), writes=[lv[0][1]])
        for gi in range(4):
            win = 2 << gi
            for kk in range(2):
                m = 2 * gi + kk
                hv = hT[:, m, :].rearrange("p (s t) -> p s t", s=nseq)
                P0, P0r = lv[0]
                self.cp("act", P0[:, :, 8:8 + S], hv, reads=[hT_r], writes=[P0r])
                A, Ar = lv[1]
                self.tt(A[:, :, 1:W], P0[:, :, 1:W], P0[:, :, 0:W - 1], ALU.add, reads=[P0r], writes=[Ar])
                sh = 1
                lo, hi = 1, W
                for level in range(2, gi + 2):
                    Bv, Br = lv[level]
                    lo2, hi2 = lo + sh, hi - sh
                    self.tt(Bv[:, :, lo2:hi2], A[:, :, lo2 - sh:hi2 - sh], A[:, :, lo2 + sh:hi2 + sh], ALU.add,
                            reads=[Ar], writes=[Br])
                    A, Ar = Bv, Br
                    lo, hi = lo2, hi2
                    sh *= 2
                pv = pooled[:, kk, :, :]
                self.stt(pv, A[:, :, 8:8 + S], 1.0 / win, hv, ALU.mult, ALU.subtract, reads=[Ar, hT_r], writes=[pooled_r])
                for side, c0 in ((0, 0), (1, S - 8)):
                    self.tt(tmpb[:, :, 0:8], A[:, :, 8 + c0:16 + c0],
                            prc[:, gi, side * 8:side * 8 + 8].unsqueeze(1).broadcast_to([128, nseq, 8]), ALU.mult,
                            reads=[Ar, prc_r], writes=[tmpb_r])
                    self.tt(pv[:, :, c0:c0 + 8], tmpb[:, :, 0:8], hv[:, :, c0:c0 + 8], ALU.subtract,
                            reads=[tmpb_r, hT_r], writes=[pooled_r])
            pf = pooled.rearrange("p k s t -> p k (s t)")
            for b in range(g.NB):
                tb = slice(b * 512, (b + 1) * 512)
                for mo in range(2):
                    m = 2 * gi + mo
                    ps, psr = self.next_ps()
                    for k in range(2):
                        self.mm(ps[:, :], pw[:, gi, k, mo * 128:(mo + 1) * 128], pf[:, k, tb], k == 0, k == 1,
                                reads=[pw_r, pooled_r], writes=[psr])
                    self.resid(b, m, tb, ps[:, :], psr, gsc[:, m:m + 1], extra_reads=[gsc_r])

    def attn_jobs(self, jobs, scale, reads, ebuf, LA=2):
        E, E_r = ebuf
        nE = len(E)
        sps = [self.next_ps(pin=True) for _ in range(nE)]
        od = [(self.next_ps(pin=True), self.next_ps(pin=True)) for _ in range(2)]
        flat = [(j, i) for j, job in enumerate(jobs) for i in range(len(job[1]))]

        def issue_s(n):
            j, i = flat[n]
            NQ, chunks = jobs[j][0], jobs[j][1]
            xr = list(jobs[j][4]) if len(jobs[j]) > 4 else []
            ps, psr = sps[n % nE]
            kparts, _ = chunks[i]
            for pi, (kT, qT) in enumerate(kparts):
                self.mm(ps[:, 0:NQ], kT, qT, pi == 0, pi == len(kparts) - 1, reads=reads + xr, writes=[psr])

        for n in range(min(LA, len(flat))):
            issue_s(n)
        rd, rd_r = self.rden
        for n, (j, i) in enumerate(flat):
            if n + LA < len(flat):
                issue_s(n + LA)
            NQ, chunks, out_ap, out_r = jobs[j][0:4]
            nch = len(chunks)
            ps, psr = sps[n % nE]
            (ops, opr), (dps, dpr) = od[j % 2]
            self.act(E[n % nE][:, 0:NQ], ps[:, 0:NQ], AF.Exp, reads=[psr], writes=[E_r[n % nE]], scale=scale)
            _, v = chunks[i]
            self.mm(ops[:, 0:NQ], v, E[n % nE][:, 0:NQ], i == 0, i == nch - 1, reads=reads + [E_r[n % nE]], writes=[opr])
            self.mm(dps[:, 0:NQ], self.onesb, E[n % nE][:, 0:NQ], i == 0, i == nch - 1, reads=[E_r[n % nE], self.c_r], writes=[dpr])
            if i == nch - 1:
                self.kb.op("dve", lambda e: e.reciprocal(out=rd[:, 0:NQ], in_=dps[:, 0:NQ]), reads=[dpr], writes=[rd_r])
                self.tt(out_ap, ops[:, 0:NQ], rd[:, 0:NQ], ALU.mult, reads=[opr, rd_r], writes=[out_r])
        for (_, r) in sps:
            self.unpin(r)
        for (a, b) in od:
            self.unpin(a[1])
            self.unpin(b[1])

    def mla(self, g, l):
        kb, ar = self.kb, self.ar
        T, NT = g.T, g.NT
        hT, hT_r = self.norm1(g, l)
        NKC = 512 if g.lat else 0
        NK = NKC + T
        cqnT, cqnT_r = ar.alloc("cqnT", [3, T], BF16)
        ckvT, ckvT_r = ar.alloc("ckvT", [2, NK], BF16)
        krT, krT_r = ar.alloc("krT", [NK], BF16)
        kb.op("dve", lambda e: e.memset(krT, 0.0), writes=[krT_r])
        wuq, wuq_r = ar.alloc("wuq", [3, 2560], BF16)
        wukv, wukv_r = ar.alloc("wukv", [2, 2048], BF16)
        self.loadw(wuq, self.mla_wuq_d.rearrange("(k p) n -> p k n", p=128), wuq_r, "d_w1")
        self.loadw(wukv, self.mla_wukv_d.rearrange("(k p) n -> p k n", p=128), wukv_r, "d_w1")
        mkA = ar.mark()
        wd, wd_r = ar.alloc("wd", [KC, 704], BF16)
        gq, gq_r = ar.alloc("gq", [384], F32)
        gkv, gkv_r = ar.alloc("gkv", [256], F32)
        self.loadw(wd, self.mla_wd_d.rearrange("(k p) n -> p k n", p=128), wd_r, "d_w0")
        self.load(gq, self.mla_gq_d[:, :], gq_r)
        self.load(gkv, self.mla_gkv_d[:, :], gkv_r)
        _jk = [ar.alloc(f"junk{i}", [384], F32) for i in range(2)]
        _st = [ar.alloc(f"st{i}", [8], F32) for i in range(2)]
        cqn = [ar.alloc(f"cqn{i}", [384], BF16) for i in range(2)]
        ckn = [ar.alloc(f"ckn{i}", [256], F32) for i in range(2)]
        cknb = [ar.alloc(f"cknb{i}", [384], BF16) for i in range(2)]
        for i in range(2):
            kb.op("dve", lambda e: e.memset(cknb[i][0], 0.0), writes=[cknb[i][1]])
        krf = [ar.alloc(f"krf{i}", [64], F32) for i in range(2)]
        if self.stop == "A00":
            return
        if g.lat:
            rt, rt_r = ar.alloc("rt", [2, 16, 32], F32)
            self.load(rt, self.mla_rtok_d[:, :, :, :], rt_r)
            _rt2 = [ar.alloc(f"rtmp{i}", [4, 32], F32) for i in range(2)]
            cc, cc_r = ar.alloc("cc", [4, 384], BF16)
            kb.op("dve", lambda e: e.memset(cc.rearrange("p a b -> p (a b)"), 0.0), writes=[cc_r])
            self.loadw(cc[:, :, 0:256], self.mla_cckv_d.rearrange("(j p) c -> p j c", p=128), cc_r, "d_w2")
            self.loadw(cc[:, :, 256:320], self.mla_ckr_d.rearrange("(j p) c -> p j c", p=128), cc_r, "d_w2")
            if self.stop == "A01":
                return
            for j in range(4):
                ps, psr = self.next_ps()
                pb = ps[:, :].bitcast(BF16)
                for k in range(2):
                    self.tr(pb[:, k * 128:(k + 1) * 128], cc[:, j, k * 128:(k + 1) * 128], self.identb, reads=[cc_r, self.c_r], writes=[psr])
                self.tr(pb[:, 256:384], cc[:, j, 256:384], self.identb, reads=[cc_r, self.c_r], writes=[psr])
                self.cp("act", ckvT[:, :, j * 128:(j + 1) * 128], pb[:, 0:256].rearrange("p (k t) -> p k t", k=2), reads=[psr], writes=[ckvT_r])
                self.cp("act", krT[0:64, j * 128:(j + 1) * 128], pb[0:64, 256:384], reads=[psr], writes=[krT_r])
        if self.stop == "A0":
            return
        def tile_gen(tt_):
            tsl = slice(tt_ * 128, (tt_ + 1) * 128)
            junk, junk_r = _jk[tt_ % 2]
            st, st_r = _st[tt_ % 2]
            if g.lat:
                rtmp, rtmp_r = _rt2[tt_ % 2]
            p1, p1r = self.next_ps()
            p2, p2r = self.next_ps()
            for k in range(KC):
                self.mm(p1[:, 0:384], hT[:, k, tsl], wd[:, k, 0:384], k == 0, k == KC - 1, reads=[hT_r, wd_r], writes=[p1r])
            for k in range(KC):
                self.mm(p2[:, 0:320], hT[:, k, tsl], wd[:, k, 384:704], k == 0, k == KC - 1, reads=[hT_r, wd_r], writes=[p2r])
            self.act(junk[:, 0:384], p1[:, 0:384], AF.Square, reads=[p1r], writes=[junk_r, st_r], accum_out=st[:, 0:1])
            self.act(junk[:, 0:256], p2[:, 0:256], AF.Square, reads=[p2r], writes=[junk_r, st_r], accum_out=st[:, 1:2])
            self.act(st[:, 2:3], st[:, 0:1], AF.Ln, reads=[st_r, self.c_r], writes=[st_r], bias=self.eps_t[:, :], scale=1.0 / 384)
            self.act(st[:, 3:4], st[:, 1:2], AF.Ln, reads=[st_r, self.c_r], writes=[st_r], bias=self.eps_t[:, :], scale=1.0 / 256)
            self.act(st[:, 4:6], st[:, 2:4], AF.Exp, reads=[st_r], writes=[st_r], scale=-0.5)
            if self.stop == "A1":
                return
            cq, cq_r = cqn[tt_ % 2]
            ck, ck_r = ckn[tt_ % 2]
            ckb, ckb_r = cknb[tt_ % 2]
            kf, kf_r = krf[tt_ % 2]
            self.stt(cq, p1[:, 0:384], st[:, 4:5], gq, ALU.mult, ALU.mult, reads=[p1r, st_r, gq_r], writes=[cq_r])
            self.stt(ck, p2[:, 0:256], st[:, 5:6], gkv, ALU.mult, ALU.mult, reads=[p2r, st_r, gkv_r], writes=[ck_r])
            self.cp("act", ckb[:, 0:256], ck, reads=[ck_r], writes=[ckb_r])
            self.cp("act", kf, p2[:, 256:320], reads=[p2r], writes=[kf_r])
            if g.lat:
                c_, s_ = rt[:, 0, tt_, :], rt[:, 1, tt_, :]
                x1, x2 = kf[:, 0:32], kf[:, 32:64]
                self.tt(rtmp[:, 0, :], x1, c_, ALU.mult, reads=[kf_r, rt_r], writes=[rtmp_r])
                self.tt(rtmp[:, 1, :], x2, s_, ALU.mult, reads=[kf_r, rt_r], writes=[rtmp_r])
                self.tt(rtmp[:, 2, :], x1, s_, ALU.mult, reads=[kf_r, rt_r], writes=[rtmp_r])
                self.tt(rtmp[:, 3, :], x2, c_, ALU.mult, reads=[kf_r, rt_r], writes=[rtmp_r])
                self.tt(ckb[:, 256:288], rtmp[:, 0, :], rtmp[:, 1, :], ALU.subtract, reads=[rtmp_r], writes=[ckb_r])
                self.tt(ckb[:, 288:320], rtmp[:, 2, :], rtmp[:, 3, :], ALU.add, reads=[rtmp_r], writes=[ckb_r])
            else:
                self.cp("dve", ckb[:, 256:320], kf, reads=[kf_r], writes=[ckb_r])
                kb.dma("sp", [(self.st_ckv_d[tsl, :], ck)], "d_st", reads=[ck_r])
                kb.dma("sp", [(self.st_kr_d[tsl, :], kf)], "d_st", reads=[kf_r])
            yield
            if self.stop == "A2":
                return
            ps, psr = self.next_ps()
            pb = ps[:, :].bitcast(BF16)
            for k in range(3):
                self.tr(pb[:, k * 128:(k + 1) * 128], cq[:, k * 128:(k + 1) * 128], self.identb, reads=[cq_r, self.c_r], writes=[psr])
            for k in range(2):
                self.tr(pb[:, (3 + k) * 128:(4 + k) * 128], ckb[:, k * 128:(k + 1) * 128], self.identb, reads=[ckb_r, self.c_r], writes=[psr])
            self.tr(pb[:, 640:768], ckb[:, 256:384], self.identb, reads=[ckb_r, self.c_r], writes=[psr])
            ksl = slice(NKC + tt_ * 128, NKC + (tt_ + 1) * 128)
            self.cp("act", cqnT[:, :, tsl], pb[:, 0:384].rearrange("p (k t) -> p k t", k=3), reads=[psr], writes=[cqnT_r])
            self.cp("act", ckvT[:, :, ksl], pb[:, 384:640].rearrange("p (k t) -> p k t", k=2), reads=[psr], writes=[ckvT_r])
            self.cp("act", krT[0:64, ksl], pb[0:64, 640:768], reads=[psr], writes=[krT_r])
        _cur = tile_gen(0)
        next(_cur)
        for tt_ in range(NT):
            _nxt = None
            if tt_ + 1 < NT:
                _nxt = tile_gen(tt_ + 1)
                next(_nxt)
            for _ in _cur:
                pass
            _cur = _nxt
        if self.stop == "A":
            return
        ar.release(mkA)
        attnT, attnT_r = hT, hT_r
        wo, wo_r = ar.alloc("wo", [KC, D], BF16)
        self.loadw(wo, self.mla_wo_d.rearrange("(k p) n -> p k n", p=128), wo_r, "d_w0")
        knT, knT_r = ar.alloc("knT", [NK], BF16)
        vh, vh_r = ar.alloc("vh", [NK // 128, 128], BF16)
        qn, qn_r = ar.alloc("qn", [T], BF16)
        qr, qr_r = ar.alloc("qr", [T], BF16)
        _eb = [ar.alloc(f"E{i}", [512], BF16) for i in range(3)]
        EB, EBr = [x[0] for x in _eb], [x[1] for x in _eb]
        self.rden = ar.alloc("rden", [512], F32)
        if g.lat:
            qrot, qrot_r = ar.alloc("qrot", [T], BF16)
            kb.op("dve", lambda e: e.memset(qrot, 0.0), writes=[qrot_r])
            rA, rA_r = ar.alloc("rA", [2, 32], F32)
            rB, rB_r = ar.alloc("rB", [2, 64], F32)
            self.load(rA[0:64], self.mla_rA_d[:, :, :], rA_r)
            self.load(rB[0:64], self.mla_rB_d[:, :, :], rB_r)
            t1, t1_r = ar.alloc("t1", [512], F32)
            t2, t2_r = ar.alloc("t2", [512], F32)
        scale = 192 ** -0.5
        qres = [[Res(f"qn{b}"), Res(f"qr{b}"), Res(f"qo{b}")] for b in range(g.NB)]
        for h in range(8):
            c0 = h * 256
            q0 = h * 320
            for kb0 in range(0, NK, 512):
                ps, psr = self.next_ps()
                for k in range(2):
                    self.mm(ps[:, :], wukv[:, k, c0:c0 + 128], ckvT[:, k, kb0:kb0 + 512], k == 0, k == 1, reads=[wukv_r, ckvT_r], writes=[psr])
                self.cp("act", knT[:, kb0:kb0 + 512], ps[:, :], reads=[psr], writes=[knT_r])
            for kc0 in range(0, NK // 128, 4):
                ps, psr = self.next_ps()
                for kk in range(4):
                    kc = kc0 + kk
                    for k in range(2):
                        self.mm(ps[:, kk * 128:(kk + 1) * 128], ckvT[:, k, kc * 128:(kc + 1) * 128], wukv[:, k, c0 + 128:c0 + 256],
                                k == 0, k == 1, reads=[wukv_r, ckvT_r], writes=[psr])
                self.cp("dve", vh[:, kc0:kc0 + 4, :], ps[:, :].rearrange("p (a b) -> p a b", a=4), reads=[psr], writes=[vh_r])
            if self.stop == "B0a":
                continue
            for b in range(g.NB):
                tb = slice(b * 512, (b + 1) * 512)
                ps, psr = self.next_ps()
                for k in range(3):
                    self.mm(ps[:, :], wuq[:, k, q0:q0 + 128], cqnT[:, k, tb], k == 0, k == 2, reads=[wuq_r, cqnT_r], writes=[psr])
                self.cp("act", qn[:, tb], ps[:, :], reads=[psr], writes=[qres[b][0]])
                ps, psr = self.next_ps()
                for k in range(3):
                    self.mm(ps[:, :], wuq[:, k, q0 + 128:q0 + 256], cqnT[:, k, tb], k == 0, k == 2, reads=[wuq_r, cqnT_r], writes=[psr])
                self.cp("act", qr[:, tb], ps[:, :], reads=[psr], writes=[qres[b][1]])
                if g.lat and self.stop != "B0b":
                    ps2, ps2r = self.next_ps()
                    for k in range(3):
                        self.mm(ps2[:, :], wuq[:, k, q0 + 192:q0 + 320], cqnT[:, k, tb], k == 0, k == 2, reads=[wuq_r, cqnT_r], writes=[ps2r])
                    r0 = b * 8
                    for rr in range(8):
                        sg_ = slice(rr * 64, (rr + 1) * 64)
                        self.stt(t1[0:64, sg_], ps[0:64, sg_], rA[0:64, 0, r0 + rr:r0 + rr + 1], rB[0:64, 0, :], ALU.mult, ALU.mult,
                                 reads=[psr, rA_r, rB_r], writes=[t1_r])
                        self.stt(t2[0:64, sg_], ps2[0:64, sg_], rA[0:64, 1, r0 + rr:r0 + rr + 1], rB[0:64, 1, :], ALU.mult, ALU.mult,
                                 reads=[ps2r, rA_r, rB_r], writes=[t2_r])
                    self.tt(qrot[0:64, tb], t1[0:64, :], t2[0:64, :], ALU.add, reads=[t1_r, t2_r, qrot_r], writes=[qres[b][2]])
            if self.stop in ("B0", "B0b"):
                continue
            rds = [knT_r, vh_r, krT_r]
            jobs = []
            if g.lat:
                for b in range(4):
                    qs = slice(b * 512, (b + 1) * 512)
                    chunks = []
                    for kc in range(NK // 128):
                        ks = slice(kc * 128, (kc + 1) * 128)
                        qrp = qr if kc < 4 else qrot
                        chunks.append(([(knT[:, ks], qn[:, qs]), (krT[:, ks], qrp[:, qs])], vh[:, kc, :]))
                    jobs.append((512, chunks, attnT[:, h, qs], attnT_r, qres[b]))
            else:
                for s_ in range(4):
                    qs = slice(s_ * 256, (s_ + 1) * 256)
                    chunks = []
                    for kc in range(2 * s_, 2 * s_ + 2):
                        ks = slice(kc * 128, (kc + 1) * 128)
                        chunks.append(([(knT[:, ks], qn[:, qs]), (krT[:, ks], qr[:, qs])], vh[:, kc, :]))
                    jobs.append((256, chunks, attnT[:, h, qs], attnT_r, qres[s_ // 2][0:2]))
            self.attn_jobs(jobs, scale, rds, (EB, EBr))
        if self.stop in ("B0", "B", "B0a", "B0b"):
            return
        for b in range(g.NB):
            tb = slice(b * 512, (b + 1) * 512)
            for m in range(KC):
                ps, psr = self.next_ps()
                for h in range(8):
                    self.mm(ps[:, :], wo[:, h, m * 128:(m + 1) * 128], attnT[:, h, tb], h == 0, h == 7, reads=[wo_r, attnT_r], writes=[psr])
                self.resid(b, m, tb, ps[:, :], psr, self.gate(l, 0, m, g.ci))

    def conv(self, g, l):
        kb, ar = self.kb, self.ar
        T, nseq, S = g.T, g.nseq, g.S
        W = S + 30
        glu, glu_r = ar.alloc("glu", [KC, nseq, W], BF16, top=True)
        vec, vec_r = ar.alloc("cvvec", [4, KC], F32)
        b1, b1_r = ar.alloc("cvb1", [16], F32)
        wdw, wdw_r = ar.alloc("wdw", [KC, 31], F32)
        self.load(vec, self.cv_vec_d[:, :, :], vec_r)
        self.load(b1, self.cv_b1_d[:, :], b1_r)
        self.load(wdw, self.cv_wdw_d[:, :, :], wdw_r)
        mk0 = ar.mark()
        hT, hT_r = self.norm1(g, l)
        w1, w1_r = ar.alloc("w1", [KC, 2048], BF16)
        self.loadw(w1[:, :, 0:1024], self.cv_w1_d[:, 0:1024].rearrange("(k p) n -> p k n", p=128), w1_r, "d_w0")
        self.loadw(w1[:, :, 1024:2048], self.cv_w1_d[:, 1024:2048].rearrange("(k p) n -> p k n", p=128), w1_r, "d_w0")
        sig, sig_r = ar.alloc("sig", [512], F32)
        kb.op("dve", lambda e: e.memset(glu.rearrange("p a b c -> p (a b c)"), 0.0), writes=[glu_r])
        NQ = 512
        spb = NQ // S if S < NQ else 1
        for b in range(g.NB):
            tb = slice(b * 512, (b + 1) * 512)
            for m in range(KC):
                pa, par = self.next_ps()
                pg, pgr = self.next_ps()
                for k in range(KC):
                    self.mm(pa[:, :], w1[:, k, m * 128:(m + 1) * 128], hT[:, k, tb], k == 0, k == KC - 1, reads=[w1_r, hT_r], writes=[par])
                for k in range(KC):
                    self.mm(pg[:, :], w1[:, k, 1024 + m * 128:1024 + (m + 1) * 128], hT[:, k, tb], k == 0, k == KC - 1, reads=[w1_r, hT_r], writes=[pgr])
                self.act(sig, pg[:, :], AF.Sigmoid, reads=[pgr, b1_r], writes=[sig_r], bias=b1[:, 8 + m:9 + m])
                if g.lat:
                    dst = glu[:, m, 0, 15 + b * 512:15 + (b + 1) * 512]
                    self.stt(dst, pa[:, :], b1[:, m:m + 1], sig, ALU.add, ALU.mult, reads=[par, b1_r, sig_r], writes=[glu_r])
                else:
                    dst = glu[:, m, 2 * b:2 * b + 2, 15:15 + S]
                    self.stt(dst, pa[:, :].rearrange("p (s t) -> p s t", s=2), b1[:, m:m + 1],
                             sig.rearrange("p (s t) -> p s t", s=2), ALU.add, ALU.mult, reads=[par, b1_r, sig_r], writes=[glu_r])
        ar.release(mk0)
        cv, cv_r = ar.alloc("cv", [KC, T], F32)
        mkd = ar.mark()
        dg = [ar.alloc(f"dg{i}", [31, 128], BF16) for i in range(2)]
        for m in range(KC):
            dgt, dg_r = dg[m % 2]
            for j in range(31):
                self.ts(dgt[:, j, :], self.identb, wdw[:, m, j:j + 1], None, ALU.mult, None, reads=[wdw_r, self.c_r], writes=[dg_r])
            for b in range(g.NB):
                ps, psr = self.next_ps()
                for j in range(31):
                    if g.lat:
                        rhs = glu[:, m, 0, b * 512 + j:b * 512 + j + 512]
                        out = ps[:, :]
                    else:
                        rhs = glu[:, m, 2 * b:2 * b + 2, j:j + S]
                        out = ps[:, :].rearrange("p (s t) -> p s t", s=2)
                    self.mm(out, dgt[:, j, :], rhs, j == 0, j == 30, reads=[dg_r, glu_r], writes=[psr])
                self.act(cv[:, m, b * 512:(b + 1) * 512], ps[:, :], AF.Identity, reads=[psr, vec_r], writes=[cv_r], bias=vec[:, 0, m:m + 1])
        ar.release(mkd)
        ar.release_top()
        w2, w2_r = ar.alloc("w2", [KC, D], BF16)
        self.loadw(w2, self.cv_w2_d.rearrange("(k p) n -> p k n", p=128), w2_r, "d_w1")
        sqf = [ar.alloc(f"sqf{i}", [512], F32) for i in range(2)]
        mean, mean_r = ar.alloc("mean", [512], F32)
        var, var_r = ar.alloc("var", [512], F32)
        lnv, lnv_r = ar.alloc("lnv", [512], F32)
        rstd, rstd_r = ar.alloc("rstd", [512], F32)
        tmf = [ar.alloc(f"tmf{i}", [512], F32) for i in range(2)]
        sb, sb_r = ar.alloc("sb", [KC, 512], BF16)
        tm2, tm2_r = ar.alloc("tm2", [512], F32)
        for b in range(g.NB):
            tb = slice(b * 512, (b + 1) * 512)
            pm, pmr = self.next_ps()
            pq, pqr = self.next_ps()
            for m in range(KC):
                s_, s_r = sqf[m % 2]
                self.act(s_, cv[:, m, tb], AF.Square, reads=[cv_r], writes=[s_r])
                self.mm(pm[:, :], self.onesf, cv[:, m, tb], m == 0, m == KC - 1, reads=[cv_r, self.c_r], writes=[pmr])
                self.mm(pq[:, :], self.onesf, s_, m == 0, m == KC - 1, reads=[s_r, self.c_r], writes=[pqr])
            self.cp("act", mean, pm[:, :], reads=[pmr], writes=[mean_r])
            self.tt(var, mean, mean, ALU.mult, reads=[mean_r], writes=[var_r])
            self.tt(var, pq[:, :], var, ALU.subtract, reads=[pqr, var_r], writes=[var_r])
            self.act(lnv, var, AF.Ln, reads=[var_r, self.c_r], writes=[lnv_r], bias=self.eps_t[:, :])
            self.act(rstd, lnv, AF.Exp, reads=[lnv_r], writes=[rstd_r], scale=-0.5)
            for m in range(KC):
                t_, t_r = tmf[m % 2]
                self.tt(t_, cv[:, m, tb], mean, ALU.subtract, reads=[cv_r, mean_r], writes=[t_r])
                self.tt(t_, t_, rstd, ALU.mult, reads=[t_r, rstd_r], writes=[t_r])
                self.act(sb[:, m, :], t_, AF.Silu, reads=[t_r, vec_r], writes=[sb_r], bias=vec[:, 2, m:m + 1], scale=vec[:, 1, m:m + 1])
            for m in range(KC):
                ps, psr = self.next_ps()
                for k in range(KC):
                    self.mm(ps[:, :], w2[:, k, m * 128:(m + 1) * 128], sb[:, k, :], k == 0, k == KC - 1, reads=[w2_r, sb_r], writes=[psr])
                self.ts(tm2, ps[:, :], vec[:, 3, m:m + 1], self.gate(l, 0, m, g.ci), ALU.add, ALU.mult,
                        reads=[psr, vec_r, self.mod_r], writes=[tm2_r])
                self.tt(self.X[:, m, tb], self.X[:, m, tb], tm2, ALU.add, reads=[tm2_r, self.Xr[b]], writes=[self.Xr[b]])

    def gqa(self, g, l):
        kb, ar = self.kb, self.ar
        T, NT = g.T, g.NT
        hT, hT_r = self.norm1(g, l)
        NKC = 512 if g.lat else 0
        NK = NKC + T
        gg, gg_r = ar.alloc("gg", [2, 128], F32)
        self.load(gg, self.gq_g_d[:, :, :], gg_r)
        if g.lat:
            rtb = [ar.alloc(f"grt{i}", [2, 64], F32) for i in range(2)]

            def load_rt(t):
                kb.dma("sp", [(rtb[t % 2][0], self.gq_rtok_d[:, :, t, :])], f"d_rt{t % 2}", writes=[rtb[t % 2][1]])
        attnT, attnT_r = ar.alloc("attnT", [4, T], BF16)
        QrT, QrT_r = ar.alloc("QrT", [4, T], BF16)
        if g.lat:
            QuT, QuT_r = ar.alloc("QuT", [4, T], BF16)
        KT, KT_r = ar.alloc("KT", [NK], BF16)
        Vt, Vt_r = ar.alloc("Vt", [NK // 128, 128], BF16)
        wq, wq_r = ar.alloc("wq", [KC, 768], BF16)
        wo = wq.rearrange("p a b -> p (a b)")[:, 0:4 * D].rearrange("p (a b) -> p a b", a=4)
        wo_r = wq_r
        _gsq = [ar.alloc(f"gsq{i}", [6, 128], F32) for i in range(2)]
        _gst = [ar.alloc(f"gst{i}", [24], F32) for i in range(2)]
        qf = [ar.alloc(f"qf{i}", [6, 128], F32) for i in range(2)]
        qb = [ar.alloc(f"qb{i}", [11, 128], BF16) for i in range(2)]
        _grt = [ar.alloc(f"grtmp{i}", [2, 5, 64], F32) for i in range(2)]
        _eb = [ar.alloc(f"E{i}", [512], BF16) for i in range(3)]
        EB, EBr = [x[0] for x in _eb], [x[1] for x in _eb]
        self.rden = ar.alloc("rden", [512], F32)
        if g.lat:
            cc, cc_r = ar.alloc("gcc", [4, 2, 128], BF16)
        scale = 128 ** -0.5
        for kvh in range(2):
            wsrc = self.gq_w_d.rearrange("(k p) n -> p k n", p=128)
            self.loadw(wq[:, :, 0:512], wsrc[:, :, kvh * 512:(kvh + 1) * 512], wq_r, "d_w0")
            self.loadw(wq[:, :, 512:640], wsrc[:, :, 1024 + kvh * 128:1024 + (kvh + 1) * 128], wq_r, "d_w0")
            self.loadw(wq[:, :, 640:768], wsrc[:, :, 1280 + kvh * 128:1280 + (kvh + 1) * 128], wq_r, "d_w0")
            if g.lat:
                self.loadw(cc[:, :, 0, :], self.gq_ck_d[:, kvh * 128:(kvh + 1) * 128].rearrange("(j p) c -> p j c", p=128), cc_r, "d_w2")
                self.loadw(cc[:, :, 1, :], self.gq_cv_d[:, kvh * 128:(kvh + 1) * 128].rearrange("(j p) c -> p j c", p=128), cc_r, "d_w2")
                ps, psr = self.next_ps()
                pb = ps[:, :].bitcast(BF16)
                for j in range(4):
                    self.tr(pb[:, j * 128:(j + 1) * 128], cc[:, j, 0, :], self.identb, reads=[cc_r, self.c_r], writes=[psr])
                self.cp("act", KT[:, 0:512], pb[:, 0:512], reads=[psr], writes=[KT_r])
                self.cp("dve", Vt[:, 0:4, :], cc[:, :, 1, :], reads=[cc_r], writes=[Vt_r])
            if g.lat:
                load_rt(0)
            def tile_gen(tt_):
                tsl = slice(tt_ * 128, (tt_ + 1) * 128)
                if g.lat:
                    if tt_ + 1 < NT:
                        load_rt(tt_ + 1)
                    rt, rt_r = rtb[tt_ % 2]
                st, st_r = _gst[tt_ % 2]
                sq, sq_r = _gsq[tt_ % 2]
                rtmp, rtmp_r = _grt[tt_ % 2]
                p1, p1r = self.next_ps()
                p2, p2r = self.next_ps()
                for k in range(KC):
                    self.mm(p1[:, :], hT[:, k, tsl], wq[:, k, 0:512], k == 0, k == KC - 1, reads=[hT_r, wq_r], writes=[p1r])
                for k in range(KC):
                    self.mm(p2[:, 0:256], hT[:, k, tsl], wq[:, k, 512:768], k == 0, k == KC - 1, reads=[hT_r, wq_r], writes=[p2r])
                self.act(sq[:, 0:4, :], p1[:, :].rearrange("p (h d) -> p h d", h=4), AF.Square, reads=[p1r], writes=[sq_r])
                self.act(sq[:, 4, :], p2[:, 0:128], AF.Square, reads=[p2r], writes=[sq_r])
                kb.op("dve", lambda e: e.tensor_reduce(out=st[:, 0:5], in_=sq[:, 0:5, :], axis=AX.X, op=ALU.add), reads=[sq_r], writes=[st_r])
                self.act(st[:, 8:13], st[:, 0:5], AF.Ln, reads=[st_r, self.c_r], writes=[st_r], bias=self.eps_t[:, :], scale=1.0 / 128)
                self.act(st[:, 16:21], st[:, 8:13], AF.Exp, reads=[st_r], writes=[st_r], scale=-0.5)
                q_, q_r = qf[tt_ % 2]
                o_, o_r = qb[tt_ % 2]
                self.tt(q_[:, 0:4, :], p1[:, :].rearrange("p (h d) -> p h d", h=4), st[:, 16:20].unsqueeze(2).broadcast_to([128, 4, 128]),
                        ALU.mult, reads=[p1r, st_r], writes=[q_r])
                self.tt(q_[:, 0:4, :], q_[:, 0:4, :], gg[:, 0, :].unsqueeze(1).broadcast_to([128, 4, 128]), ALU.mult, reads=[q_r, gg_r], writes=[q_r])
                self.stt(q_[:, 4, :], p2[:, 0:128], st[:, 20:21], gg[:, 1, :], ALU.mult, ALU.mult, reads=[p2r, st_r, gg_r], writes=[q_r])
                self.cp("act", q_[:, 5, :], p2[:, 128:256], reads=[p2r], writes=[q_r])
                self.cp("act", o_[:, 9, :], p2[:, 128:256], reads=[p2r], writes=[o_r])
                if g.lat:
                    c_ = rt[:, 0, :].unsqueeze(1).broadcast_to([128, 5, 64])
                    s_ = rt[:, 1, :].unsqueeze(1).broadcast_to([128, 5, 64])
                    x1, x2 = q_[:, 0:5, 0:64], q_[:, 0:5, 64:128]
                    self.tt(rtmp[:, 0, :, :], x1, c_, ALU.mult, reads=[q_r, rt_r], writes=[rtmp_r])
                    self.tt(rtmp[:, 1, :, :], x2, s_, ALU.mult, reads=[q_r, rt_r], writes=[rtmp_r])
                    self.tt(o_[:, 0:5, 0:64], rtmp[:, 0, :, :], rtmp[:, 1, :, :], ALU.subtract, reads=[rtmp_r], writes=[o_r])
                    self.tt(rtmp[:, 0, :, :], x1, s_, ALU.mult, reads=[q_r, rt_r], writes=[rtmp_r])
                    self.tt(rtmp[:, 1, :, :], x2, c_, ALU.mult, reads=[q_r, rt_r], writes=[rtmp_r])
                    self.tt(o_[:, 0:5, 64:128], rtmp[:, 0, :, :], rtmp[:, 1, :, :], ALU.add, reads=[rtmp_r], writes=[o_r])
                    self.cp("act", o_[:, 5:9, :], q_[:, 0:4, :], reads=[q_r], writes=[o_r])
                    ntr = 9
                else:
                    self.cp("act", o_[:, 0:5, :], q_[:, 0:5, :], reads=[q_r], writes=[o_r])
                    ntr = 5
                    kb.dma("sp", [(self.st_k_d[tsl, kvh * 128:(kvh + 1) * 128], q_[:, 4, :])], "d_st", reads=[q_r])
                    kb.dma("sp", [(self.st_v_d[tsl, kvh * 128:(kvh + 1) * 128], q_[:, 5, :])], "d_st", reads=[q_r])
                yield
                pa, par = self.next_ps()
                pba = pa[:, :].bitcast(BF16)
                for i in range(5):
                    self.tr(pba[:, i * 128:(i + 1) * 128], o_[:, i, :], self.identb, reads=[o_r, self.c_r], writes=[par])
                ksl = slice(NKC + tt_ * 128, NKC + (tt_ + 1) * 128)
                self.cp("act", QrT[:, :, tsl], pba[:, 0:512].rearrange("p (h t) -> p h t", h=4), reads=[par], writes=[QrT_r])
                self.cp("act", KT[:, ksl], pba[:, 512:640], reads=[par], writes=[KT_r])
                self.cp("dve", Vt[:, NKC // 128 + tt_, :], o_[:, 9, :], reads=[o_r], writes=[Vt_r])
                if g.lat:
                    pc, pcr = self.next_ps()
                    pbc = pc[:, :].bitcast(BF16)
                    for i in range(4):
                        self.tr(pbc[:, i * 128:(i + 1) * 128], o_[:, 5 + i, :], self.identb, reads=[o_r, self.c_r], writes=[pcr])
                    self.cp("act", QuT[:, :, tsl], pbc[:, 0:512].rearrange("p (h t) -> p h t", h=4), reads=[pcr], writes=[QuT_r])
            _cur = tile_gen(0)
            next(_cur)
            for tt_ in range(NT):
                _nxt = None
                if tt_ + 1 < NT:
                    _nxt = tile_gen(tt_ + 1)
                    next(_nxt)
                for _ in _cur:
                    pass
                _cur = _nxt
            rds = [KT_r, Vt_r, QrT_r] + ([QuT_r] if g.lat else [])
            for hh in range(4):
                jobs = []
                if g.lat:
                    for b in range(4):
                        qs = slice(b * 512, (b + 1) * 512)
                        chunks = []
                        for kc in range(NK // 128):
                            ks = slice(kc * 128, (kc + 1) * 128)
                            qsrc = QuT if kc < 4 else QrT
                            chunks.append(([(KT[:, ks], qsrc[:, hh, qs])], Vt[:, kc, :]))
                        jobs.append((512, chunks, attnT[:, hh, qs], attnT_r))
                else:
                    for s_ in range(4):
                        qs = slice(s_ * 256, (s_ + 1) * 256)
                        chunks = []
                        for kc in range(2 * s_, 2 * s_ + 2):
                            ks = slice(kc * 128, (kc + 1) * 128)
                            chunks.append(([(KT[:, ks], QrT[:, hh, qs])], Vt[:, kc, :]))
                        jobs.append((256, chunks, attnT[:, hh, qs], attnT_r))
                self.attn_jobs(jobs, scale, rds, (EB, EBr))
            self.loadw(wo, self.gq_wo_d[kvh * 512:(kvh + 1) * 512, :].rearrange("(k p) n -> p k n", p=128), wo_r, "d_w0")
            for b in range(g.NB):
                tb = slice(b * 512, (b + 1) * 512)
                for m in range(KC):
                    ps, psr = self.next_ps()
                    for hh in range(4):
                        self.mm(ps[:, :], wo[:, hh, m * 128:(m + 1) * 128], attnT[:, hh, tb], hh == 0, hh == 3, reads=[wo_r, attnT_r], writes=[psr])
                    self.resid(b, m, tb, ps[:, :], psr, self.gate(l, 0, m, g.ci))

    def moe(self, g, l):
        kb, ar = self.kb, self.ar
        T, NT, NB, C, NS, NCC = g.T, g.NT, g.NB, g.C, g.NSLOT, g.NCC
        ci = g.ci
        h2tok, h2tok_r = ar.alloc("h2tok", [NT, D], BF16)
        aff, aff_r = ar.alloc("aff", [NT, NE], F32)
        affhl, affhl_r = ar.alloc("affhl", [NT, NE, 2], BF16)
        posg, posg_r = ar.alloc("posg", [NT, NE], F32)
        mk0 = ar.mark()
        affT, affT_r = ar.alloc("affT", [T], F32)
        mk1 = ar.mark()
        rw, rw_r = ar.alloc("rw", [KC, NE], F32)
        self.load(rw, self.rw_d[:, l, :, :], rw_r)
        tmps = self.norm_tmps()
        h2f = [ar.alloc(f"h2f{i}", [KC, 512], F32) for i in range(2)]
        sm, sm_r = ar.alloc("sm", [16], F32)
        ex, ex_r = ar.alloc("ex", [4, NE], F32)
        def norm_b(b):
            hf, hf_r = h2f[b % 2]
            self.norm_block(b, lambda m: self.gs[:, l, 1, m, ci:ci + 1], lambda m: self.shift(l, 1, m, ci),
                            lambda m: hf[:, m, :], hf_r, tmps, precise=True)

        def route_b(b):
            hf, hf_r = h2f[b % 2]
            pT, pTr = self.next_ps(pin=True)
            pl, plr = self.next_ps(pin=True)
            t4 = slice(b * 4, b * 4 + 4)
            for q in range(4):
                qs = slice(q * 128, (q + 1) * 128)
                for k in range(KC):
                    self.mm(pl[:, q * NE:(q + 1) * NE], hf[:, k, qs], rw[:, k, :], k == 0, k == KC - 1, reads=[hf_r, rw_r], writes=[plr])
            plv = pl[:, 0:4 * NE].rearrange("p (q e) -> p q e", q=4)
            kb.op("dve", lambda e: e.tensor_reduce(out=sm[:, 0:4], in_=plv, axis=AX.X, op=ALU.max), reads=[plr], writes=[sm_r])
            self.tt(ex, plv, sm[:, 0:4].unsqueeze(2).broadcast_to([128, 4, NE]), ALU.subtract, reads=[plr, sm_r], writes=[ex_r])
            self.act(ex, ex, AF.Exp, reads=[ex_r], writes=[ex_r])
            kb.op("dve", lambda e: e.tensor_reduce(out=sm[:, 4:8], in_=ex, axis=AX.X, op=ALU.add), reads=[ex_r], writes=[sm_r])
            kb.op("dve", lambda e: e.reciprocal(out=sm[:, 8:12], in_=sm[:, 4:8]), reads=[sm_r], writes=[sm_r])
            self.tt(aff[:, t4, :], ex, sm[:, 8:12].unsqueeze(2).broadcast_to([128, 4, NE]), ALU.mult, reads=[ex_r, sm_r], writes=[aff_r])
            self.unpin(plr)
            self.cp("dve", affhl[:, t4, :, 0], aff[:, t4, :], reads=[aff_r], writes=[affhl_r])
            self.tt(ex, aff[:, t4, :], affhl[:, t4, :, 0], ALU.subtract, reads=[aff_r, affhl_r], writes=[ex_r])
            self.cp("dve", affhl[:, t4, :, 1], ex, reads=[ex_r], writes=[affhl_r])
            for q in range(4):
                tt_ = b * 4 + q
                qs = slice(q * 128, (q + 1) * 128)
                self.tr(pT[0:NE, qs], aff[:, tt_, :], self.ident, reads=[aff_r, self.c_r], writes=[pTr])
                for half in range(2):
                    ph, phr = self.next_ps()
                    for mm_ in range(4):
                        m = half * 4 + mm_
                        self.tr(ph[:, mm_ * 128:(mm_ + 1) * 128], hf[:, m, qs], self.ident, reads=[hf_r, self.c_r], writes=[phr])
                    self.cp("act" if half == 0 else "dve", h2tok[:, tt_, half * 512:(half + 1) * 512], ph[:, :], reads=[phr], writes=[h2tok_r])
            self.cp("act", affT[0:NE, b * 512:(b + 1) * 512], pT[0:NE, :], reads=[pTr], writes=[affT_r])
            self.unpin(pTr)

        norm_b(0)
        for b in range(NB):
            if b + 1 < NB:
                norm_b(b + 1)
            route_b(b)
        ar.release(mk1)
        S = g.S
        work, work_r = ar.alloc("work", [S], F32)
        vals, vals_r = ar.alloc("vals", [C], F32)
        mask, mask_r = ar.alloc("mask", [S], F32)
        cum, cum_r = ar.alloc("cum", [S], F32)
        zer, zer_r = ar.alloc("zer", [S], F32)
        pgT, pgT_r = ar.alloc("pgT", [T], F32)
        kb.op("dve", lambda e: e.memset(zer[0:NE, :], 0.0), writes=[zer_r])
        ada_next = None
        if g.lat and l + 1 < self.depth:
            wb = [ar.alloc(f"adaw{i}", [KC, 1024], BF16) for i in range(2)]
            ada_next = self.adaln_issue(l + 1, wb)
        for s in range(g.nseq):
            ss = slice(s * S, (s + 1) * S)
            self.cp("dve", work[0:NE, :], affT[0:NE, ss], reads=[affT_r], writes=[work_r])
            for r in range(C // 8):
                kb.op("dve", lambda e: e.max(out=vals[0:NE, r * 8:(r + 1) * 8], in_=work[0:NE, :]), reads=[work_r], writes=[vals_r])
                if r < C // 8 - 1:
                    kb.op("dve", lambda e: e.match_replace(out=work[0:NE, :], in_to_replace=vals[0:NE, r * 8:(r + 1) * 8],
                                                           in_values=work[0:NE, :], imm_value=-1.0), reads=[work_r, vals_r], writes=[work_r])
            self.ts(mask[0:NE, :], affT[0:NE, ss], vals[0:NE, C - 1:C], None, ALU.is_ge, None, reads=[affT_r, vals_r], writes=[mask_r])
            kb.op("dve", lambda e: e.tensor_tensor_scan(out=cum[0:NE, :], data0=mask[0:NE, :], data1=zer[0:NE, :], initial=0.0,
                                                        op0=ALU.add, op1=ALU.add), reads=[mask_r, zer_r], writes=[cum_r])
            self.stt(mask[0:NE, :], cum[0:NE, :], float(C), mask[0:NE, :], ALU.is_le, ALU.mult, reads=[cum_r, mask_r], writes=[mask_r])
            self.ts(cum[0:NE, :], cum[0:NE, :], float(s * C - 1), None, ALU.add, None, reads=[cum_r], writes=[cum_r])
            self.tt(cum[0:NE, :], cum[0:NE, :], mask[0:NE, :], ALU.mult, reads=[cum_r, mask_r], writes=[cum_r])
            self.ts(mask[0:NE, :], mask[0:NE, :], 4096.0, -4096.0, ALU.mult, ALU.add, reads=[mask_r], writes=[mask_r])
            self.tt(pgT[0:NE, ss], cum[0:NE, :], mask[0:NE, :], ALU.add, reads=[cum_r, mask_r], writes=[pgT_r])
        pp, ppr = self.next_ps()
        for tt_ in range(NT):
            self.tr(pp[:, tt_ * NE:(tt_ + 1) * NE], pgT[0:NE, tt_ * 128:(tt_ + 1) * 128], self.ident[0:NE, 0:NE], reads=[pgT_r, self.c_r], writes=[ppr])
        self.cp("dve", posg, pp[:, 0:NT * NE].rearrange("p (t e) -> p t e", e=NE), reads=[ppr], writes=[posg_r])
        if ada_next is not None:
            self.adaln_finish(l + 1, *ada_next)
        self.dbg("aff", aff, aff_r)
        self.dbg("posg", posg, posg_r)
        self.dbg("h2tok", h2tok, h2tok_r)
        self.dbg("affT", affT[0:NE, :], affT_r)
        self.dbg("pgT", pgT[0:NE, :], pgT_r)
        self.dbg("vals", vals[0:NE, :], vals_r)
        ar.release(mk0)
        wslots = []
        NWS = 2 if (g.lat or "L" in self.groups) else 4
        CG = 1 if g.lat else 8
        NBUF = 2 if g.lat else 12
        for i in range(NWS):
            a = ar.alloc(f"wg{i}", [KC, FF], BF16)
            b_ = ar.alloc(f"wu{i}", [KC, FF], BF16)
            c_ = ar.alloc(f"wd{i}", [4, D], BF16)
            wslots.append((a, b_, c_))
        Sel = [ar.alloc(f"Sel{i}", [NT, NS], BF16) for i in range(2)]
        SelT = [ar.alloc(f"SelT{i}", [NCC, T], BF16) for i in range(NBUF)]
        XG = [ar.alloc(f"xg{i}", [KC, NS], BF16) for i in range(2)]
        sg, sg_r = ar.alloc("sg", [NS], F32)
        hid, hid_r = ar.alloc("hid", [4, NS], BF16)
        YO = [ar.alloc(f"yo{i}", [NCC, D], BF16) for i in range(NBUF)]
        GSL = [ar.alloc(f"gsl{i}", [4], F32) for i in range(2)]

        use_scr = (not g.lat) and ("L" in self.groups)

        def load_w(e):
            (wg, wg_r), (wu, wu_r), (wd, wd_r) = wslots[e % NWS]
            if use_scr:
                sc = self.wscr_d[l, e]
                kb.dma("sp", [(wg.rearrange("p a b -> p (a b)"), sc[:, 0:4096])], f"d_m{e % NWS}a", reads=[self.wscr_rs[l]], writes=[wg_r])
                kb.dma("sp", [(wu.rearrange("p a b -> p (a b)"), sc[:, 4096:8192])], f"d_m{e % NWS}b", reads=[self.wscr_rs[l]], writes=[wu_r])
                kb.dma("sp", [(wd.rearrange("p a b -> p (a b)"), sc[:, 8192:12288])], f"d_m{e % NWS}c", reads=[self.wscr_rs[l]], writes=[wd_r])
                return
            self.loadw(wg, self.wg_d[l, e].rearrange("(k p) n -> p k n", p=128), wg_r, f"d_m{e % NWS}a")
            self.loadw(wu, self.wu_d[l, e].rearrange("(k p) n -> p k n", p=128), wu_r, f"d_m{e % NWS}b")
            self.loadw(wd, self.wd_d[l, e].rearrange("(k p) n -> p k n", p=128), wd_r, f"d_m{e % NWS}c")
            if g.lat:
                sc = self.wscr_d[l, e]
                kb.dma("sp", [(sc[:, 0:4096], wg.rearrange("p a b -> p (a b)")),
                              (sc[:, 4096:8192], wu.rearrange("p a b -> p (a b)")),
                              (sc[:, 8192:12288], wd.rearrange("p a b -> p (a b)"))], "d_ws",
                       reads=[wg_r, wu_r, wd_r], writes=[self.wscr_rs[l]])

        def s1_sel(e):
            sel, sel_r = Sel[e % 2]
            for tt_ in range(NT):
                self.ts(sel[:, tt_, :], self.iotab[:, 0:NS], posg[:, tt_, e:e + 1], None, ALU.is_equal, None,
                        reads=[posg_r, self.c_r], writes=[sel_r])

        def s1_rest(e, comb=None):
            sel, sel_r = Sel[e % 2]
            selT, selT_r = SelT[e % NBUF]
            xg, xg_r = XG[e % 2]
            gsl, gsl_r = GSL[e % 2]
            for m in range(KC):
                if m % 2 == 0:
                    pgm, pgmr = self.next_ps(pin=True)
                o = pgm[:, (m % 2) * 256:(m % 2) * 256 + NS]
                for tt_ in range(NT):
                    self.mm(o, h2tok[:, tt_, m * 128:(m + 1) * 128], sel[:, tt_, :], tt_ == 0, tt_ == NT - 1, reads=[h2tok_r, sel_r], writes=[pgmr])
                if comb is not None:
                    for _ in range((NB * KC + KC - 1) // KC):
                        next(comb, None)
                if m % 2 == 1:
                    self.cp("act", xg[:, m - 1:m + 1, :],
                            pgm[:, :].rearrange("p (a b) -> p a b", a=2)[:, :, 0:NS], reads=[pgmr], writes=[xg_r])
                    self.unpin(pgmr)
            if comb is not None:
                for _ in comb:
                    pass
            pg_, pg_r = self.next_ps()
            for cc in range(NCC):
                for tt_ in range(NT):
                    self.mm(pg_[:, 2 * cc:2 * cc + 2], sel[:, tt_, cc * 128:(cc + 1) * 128], affhl[:, tt_, e, :], tt_ == 0, tt_ == NT - 1,
                            reads=[sel_r, affhl_r], writes=[pg_r])
            for cc in range(NCC):
                kb.op("dve", lambda en: en.tensor_reduce(out=gsl[:, cc:cc + 1], in_=pg_[:, 2 * cc:2 * cc + 2], axis=AX.X, op=ALU.add), reads=[pg_r], writes=[gsl_r])
            for cc in range(NCC):
                for t0 in range(0, NT, 8):
                    pt, ptr = self.next_ps()
                    pbt = pt[:, :].bitcast(BF16)
                    for q in range(8):
                        self.tr(pbt[:, q * 128:(q + 1) * 128], sel[:, t0 + q, cc * 128:(cc + 1) * 128], self.identb, reads=[sel_r, self.c_r], writes=[ptr])
                    self.cp("act", selT[:, cc, t0 * 128:(t0 + 8) * 128], pbt[:, :], reads=[ptr], writes=[selT_r])

        def s2a(e):
            (wg, wg_r), (wu, wu_r), (wd, wd_r) = wslots[e % NWS]
            xg, xg_r = XG[e % 2]
            for f in range(4):
                pf, pfr = self.next_ps()
                for k in range(KC):
                    self.mm(pf[:, 0:NS], wg[:, k, f * 128:(f + 1) * 128], xg[:, k, :], k == 0, k == KC - 1, reads=[wg_r, xg_r], writes=[pfr])
                for k in range(KC):
                    self.mm(pf[:, 256:256 + NS], wu[:, k, f * 128:(f + 1) * 128], xg[:, k, :], k == 0, k == KC - 1, reads=[wu_r, xg_r], writes=[pfr])
                self.act(sg, pf[:, 0:NS], AF.Silu, reads=[pfr], writes=[sg_r])
                self.tt(hid[:, f, :], sg, pf[:, 256:256 + NS], ALU.mult, reads=[sg_r, pfr], writes=[hid_r])

        def s2b(e):
            (wg, wg_r), (wu, wu_r), (wd, wd_r) = wslots[e % NWS]
            yo, yo_r = YO[e % NBUF]
            gsl, gsl_r = GSL[e % 2]
            for cc in range(NCC):
                for db in range(2):
                    py, pyr = self.next_ps()
                    for f in range(4):
                        self.mm(py[:, :], hid[:, f, cc * 128:(cc + 1) * 128], wd[:, f, db * 512:(db + 1) * 512], f == 0, f == 3, reads=[hid_r, wd_r], writes=[pyr])
                    self.act(yo[:, cc, db * 512:(db + 1) * 512], py[:, :], AF.Copy, reads=[pyr, gsl_r], writes=[yo_r], scale=gsl[:, cc:cc + 1])

        def s3(es):
            for b in range(NB):
                tb = slice(b * 512, (b + 1) * 512)
                for m in range(KC):
                    pc, pcr = self.next_ps()
                    n = len(es) * NCC
                    i_ = 0
                    for e in es:
                        yo, yo_r = YO[e % NBUF]
                        selT, selT_r = SelT[e % NBUF]
                        for cc in range(NCC):
                            self.mm(pc[:, :], yo[:, cc, m * 128:(m + 1) * 128], selT[:, cc, tb], i_ == 0, i_ == n - 1, reads=[yo_r, selT_r], writes=[pcr])
                            i_ += 1
                    self.resid(b, m, tb, pc[:, :], pcr, self.gate(l, 1, m, ci))
                    yield

        load_w(0)
        s1_sel(0)
        s1_rest(0)
        for i in range(1, min(NWS - 1, NE)):
            load_w(i)
        for i in range(NE):
            if i + NWS - 1 < NE:
                load_w(i + NWS - 1)
            if i + 1 < NE:
                s1_sel(i + 1)
            s2a(i)
            comb = s3(list(range(i - CG, i))) if (i >= CG and i % CG == 0) else None
            if i + 1 < NE:
                s1_rest(i + 1, comb)
            elif comb is not None:
                for _ in comb:
                    pass
            s2b(i)
        for _ in s3(list(range(NE - CG, NE))):
            pass


def _fm(v):
    v = np.asarray(v, np.float32)
    lead = v.shape[:-1]
    n = v.shape[-1] // 128
    return np.ascontiguousarray(np.moveaxis(v.reshape(*lead, n, 128), -1, 0))


def _rope_tables(n_tokens, rot_dim, grid_w=64, theta=10000.0):
    rows = n_tokens // grid_w
    row = np.repeat(np.arange(rows), grid_w).astype(np.float32)
    col = np.tile(np.arange(grid_w), rows).astype(np.float32)
    axis_dim = rot_dim // 2
    inv = (np.float32(theta) ** (-np.arange(0, axis_dim, 2, dtype=np.float32) / np.float32(axis_dim))).astype(np.float32)
    ang = np.concatenate([row[:, None] * inv, col[:, None] * inv], axis=-1).astype(np.float32)
    return np.cos(ang).astype(np.float32), np.sin(ang).astype(np.float32), inv


def _consts():
    c = np.zeros((128, 512), np.float32)
    c[:, 0:128] = np.eye(128, dtype=np.float32)
    c[:, 128:256] = 1.0 / 1024
    c[:, 256:512] = np.arange(256, dtype=np.float32)[None, :]
    return c


def _pool_rc(S):
    out = np.zeros((128, 4, 16), np.float32)
    for gi, win in enumerate((2, 4, 8, 16)):
        t = np.concatenate([np.arange(8), np.arange(S - 8, S)])
        lo = np.clip(t - win // 2, 0, S)
        hi = np.clip(t + win // 2, 0, S)
        out[:, gi, :] = (1.0 / (hi - lo).astype(np.float32))[None, :]
    return out


_PROG_CACHE = {}


def _get_prog(depth=4, **kw):
    if depth not in _PROG_CACHE:
        p = Prog(depth, **kw)
        p.build()
        _PROG_CACHE[depth] = p
    return _PROG_CACHE[depth]


def make_in_maps(inp, cores=range(NCORES)):
    f32 = lambda a: np.ascontiguousarray(np.asarray(a, np.float32))
    L = 4
    shared = {}
    shared["ada_w"] = f32(inp["ada_w"])
    shared["ada_b"] = f32(np.moveaxis(np.asarray(inp["ada_b"], np.float32).reshape(L, 48, 128), -1, 0))
    shared["g1"] = _fm(inp["norm1_g"])
    shared["g2"] = _fm(inp["norm2_g"])
    shared["gf"] = _fm(inp["final_g"])
    shared["cst"] = _consts()
    shared["pool_w"] = f32(inp["pool_w"][0])
    shared["pool_s"] = _fm(inp["pool_scale"][0])
    shared["mla_wd"] = f32(inp["mla_w_down"][0])
    shared["mla_gq"] = f32(np.broadcast_to(np.asarray(inp["mla_g_q"][0], np.float32)[None, :], (128, 384)))
    wuq = np.asarray(inp["mla_w_uq"][0], np.float32).reshape(384, 8, 192)
    rope = wuq[:, :, 128:192]
    swapped = np.concatenate([rope[:, :, 32:64], rope[:, :, 0:32]], axis=-1)
    shared["mla_wuq"] = f32(np.concatenate([wuq[:, :, 0:128], rope, swapped, rope], axis=-1).reshape(384, 2560))
    shared["mla_gkv"] = f32(np.broadcast_to(np.asarray(inp["mla_g_kv"][0], np.float32)[None, :], (128, 256)))
    shared["mla_wukv"] = f32(inp["mla_w_ukv"][0])
    shared["mla_wo"] = f32(inp["mla_w_o"][0])
    cos, sin, _ = _rope_tables(2048, 64)
    rtok = np.stack([cos, sin], 0).reshape(2, 16, 128, 32).transpose(2, 0, 1, 3)
    shared["mla_rtok"] = f32(rtok)
    rows = np.arange(32, dtype=np.float32)
    cols = np.arange(64, dtype=np.float32)
    inv = _rope_tables(2048, 64)[2]
    rA = np.ones((64, 2, 32), np.float32)
    rB = np.ones((64, 2, 64), np.float32)
    for i in range(64):
        ii = i % 32
        sgn = -1.0 if i < 32 else 1.0
        if ii < 16:
            a = (rows * inv[ii]).astype(np.float32)
            rA[i, 0] = np.cos(a)
            rA[i, 1] = sgn * np.sin(a)
        else:
            a = (cols * inv[ii - 16]).astype(np.float32)
            rB[i, 0] = np.cos(a)
            rB[i, 1] = sgn * np.sin(a)
    shared["mla_rA"] = rA
    shared["mla_rB"] = rB
    shared["cv_w1"] = f32(inp["conv_w_pw1"][0])
    shared["cv_b1"] = _fm(inp["conv_b_pw1"][0])
    shared["cv_wdw"] = f32(np.asarray(inp["conv_w_dw"][0], np.float32).T.reshape(8, 128, 31).transpose(1, 0, 2))
    shared["cv_vec"] = f32(np.stack([_fm(inp["conv_b_dw"][0]), _fm(inp["conv_ln_g"][0]), _fm(inp["conv_ln_b"][0]),
                                     _fm(inp["conv_b_pw2"][0])], axis=1))
    shared["cv_w2"] = f32(inp["conv_w_pw2"][0])
    shared["gq_w"] = f32(inp["gqa_w_qkv"][0])
    shared["gq_g"] = f32(np.broadcast_to(np.stack([np.asarray(inp["gqa_g_q"][0], np.float32),
                                                    np.asarray(inp["gqa_g_k"][0], np.float32)], 0)[None], (128, 2, 128)))
    shared["gq_wo"] = f32(inp["gqa_w_o"][0])
    cos, sin, _ = _rope_tables(2048, 128)
    shared["gq_rtok"] = f32(np.stack([cos, sin], 0).reshape(2, 16, 128, 64).transpose(2, 0, 1, 3))
    shared["rw"] = f32(np.asarray(inp["router_w"], np.float32).reshape(L, 8, 128, NE).transpose(2, 0, 1, 3))
    shared["moe_wg"] = f32(inp["moe_w_gate"])
    shared["moe_wu"] = f32(inp["moe_w_up"])
    shared["moe_wd"] = f32(inp["moe_w_down"])
    shared["pool_rc"] = _pool_rc(2048)
    maps = []
    xs = np.asarray(inp["x_sample"], np.float32)
    xp = np.asarray(inp["x_prompt"], np.float32)
    cc = np.asarray(inp["c"], np.float32)
    cctx = np.asarray(inp["c_ctx"], np.float32)
    for i in cores:
        m = dict(shared)
        m["xl"] = f32(xs[i].T.reshape(8, 128, 2048).transpose(1, 0, 2))
        m["xc"] = f32(xp[4 * i:4 * i + 4].reshape(1024, 1024).T.reshape(8, 128, 1024).transpose(1, 0, 2))
        m["cond"] = f32(np.stack([_fm(cc[i]), _fm(cctx)], axis=-1))
        m["mla_cckv"] = f32(inp["cache_mla_ckv"][i, 0])
        m["mla_ckr"] = f32(inp["cache_mla_krope"][i, 0])
        m["gq_ck"] = f32(np.asarray(inp["cache_gqa_k"][i, 0]).reshape(512, 256))
        m["gq_cv"] = f32(np.asarray(inp["cache_gqa_v"][i, 0]).reshape(512, 256))
        maps.append(m)
    return maps


def kernel(**inputs):
    prog = _get_prog(4)
    maps = make_in_maps(inputs)
    res = run_bass_kernel_spmd(prog.nc, maps, core_ids=list(range(NCORES)))
    return assemble(res.results)


def assemble(results):
    n = len(results)
    y_prompt = np.zeros((4 * n, 256, D), np.float32)
    y_sample = np.zeros((n, 2048, D), np.float32)
    s_ckv = np.zeros((4 * n, 1, 256, 256), np.float32)
    s_kr = np.zeros((4 * n, 1, 256, 64), np.float32)
    s_k = np.zeros((4 * n, 1, 256, 2, 128), np.float32)
    s_v = np.zeros((4 * n, 1, 256, 2, 128), np.float32)
    for i, r in enumerate(results):
        y_sample[i] = r["yl"].transpose(1, 0, 2).reshape(D, 2048).T
        y_prompt[4 * i:4 * i + 4] = r["yc"].transpose(1, 0, 2).reshape(D, 1024).T.reshape(4, 256, D)
        s_ckv[4 * i:4 * i + 4, 0] = r["st_ckv"].reshape(4, 256, 256)
        s_kr[4 * i:4 * i + 4, 0] = r["st_kr"].reshape(4, 256, 64)
        s_k[4 * i:4 * i + 4, 0] = r["st_k"].reshape(4, 256, 2, 128)
        s_v[4 * i:4 * i + 4, 0] = r["st_v"].reshape(4, 256, 2, 128)
    return (y_prompt, y_sample, s_ckv, s_kr, s_k, s_v)
```

```python
import contextlib
import numpy as np
import concourse.bass as bass
import concourse.mybir as mybir
from concourse.bass_utils import run_bass_kernel_spmd

F32 = mybir.dt.float32
BF16 = mybir.dt.bfloat16
AF = mybir.ActivationFunctionType
ALU = mybir.AluOpType
AX = mybir.AxisListType

D = 1024
KC = 8
NE = 16
FF = 512
EPS = 1e-6
NCORES = 8


class Res:
    __slots__ = ("name", "w", "r", "excl")

    def __init__(self, name, excl=False):
        self.name = name
        self.w = None
        self.r = {}
        self.excl = excl


class Eng:
    def __init__(self, name, eng, sem):
        self.name = name
        self.eng = eng
        self.sem = sem
        self.count = 0
        self.waited = {}


class KB:
    def __init__(self, nc):
        self.nc = nc
        self.stack = contextlib.ExitStack()
        self.sems = {}
        self.dma_tot = {}
        self.E = {}
        for name, e in (("pe", nc.tensor), ("act", nc.scalar), ("dve", nc.vector),
                        ("pool", nc.gpsimd), ("sp", nc.sync)):
            s = self.stack.enter_context(nc.semaphore("s_" + name))
            self.sems["s_" + name] = s
            self.E[name] = Eng(name, e, "s_" + name)
        self.n_wait = 0
        self.n_ins = 0
        self.snaps = {}

    def sbuf(self, name, shape, dtype):
        return self.stack.enter_context(self.nc.sbuf_tensor(name, list(shape), dtype))

    def psum(self, name, shape, dtype=F32):
        return self.stack.enter_context(self.nc.psum_tensor(name, list(shape), dtype))

    def dsem(self, key):
        if key not in self.sems:
            self.sems[key] = self.stack.enter_context(self.nc.semaphore(key))
            self.dma_tot[key] = 0
        return key

    def _wait(self, E, key, val):
        if key in self.dma_tot:
            val = max(val, self.dma_tot[key])
        if E.waited.get(key, 0) >= val:
            return
        E.eng.wait_ge(self.sems[key], val)
        E.waited[key] = val
        self.n_wait += 1
        snap = self.snaps.get((key, val))
        if snap:
            for k2, v2 in snap.items():
                if E.waited.get(k2, 0) < v2:
                    E.waited[k2] = v2

    def _dep(self, E, tok):
        key, val = tok
        if E.name == "pe" and key == "s_pe":
            return
        self._wait(E, key, val)

    def _sync(self, E, reads, writes):
        for res in reads:
            if res.w is not None:
                self._dep(E, res.w)
            if res.excl:
                for k, v in res.r.items():
                    if k != E.sem:
                        self._dep(E, (k, v))
        for res in writes:
            if res.w is not None and res.w[0] != E.sem:
                self._dep(E, res.w)
            for k, v in res.r.items():
                if k != E.sem:
                    self._dep(E, (k, v))

    def _mark(self, tok, reads, writes):
        key, val = tok
        for res in reads:
            if res.r.get(key, 0) < val:
                res.r[key] = val
        for res in writes:
            res.w = tok
            res.r = {}

    def op(self, ename, fn, reads=(), writes=()):
        E = self.E[ename]
        self._sync(E, reads, writes)
        ins = fn(E.eng)
        E.count += 1
        ins.then_inc(self.sems[E.sem], 1)
        self.snaps[(E.sem, E.count)] = dict(E.waited)
        self._mark((E.sem, E.count), reads, writes)
        self.n_ins += 1
        return ins

    def dma(self, qname, pairs, sem_key, reads=(), writes=(), **kw):
        E = self.E[qname]
        self.dsem(sem_key)
        self._sync(E, reads, writes)
        for (o, i) in pairs:
            ins = E.eng.dma_start(out=o, in_=i, **kw)
            ins.then_inc(self.sems[sem_key], 16)
            self.dma_tot[sem_key] += 16
            self.n_ins += 1
        tok = (sem_key, self.dma_tot[sem_key])
        self._mark(tok, reads, writes)
        return tok

    def barrier(self):
        for E in self.E.values():
            for F in self.E.values():
                if F is not E and F.count:
                    self._wait(E, F.sem, F.count)
            for k, tot in self.dma_tot.items():
                if tot:
                    self._wait(E, k, tot)

    def close(self):
        self.stack.close()


class Arena:
    def __init__(self, kb, nbytes):
        self.kb = kb
        self.n = nbytes
        self.t = kb.sbuf("arena", [128, nbytes // 4], F32)
        self.off = 0
        self.top = nbytes

    def reset(self):
        self.kb.barrier()
        self.off = 0
        self.top = self.n

    def release_top(self):
        self.kb.barrier()
        self.top = self.n

    def mark(self):
        return self.off

    def release(self, mark):
        self.kb.barrier()
        self.off = mark

    def alloc(self, name, shape, dtype, parts=128, top=False):
        esz = 2 if dtype == BF16 else 4
        n = int(np.prod(shape))
        nb = (n * esz + 31) // 32 * 32
        assert self.off + nb <= self.top, f"arena overflow at {name}: {self.off}+{nb}>{self.top}"
        if top:
            self.top -= nb
            start = self.top
        else:
            start = self.off
            self.off += nb
        v = self.t[0:parts, start // 4:(start + nb) // 4]
        if dtype != F32:
            v = v.bitcast(dtype)
        v = v[:, 0:n]
        if len(shape) == 2:
            v = v.rearrange("p (a b) -> p a b", a=shape[0])
        elif len(shape) == 3:
            v = v.rearrange("p (a b c) -> p a b c", a=shape[0], b=shape[1])
        return v, Res(name)


class Grp:
    def __init__(self, name, T, nseq, S, ci):
        self.name = name
        self.T = T
        self.nseq = nseq
        self.S = S
        self.ci = ci
        self.NT = T // 128
        self.NB = T // 512
        self.C = S // 8
        self.NSLOT = nseq * self.C
        self.NCC = self.NSLOT // 128
        self.lat = (ci == 0)


class Prog:
    def __init__(self, depth=4, do_moe=True, do_mixer=True, debug=False, groups="LC", stop=""):
        self.depth = depth
        self.groups = groups
        self.stop = stop
        self.debug = debug
        self.do_moe = do_moe
        self.do_mixer = do_mixer
        nc = self.nc = bass.Bass("TRN2", target_bir_lowering=False)
        self.kb = kb = KB(nc)
        self.din = {}
        self.dout = {}

    def inp(self, name, shape, dt=F32):
        ap = self.nc.dram_tensor(name, list(shape), dt, kind="ExternalInput").ap()
        self.din[name] = ap
        return ap

    def outp(self, name, shape):
        ap = self.nc.dram_tensor(name, list(shape), F32, kind="ExternalOutput").ap()
        self.dout[name] = ap
        return ap

    def dbg(self, name, ap, res):
        if not getattr(self, "debug", False) or name in self.dout:
            return
        shp = list(ap.shape)
        d = self.nc.dram_tensor("dbg_" + name, shp, ap.dtype, kind="ExternalOutput").ap()
        self.dout[name] = d
        self.kb.dma("sp", [(d, ap)], "d_dbg", reads=[res])

    def next_ps(self, pin=False):
        i = self.ps_i
        while i in self.ps_pinned:
            i = (i + 1) % 8
        self.ps_i = (i + 1) % 8
        if pin:
            self.ps_pinned.add(i)
        return self.ps[i], self.psr[i]

    def unpin(self, psr):
        self.ps_pinned.discard(self.psr.index(psr))

    def mm(self, out, lhsT, rhs, start, stop, reads, writes):
        self.kb.op("pe", lambda e: e.matmul(out, lhsT, rhs, start=start, stop=stop), reads=reads, writes=writes)

    def tr(self, out, in_, ident, reads, writes):
        self.kb.op("pe", lambda e: e.transpose(out, in_, ident), reads=reads, writes=writes)

    def act(self, out, in_, func, reads, writes, bias=None, scale=1.0, accum_out=None):
        kw = {}
        if bias is not None:
            kw["bias"] = bias
        if accum_out is not None:
            kw["accum_out"] = accum_out
        self.kb.op("act", lambda e: e.activation(out=out, in_=in_, func=func, scale=scale, **kw),
                   reads=reads, writes=writes)

    def tt(self, out, in0, in1, op, reads, writes, eng="dve"):
        self.kb.op(eng, lambda e: e.tensor_tensor(out=out, in0=in0, in1=in1, op=op), reads=reads, writes=writes)

    def ts(self, out, in0, s1, s2, op0, op1, reads, writes, eng="dve", accum_out=None):
        if s2 is None:
            self.kb.op(eng, lambda e: e.tensor_scalar(out=out, in0=in0, scalar1=s1, scalar2=None, op0=op0),
                       reads=reads, writes=writes)
        else:
            self.kb.op(eng, lambda e: e.tensor_scalar(out=out, in0=in0, scalar1=s1, scalar2=s2, op0=op0, op1=op1),
                       reads=reads, writes=writes)

    def stt(self, out, in0, scalar, in1, op0, op1, reads, writes):
        self.kb.op("dve", lambda e: e.scalar_tensor_tensor(out=out, in0=in0, scalar=scalar, in1=in1, op0=op0, op1=op1),
                   reads=reads, writes=writes)

    def cp(self, eng, out, in_, reads, writes):
        if eng == "act":
            self.kb.op("act", lambda e: e.copy(out=out, in_=in_), reads=reads, writes=writes)
        else:
            self.kb.op(eng, lambda e: e.tensor_copy(out=out, in_=in_), reads=reads, writes=writes)

    def load(self, out, in_, res, sem="d_ld", q="sp", **kw):
        self.kb.dma(q, [(out, in_)], sem, writes=[res], **kw)

    def loadw(self, out, in_, res, sem):
        n = out.shape[-1]
        pairs = []
        if n <= 1024:
            self.kb.dma("pool", [(out, in_)], sem, writes=[res])
            return
        for c0 in range(0, n, 1024):
            c1 = min(n, c0 + 1024)
            if len(out.shape) == 3:
                pairs.append((out[:, :, c0:c1], in_[:, :, c0:c1]))
            else:
                pairs.append((out[:, c0:c1], in_[:, c0:c1]))
        self.kb.dma("pool", pairs, sem, writes=[res])

    def rstd_from(self, out, in_, n, reads, writes, tmp, tmp_r):
        P = out.shape[0]
        self.act(tmp, in_, AF.Ln, reads=list(reads) + [self.c_r], writes=[tmp_r], bias=self.eps_t[0:P, :], scale=1.0 / n)
        self.act(out, tmp, AF.Exp, reads=[tmp_r], writes=writes, scale=-0.5)

    def build(self):
        nc, kb = self.nc, self.kb
        L = 4
        inp, outp = self.inp, self.outp
        xl_d = inp("xl", [128, KC, 2048])
        xc_d = inp("xc", [128, KC, 1024])
        cond_d = inp("cond", [128, KC, 2])
        ada_w_d = inp("ada_w", [L, D, 6 * D])
        ada_b_d = inp("ada_b", [128, L, 48])
        g1_d = inp("g1", [128, L, KC])
        g2_d = inp("g2", [128, L, KC])
        gf_d = inp("gf", [128, KC])
        cst_d = inp("cst", [128, 128 + 128 + 256])
        pool_w_d = inp("pool_w", [4, 256, 256])
        pool_s_d = inp("pool_s", [128, KC])
        pool_rc_d = inp("pool_rc", [128, 4, 16])
        mla_wd_d = inp("mla_wd", [D, 704])
        mla_gq_d = inp("mla_gq", [128, 384])
        mla_wuq_d = inp("mla_wuq", [384, 2560])
        mla_gkv_d = inp("mla_gkv", [128, 256])
        mla_wukv_d = inp("mla_wukv", [256, 2048])
        mla_wo_d = inp("mla_wo", [D, D])
        mla_cckv_d = inp("mla_cckv", [512, 256])
        mla_ckr_d = inp("mla_ckr", [512, 64])
        mla_rtok_d = inp("mla_rtok", [128, 2, 16, 32])
        mla_rA_d = inp("mla_rA", [64, 2, 32])
        mla_rB_d = inp("mla_rB", [64, 2, 64])
        cv_w1_d = inp("cv_w1", [D, 2 * D])
        cv_b1_d = inp("cv_b1", [128, 16])
        cv_wdw_d = inp("cv_wdw", [128, KC, 31])
        cv_vec_d = inp("cv_vec", [128, 4, KC])
        cv_w2_d = inp("cv_w2", [D, D])
        gq_w_d = inp("gq_w", [D, 1536])
        gq_g_d = inp("gq_g", [128, 2, 128])
        gq_wo_d = inp("gq_wo", [D, D])
        gq_ck_d = inp("gq_ck", [512, 256])
        gq_cv_d = inp("gq_cv", [512, 256])
        gq_rtok_d = inp("gq_rtok", [128, 2, 16, 64])
        rw_d = inp("rw", [128, L, KC, NE])
        wg_d = inp("moe_wg", [L, NE, D, FF])
        wu_d = inp("moe_wu", [L, NE, D, FF])
        wd_d = inp("moe_wd", [L, NE, FF, D])
        wscr_d = self.nc.dram_tensor("wscr", [L, NE, 128, 12288], BF16, kind="Internal").ap()
        self.wscr_rs = [Res(f"wscr{i}") for i in range(L)]
        yl_d = outp("yl", [128, KC, 2048])
        yc_d = outp("yc", [128, KC, 1024])
        st_ckv_d = outp("st_ckv", [1024, 256])
        st_kr_d = outp("st_kr", [1024, 64])
        st_k_d = outp("st_k", [1024, 256])
        st_v_d = outp("st_v", [1024, 256])
        self.__dict__.update(locals())

        self.X = kb.sbuf("X", [128, KC, 2048], F32)
        self.Xr = [Res(f"X{b}") for b in range(4)]
        self.cst = kb.sbuf("cstt", [128, 512], F32)
        self.c_r = Res("cst")
        self.ident = self.cst[:, 0:128]
        self.onesf = self.cst[:, 128:256]
        self.iota = self.cst[:, 256:512]
        self.cb = kb.sbuf("cb", [128, 640], BF16)
        self.identb = self.cb[:, 0:128]
        self.onesb_s = self.cb[:, 128:256]
        self.onesb = self.cb[:, 256:384]
        self.iotab = self.cb[:, 384:640]
        self.eps_t = kb.sbuf("eps", [128, 1], F32)
        self.mods = kb.sbuf("mods", [128, L, 48, 2], F32)
        self.mod_rs = [Res(f"mods{i}") for i in range(L)]
        self.mod_r = self.mod_rs[0]
        self.cnd = kb.sbuf("cnd", [128, KC, 2], F32)[:, :, :]
        self.cndb = kb.sbuf("cndb", [128, KC, 2], BF16)[:, :, :]
        self.adab = kb.sbuf("adab", [128, L, 48], F32)[:, :, :]
        self.cnd_r = Res("cnd")
        self.gs = kb.sbuf("gs", [128, L, 2, KC, 2], F32)
        self.vecs = kb.sbuf("vecs", [128, 3 * L * KC + KC + KC], F32)
        self.g1 = self.vecs[:, 0:L * KC].rearrange("p (l m) -> p l m", l=L)
        self.g2 = self.vecs[:, L * KC:2 * L * KC].rearrange("p (l m) -> p l m", l=L)
        self.gf = self.vecs[:, 3 * L * KC:3 * L * KC + KC]
        self.ps = [kb.psum(f"ps{i}", [128, 512], F32) for i in range(8)]
        self.psr = [Res(f"ps{i}", excl=True) for i in range(8)]
        self.ps_i = 0
        self.ps_pinned = set()
        self.ar = Arena(kb, 136 * 1024)

        kb.dma("sp", [(self.cst[:], cst_d[:, :])], "d_c", writes=[self.c_r])
        kb.dma("sp", [(self.g1, g1_d[:, :, :]), (self.g2, g2_d[:, :, :]), (self.gf, gf_d[:, :])], "d_c", writes=[self.c_r])
        kb.op("dve", lambda e: e.memset(self.eps_t[:], EPS), writes=[self.c_r])
        kb.op("dve", lambda e: e.tensor_copy(out=self.identb, in_=self.ident), reads=[self.c_r], writes=[self.c_r])
        kb.op("dve", lambda e: e.tensor_copy(out=self.onesb_s, in_=self.onesf), reads=[self.c_r], writes=[self.c_r])
        kb.op("dve", lambda e: e.memset(self.onesb, 1.0), writes=[self.c_r])
        kb.op("dve", lambda e: e.tensor_copy(out=self.iotab, in_=self.iota), reads=[self.c_r], writes=[self.c_r])

        self.adaln_setup()
        for grp in (Grp("L", 2048, 1, 2048, 0), Grp("C", 1024, 4, 256, 1)):
            if grp.name in getattr(self, "groups", "LC"):
                self.run_group(grp)
        kb.barrier()

    def adaln_setup(self):
        self.load(self.cnd, self.cond_d[:, :, :], self.cnd_r)
        self.load(self.adab, self.ada_b_d[:, :, :], self.cnd_r)
        self.act(self.cndb, self.cnd, AF.Silu, reads=[self.cnd_r], writes=[self.cnd_r])

    def adaln_issue(self, l, wb):
        ps, psr = self.next_ps(pin=True)
        for jb in range(6):
            w, w_r = wb[jb % 2]
            self.loadw(w, self.ada_w_d[l, :, jb * 1024:(jb + 1) * 1024].rearrange("(k p) n -> p k n", p=128), w_r, f"d_a{jb % 2}")
            for jj in range(8):
                j = jb * 8 + jj
                for k in range(KC):
                    self.mm(ps[:, 2 * j:2 * j + 2], w[:, k, jj * 128:(jj + 1) * 128], self.cndb[:, k, :],
                            k == 0, k == KC - 1, reads=[w_r, self.cnd_r], writes=[psr])
        return ps, psr

    def adaln_finish(self, l, ps, psr):
        mr = self.mod_rs[l]
        self.tt(self.mods[:, l, :, :], ps[:, 0:96].rearrange("p (j c) -> p j c", c=2),
                self.adab[:, l, :].unsqueeze(2).broadcast_to([128, 48, 2]), ALU.add,
                reads=[psr, self.cnd_r], writes=[mr])
        self.unpin(psr)
        for which, g in ((0, self.g1), (1, self.g2)):
            j0 = 8 + 24 * which
            self.stt(self.gs[:, l, which, :, :], self.mods[:, l, j0:j0 + 8, :], 1.0,
                     g[:, l, :].unsqueeze(2).broadcast_to([128, KC, 2]), ALU.add, ALU.mult,
                     reads=[mr, self.c_r], writes=[mr])

    def shift(self, l, which, m, ci):
        return self.mods[:, l, 24 * which + m, ci:ci + 1]

    def gate(self, l, which, m, ci):
        return self.mods[:, l, 16 + 24 * which + m, ci:ci + 1]

    def norm_block(self, b, gs_fn, sh_fn, out_fn, out_r, tmps, precise=False):
        X, Xr = self.X, self.Xr
        sq, sq_r, lnv, lnv_r, rstd, rstd_r, tm, tm_r = tmps
        tb = slice(b * 512, (b + 1) * 512)
        ps, psr = self.next_ps()
        for m in range(KC):
            if precise:
                s_ = tm[m % 2]
                s_r = tm_r[m % 2]
                self.tt(s_, X[:, m, tb], X[:, m, tb], ALU.mult, reads=[Xr[b]], writes=[s_r])
                self.mm(ps[:, :], self.onesf, s_, m == 0, m == KC - 1, reads=[s_r, self.c_r], writes=[psr])
            else:
                self.tt(sq[m % 2], X[:, m, tb], X[:, m, tb], ALU.mult, reads=[Xr[b]], writes=[sq_r[m % 2]])
                self.mm(ps[:, :], self.onesb_s, sq[m % 2], m == 0, m == KC - 1, reads=[sq_r[m % 2], self.c_r], writes=[psr])
        self.act(lnv, ps[:, :], AF.Ln, reads=[psr, self.c_r], writes=[lnv_r], bias=self.eps_t[:, :])
        self.act(rstd, lnv, AF.Exp, reads=[lnv_r], writes=[rstd_r], scale=-0.5)
        self.tt(lnv, rstd, rstd, ALU.mult, reads=[rstd_r], writes=[lnv_r])
        self.stt(lnv, ps[:, :], EPS, lnv, ALU.add, ALU.mult, reads=[psr, lnv_r], writes=[lnv_r])
        self.ts(lnv, lnv, -0.5, 1.5, ALU.mult, ALU.add, reads=[lnv_r], writes=[lnv_r])
        self.tt(rstd, rstd, lnv, ALU.mult, reads=[rstd_r, lnv_r], writes=[rstd_r])
        for m in range(KC):
            self.tt(tm[m % 2], X[:, m, tb], rstd, ALU.mult, reads=[Xr[b], rstd_r], writes=[tm_r[m % 2]])
            if gs_fn is None:
                self.cp("act", out_fn(m), tm[m % 2], reads=[tm_r[m % 2]], writes=[out_r])
            else:
                self.act(out_fn(m), tm[m % 2], AF.Identity, reads=[tm_r[m % 2], self.mod_r], writes=[out_r],
                         bias=sh_fn(m), scale=gs_fn(m))

    def norm_tmps(self):
        ar = self.ar
        sq0, r0 = ar.alloc("sq0", [512], BF16)
        sq1, r1 = ar.alloc("sq1", [512], BF16)
        lnv, lr = ar.alloc("lnv", [512], F32)
        rstd, rr = ar.alloc("rstd", [512], F32)
        t0, tr0 = ar.alloc("tm0", [512], F32)
        t1, tr1 = ar.alloc("tm1", [512], F32)
        return ([sq0, sq1], [r0, r1], lnv, lr, rstd, rr, [t0, t1], [tr0, tr1])

    def norm1(self, g, l):
        ar = self.ar
        hT, hT_r = ar.alloc("hT", [KC, g.T], BF16)
        mk = ar.mark()
        tmps = self.norm_tmps()
        for b in range(g.NB):
            self.norm_block(b, lambda m: self.gs[:, l, 0, m, g.ci:g.ci + 1], lambda m: self.shift(l, 0, m, g.ci),
                            lambda m: hT[:, m, b * 512:(b + 1) * 512], hT_r, tmps)
        ar.release(mk)
        return hT, hT_r

    def resid(self, b, m, cols, ps_ap, psr, gate_ap, extra_reads=()):
        self.stt(self.X[:, m, cols], ps_ap, gate_ap, self.X[:, m, cols], ALU.mult, ALU.add,
                 reads=[psr, self.Xr[b], self.mod_r] + list(extra_reads), writes=[self.Xr[b]])

    def run_group(self, g):
        kb, ar = self.kb, self.ar
        ar.reset()
        src = self.xl_d if g.lat else self.xc_d
        for b in range(g.NB):
            kb.dma("sp", [(self.X[:, :, b * 512:(b + 1) * 512], src[:, :, b * 512:(b + 1) * 512])], "d_x", writes=[self.Xr[b]])
        for l in range(self.depth):
            ar.reset()
            self.mod_r = self.mod_rs[l]
            if g.lat and l == 0:
                wb = [ar.alloc(f"adaw{i}", [KC, 1024], BF16) for i in range(2)]
                self.adaln_finish(0, *self.adaln_issue(0, wb))
                ar.reset()
            kind = l % 4
            if not self.do_mixer:
                pass
            elif kind == 0:
                self.pool_mixer(g, l)
            elif kind == 1:
                self.mla(g, l)
            elif kind == 2:
                self.conv(g, l)
            else:
                self.gqa(g, l)
            ar.reset()
            if self.do_moe:
                self.moe(g, l)
        ar.reset()
        tmps = self.norm_tmps()
        yb = [ar.alloc(f"yb{i}", [KC, 512], F32) for i in range(2)]
        dst = self.yl_d if g.lat else self.yc_d
        for b in range(g.NB):
            y, y_r = yb[b % 2]
            self.norm_block(b, lambda m: self.gf[:, m:m + 1], lambda m: 0.0, lambda m: y[:, m, :], y_r, tmps)
            kb.dma("sp", [(dst[:, :, b * 512:(b + 1) * 512], y[:, :, :])], "d_y", reads=[y_r])

    def pool_mixer(self, g, l):
        kb, ar = self.kb, self.ar
        hT, hT_r = self.norm1(g, l)
        nseq, S = g.nseq, g.S
        W = S + 16
        pw, pw_r = ar.alloc("pw", [4, 2, 256], BF16)
        psc, psc_r = ar.alloc("psc", [KC], F32)
        prc, prc_r = ar.alloc("prc", [4, 16], F32)
        gsc, gsc_r = ar.alloc("gsc", [KC], F32)
        self.loadw(pw, self.pool_w_d.rearrange("g (k p) n -> p g k n", p=128), pw_r, "d_w0")
        self.load(psc, self.pool_s_d[:, :], psc_r)
        self.load(prc, self.pool_rc_d[:, :, :], prc_r)
        self.tt(gsc, self.mods[:, l, 16:24, g.ci], psc, ALU.mult, reads=[self.mod_r, psc_r], writes=[gsc_r])
        lv = [ar.alloc(f"lv{i}", [nseq, W], F32) for i in range(5)]
        pooled, pooled_r = ar.alloc("pooled", [2, nseq, S], BF16)
        tmpb, tmpb_r = ar.alloc("tmpb", [nseq, 16], F32)
        kb.op("dve", lambda e: e.memset(lv[0][0].rearrange("p a b -> p (a b)"), 0.0), writes=[lv[0][1]])
        for gi in range(4):
            win = 2 << gi
            for kk in range(2):
                m = 2 * gi + kk
                hv = hT[:, m, :].rearrange("p (s t) -> p s t", s=nseq)
                P0, P0r = lv[0]
                self.cp("act", P0[:, :, 8:8 + S], hv, reads=[hT_r], writes=[P0r])
                A, Ar = lv[1]
                self.tt(A[:, :, 1:W], P0[:, :, 1:W], P0[:, :, 0:W - 1], ALU.add, reads=[P0r], writes=[Ar])
                sh = 1
                lo, hi = 1, W
                for level in range(2, gi + 2):
                    Bv, Br = lv[level]
                    lo2, hi2 = lo + sh, hi - sh
                    self.tt(Bv[:, :, lo2:hi2], A[:, :, lo2 - sh:hi2 - sh], A[:, :, lo2 + sh:hi2 + sh], ALU.add,
                            reads=[Ar], writes=[Br])
                    A, Ar = Bv, Br
                    lo, hi = lo2, hi2
                    sh *= 2
                pv = pooled[:, kk, :, :]
                self.stt(pv, A[:, :, 8:8 + S], 1.0 / win, hv, ALU.mult, ALU.subtract, reads=[Ar, hT_r], writes=[pooled_r])
                for side, c0 in ((0, 0), (1, S - 8)):
                    self.tt(tmpb[:, :, 0:8], A[:, :, 8 + c0:16 + c0],
                            prc[:, gi, side * 8:side * 8 + 8].unsqueeze(1).broadcast_to([128, nseq, 8]), ALU.mult,
                            reads=[Ar, prc_r], writes=[tmpb_r])
                    self.tt(pv[:, :, c0:c0 + 8], tmpb[:, :, 0:8], hv[:, :, c0:c0 + 8], ALU.subtract,
                            reads=[tmpb_r, hT_r], writes=[pooled_r])
            pf = pooled.rearrange("p k s t -> p k (s t)")
            for b in range(g.NB):
                tb = slice(b * 512, (b + 1) * 512)
                for mo in range(2):
                    m = 2 * gi + mo
                    ps, psr = self.next_ps()
                    for k in range(2):
                        self.mm(ps[:, :], pw[:, gi, k, mo * 128:(mo + 1) * 128], pf[:, k, tb], k == 0, k == 1,
                                reads=[pw_r, pooled_r], writes=[psr])
                    self.resid(b, m, tb, ps[:, :], psr, gsc[:, m:m + 1], extra_reads=[gsc_r])

    def attn_jobs(self, jobs, scale, reads, ebuf, LA=2):
        E, E_r = ebuf
        nE = len(E)
        sps = [self.next_ps(pin=True) for _ in range(nE)]
        od = [(self.next_ps(pin=True), self.next_ps(pin=True)) for _ in range(2)]
        flat = [(j, i) for j, job in enumerate(jobs) for i in range(len(job[1]))]

        def issue_s(n):
            j, i = flat[n]
            NQ, chunks = jobs[j][0], jobs[j][1]
            xr = list(jobs[j][4]) if len(jobs[j]) > 4 else []
            ps, psr = sps[n % nE]
            kparts, _ = chunks[i]
            for pi, (kT, qT) in enumerate(kparts):
                self.mm(ps[:, 0:NQ], kT, qT, pi == 0, pi == len(kparts) - 1, reads=reads + xr, writes=[psr])

        for n in range(min(LA, len(flat))):
            issue_s(n)
        rd, rd_r = self.rden
        for n, (j, i) in enumerate(flat):
            if n + LA < len(flat):
                issue_s(n + LA)
            NQ, chunks, out_ap, out_r = jobs[j][0:4]
            nch = len(chunks)
            ps, psr = sps[n % nE]
            (ops, opr), (dps, dpr) = od[j % 2]
            self.act(E[n % nE][:, 0:NQ], ps[:, 0:NQ], AF.Exp, reads=[psr], writes=[E_r[n % nE]], scale=scale)
            _, v = chunks[i]
            self.mm(ops[:, 0:NQ], v, E[n % nE][:, 0:NQ], i == 0, i == nch - 1, reads=reads + [E_r[n % nE]], writes=[opr])
            self.mm(dps[:, 0:NQ], self.onesb, E[n % nE][:, 0:NQ], i == 0, i == nch - 1, reads=[E_r[n % nE], self.c_r], writes=[dpr])
            if i == nch - 1:
                self.kb.op("dve", lambda e: e.reciprocal(out=rd[:, 0:NQ], in_=dps[:, 0:NQ]), reads=[dpr], writes=[rd_r])
                self.tt(out_ap, ops[:, 0:NQ], rd[:, 0:NQ], ALU.mult, reads=[opr, rd_r], writes=[out_r])
        for (_, r) in sps:
            self.unpin(r)
        for (a, b) in od:
            self.unpin(a[1])
            self.unpin(b[1])

    def mla(self, g, l):
        kb, ar = self.kb, self.ar
        T, NT = g.T, g.NT
        hT, hT_r = self.norm1(g, l)
        NKC = 512 if g.lat else 0
        NK = NKC + T
        cqnT, cqnT_r = ar.alloc("cqnT", [3, T], BF16)
        ckvT, ckvT_r = ar.alloc("ckvT", [2, NK], BF16)
        krT, krT_r = ar.alloc("krT", [NK], BF16)
        kb.op("dve", lambda e: e.memset(krT, 0.0), writes=[krT_r])
        wuq, wuq_r = ar.alloc("wuq", [3, 2560], BF16)
        wukv, wukv_r = ar.alloc("wukv", [2, 2048], BF16)
        self.loadw(wuq, self.mla_wuq_d.rearrange("(k p) n -> p k n", p=128), wuq_r, "d_w1")
        self.loadw(wukv, self.mla_wukv_d.rearrange("(k p) n -> p k n", p=128), wukv_r, "d_w1")
        mkA = ar.mark()
        wd, wd_r = ar.alloc("wd", [KC, 704], BF16)
        gq, gq_r = ar.alloc("gq", [384], F32)
        gkv, gkv_r = ar.alloc("gkv", [256], F32)
        self.loadw(wd, self.mla_wd_d.rearrange("(k p) n -> p k n", p=128), wd_r, "d_w0")
        self.load(gq, self.mla_gq_d[:, :], gq_r)
        self.load(gkv, self.mla_gkv_d[:, :], gkv_r)
        _jk = [ar.alloc(f"junk{i}", [384], F32) for i in range(2)]
        _st = [ar.alloc(f"st{i}", [8], F32) for i in range(2)]
        cqn = [ar.alloc(f"cqn{i}", [384], BF16) for i in range(2)]
        ckn = [ar.alloc(f"ckn{i}", [256], F32) for i in range(2)]
        cknb = [ar.alloc(f"cknb{i}", [384], BF16) for i in range(2)]
        for i in range(2):
            kb.op("dve", lambda e: e.memset(cknb[i][0], 0.0), writes=[cknb[i][1]])
        krf = [ar.alloc(f"krf{i}", [64], F32) for i in range(2)]
        if self.stop == "A00":
            return
        if g.lat:
            rt, rt_r = ar.alloc("rt", [2, 16, 32], F32)
            self.load(rt, self.mla_rtok_d[:, :, :, :], rt_r)
            _rt2 = [ar.alloc(f"rtmp{i}", [4, 32], F32) for i in range(2)]
            cc, cc_r = ar.alloc("cc", [4, 384], BF16)
            kb.op("dve", lambda e: e.memset(cc.rearrange("p a b -> p (a b)"), 0.0), writes=[cc_r])
            self.loadw(cc[:, :, 0:256], self.mla_cckv_d.rearrange("(j p) c -> p j c", p=128), cc_r, "d_w2")
            self.loadw(cc[:, :, 256:320], self.mla_ckr_d.rearrange("(j p) c -> p j c", p=128), cc_r, "d_w2")
            if self.stop == "A01":
                return
            for j in range(4):
                ps, psr = self.next_ps()
                pb = ps[:, :].bitcast(BF16)
                for k in range(2):
                    self.tr(pb[:, k * 128:(k + 1) * 128], cc[:, j, k * 128:(k + 1) * 128], self.identb, reads=[cc_r, self.c_r], writes=[psr])
                self.tr(pb[:, 256:384], cc[:, j, 256:384], self.identb, reads=[cc_r, self.c_r], writes=[psr])
                self.cp("act", ckvT[:, :, j * 128:(j + 1) * 128], pb[:, 0:256].rearrange("p (k t) -> p k t", k=2), reads=[psr], writes=[ckvT_r])
                self.cp("act", krT[0:64, j * 128:(j + 1) * 128], pb[0:64, 256:384], reads=[psr], writes=[krT_r])
        if self.stop == "A0":
            return
        def tile_gen(tt_):
            tsl = slice(tt_ * 128, (tt_ + 1) * 128)
            junk, junk_r = _jk[tt_ % 2]
            st, st_r = _st[tt_ % 2]
            if g.lat:
                rtmp, rtmp_r = _rt2[tt_ % 2]
            p1, p1r = self.next_ps()
            p2, p2r = self.next_ps()
            for k in range(KC):
                self.mm(p1[:, 0:384], hT[:, k, tsl], wd[:, k, 0:384], k == 0, k == KC - 1, reads=[hT_r, wd_r], writes=[p1r])
            for k in range(KC):
                self.mm(p2[:, 0:320], hT[:, k, tsl], wd[:, k, 384:704], k == 0, k == KC - 1, reads=[hT_r, wd_r], writes=[p2r])
            self.act(junk[:, 0:384], p1[:, 0:384], AF.Square, reads=[p1r], writes=[junk_r, st_r], accum_out=st[:, 0:1])
            self.act(junk[:, 0:256], p2[:, 0:256], AF.Square, reads=[p2r], writes=[junk_r, st_r], accum_out=st[:, 1:2])
            self.act(st[:, 2:3], st[:, 0:1], AF.Ln, reads=[st_r, self.c_r], writes=[st_r], bias=self.eps_t[:, :], scale=1.0 / 384)
            self.act(st[:, 3:4], st[:, 1:2], AF.Ln, reads=[st_r, self.c_r], writes=[st_r], bias=self.eps_t[:, :], scale=1.0 / 256)
            self.act(st[:, 4:6], st[:, 2:4], AF.Exp, reads=[st_r], writes=[st_r], scale=-0.5)
            if self.stop == "A1":
                return
            cq, cq_r = cqn[tt_ % 2]
            ck, ck_r = ckn[tt_ % 2]
            ckb, ckb_r = cknb[tt_ % 2]
            kf, kf_r = krf[tt_ % 2]
            self.stt(cq, p1[:, 0:384], st[:, 4:5], gq, ALU.mult, ALU.mult, reads=[p1r, st_r, gq_r], writes=[cq_r])
            self.stt(ck, p2[:, 0:256], st[:, 5:6], gkv, ALU.mult, ALU.mult, reads=[p2r, st_r, gkv_r], writes=[ck_r])
            self.cp("act", ckb[:, 0:256], ck, reads=[ck_r], writes=[ckb_r])
            self.cp("act", kf, p2[:, 256:320], reads=[p2r], writes=[kf_r])
            if g.lat:
                c_, s_ = rt[:, 0, tt_, :], rt[:, 1, tt_, :]
                x1, x2 = kf[:, 0:32], kf[:, 32:64]
                self.tt(rtmp[:, 0, :], x1, c_, ALU.mult, reads=[kf_r, rt_r], writes=[rtmp_r])
                self.tt(rtmp[:, 1, :], x2, s_, ALU.mult, reads=[kf_r, rt_r], writes=[rtmp_r])
                self.tt(rtmp[:, 2, :], x1, s_, ALU.mult, reads=[kf_r, rt_r], writes=[rtmp_r])
                self.tt(rtmp[:, 3, :], x2, c_, ALU.mult, reads=[kf_r, rt_r], writes=[rtmp_r])
                self.tt(ckb[:, 256:288], rtmp[:, 0, :], rtmp[:, 1, :], ALU.subtract, reads=[rtmp_r], writes=[ckb_r])
                self.tt(ckb[:, 288:320], rtmp[:, 2, :], rtmp[:, 3, :], ALU.add, reads=[rtmp_r], writes=[ckb_r])
            else:
                self.cp("dve", ckb[:, 256:320], kf, reads=[kf_r], writes=[ckb_r])
                kb.dma("sp", [(self.st_ckv_d[tsl, :], ck)], "d_st", reads=[ck_r])
                kb.dma("sp", [(self.st_kr_d[tsl, :], kf)], "d_st", reads=[kf_r])
            yield
            if self.stop == "A2":
                return
            ps, psr = self.next_ps()
            pb = ps[:, :].bitcast(BF16)
            for k in range(3):
                self.tr(pb[:, k * 128:(k + 1) * 128], cq[:, k * 128:(k + 1) * 128], self.identb, reads=[cq_r, self.c_r], writes=[psr])
            for k in range(2):
                self.tr(pb[:, (3 + k) * 128:(4 + k) * 128], ckb[:, k * 128:(k + 1) * 128], self.identb, reads=[ckb_r, self.c_r], writes=[psr])
            self.tr(pb[:, 640:768], ckb[:, 256:384], self.identb, reads=[ckb_r, self.c_r], writes=[psr])
            ksl = slice(NKC + tt_ * 128, NKC + (tt_ + 1) * 128)
            self.cp("act", cqnT[:, :, tsl], pb[:, 0:384].rearrange("p (k t) -> p k t", k=3), reads=[psr], writes=[cqnT_r])
            self.cp("act", ckvT[:, :, ksl], pb[:, 384:640].rearrange("p (k t) -> p k t", k=2), reads=[psr], writes=[ckvT_r])
            self.cp("act", krT[0:64, ksl], pb[0:64, 640:768], reads=[psr], writes=[krT_r])
        _cur = tile_gen(0)
        next(_cur)
        for tt_ in range(NT):
            _nxt = None
            if tt_ + 1 < NT:
                _nxt = tile_gen(tt_ + 1)
                next(_nxt)
            for _ in _cur:
                pass
            _cur = _nxt
        if self.stop == "A":
            return
        ar.release(mkA)
        attnT, attnT_r = hT, hT_r
        wo, wo_r = ar.alloc("wo", [KC, D], BF16)
        self.loadw(wo, self.mla_wo_d.rearrange("(k p) n -> p k n", p=128), wo_r, "d_w0")
        knT, knT_r = ar.alloc("knT", [NK], BF16)
        vh, vh_r = ar.alloc("vh", [NK // 128, 128], BF16)
        qn, qn_r = ar.alloc("qn", [T], BF16)
        qr, qr_r = ar.alloc("qr", [T], BF16)
        _eb = [ar.alloc(f"E{i}", [512], BF16) for i in range(3)]
        EB, EBr = [x[0] for x in _eb], [x[1] for x in _eb]
        self.rden = ar.alloc("rden", [512], F32)
        if g.lat:
            qrot, qrot_r = ar.alloc("qrot", [T], BF16)
            kb.op("dve", lambda e: e.memset(qrot, 0.0), writes=[qrot_r])
            rA, rA_r = ar.alloc("rA", [2, 32], F32)
            rB, rB_r = ar.alloc("rB", [2, 64], F32)
            self.load(rA[0:64], self.mla_rA_d[:, :, :], rA_r)
            self.load(rB[0:64], self.mla_rB_d[:, :, :], rB_r)
            t1, t1_r = ar.alloc("t1", [512], F32)
            t2, t2_r = ar.alloc("t2", [512], F32)
        scale = 192 ** -0.5
        qres = [[Res(f"qn{b}"), Res(f"qr{b}"), Res(f"qo{b}")] for b in range(g.NB)]
        for h in range(8):
            c0 = h * 256
            q0 = h * 320
            for kb0 in range(0, NK, 512):
                ps, psr = self.next_ps()
                for k in range(2):
                    self.mm(ps[:, :], wukv[:, k, c0:c0 + 128], ckvT[:, k, kb0:kb0 + 512], k == 0, k == 1, reads=[wukv_r, ckvT_r], writes=[psr])
                self.cp("act", knT[:, kb0:kb0 + 512], ps[:, :], reads=[psr], writes=[knT_r])
            for kc0 in range(0, NK // 128, 4):
                ps, psr = self.next_ps()
                for kk in range(4):
                    kc = kc0 + kk
                    for k in range(2):
                        self.mm(ps[:, kk * 128:(kk + 1) * 128], ckvT[:, k, kc * 128:(kc + 1) * 128], wukv[:, k, c0 + 128:c0 + 256],
                                k == 0, k == 1, reads=[wukv_r, ckvT_r], writes=[psr])
                self.cp("dve", vh[:, kc0:kc0 + 4, :], ps[:, :].rearrange("p (a b) -> p a b", a=4), reads=[psr], writes=[vh_r])
            if self.stop == "B0a":
                continue
            for b in range(g.NB):
                tb = slice(b * 512, (b + 1) * 512)
                ps, psr = self.next_ps()
                for k in range(3):
                    self.mm(ps[:, :], wuq[:, k, q0:q0 + 128], cqnT[:, k, tb], k == 0, k == 2, reads=[wuq_r, cqnT_r], writes=[psr])
                self.cp("act", qn[:, tb], ps[:, :], reads=[psr], writes=[qres[b][0]])
                ps, psr = self.next_ps()
                for k in range(3):
                    self.mm(ps[:, :], wuq[:, k, q0 + 128:q0 + 256], cqnT[:, k, tb], k == 0, k == 2, reads=[wuq_r, cqnT_r], writes=[psr])
                self.cp("act", qr[:, tb], ps[:, :], reads=[psr], writes=[qres[b][1]])
                if g.lat and self.stop != "B0b":
                    ps2, ps2r = self.next_ps()
                    for k in range(3):
                        self.mm(ps2[:, :], wuq[:, k, q0 + 192:q0 + 320], cqnT[:, k, tb], k == 0, k == 2, reads=[wuq_r, cqnT_r], writes=[ps2r])
                    r0 = b * 8
                    for rr in range(8):
                        sg_ = slice(rr * 64, (rr + 1) * 64)
                        self.stt(t1[0:64, sg_], ps[0:64, sg_], rA[0:64, 0, r0 + rr:r0 + rr + 1], rB[0:64, 0, :], ALU.mult, ALU.mult,
                                 reads=[psr, rA_r, rB_r], writes=[t1_r])
                        self.stt(t2[0:64, sg_], ps2[0:64, sg_], rA[0:64, 1, r0 + rr:r0 + rr + 1], rB[0:64, 1, :], ALU.mult, ALU.mult,
                                 reads=[ps2r, rA_r, rB_r], writes=[t2_r])
                    self.tt(qrot[0:64, tb], t1[0:64, :], t2[0:64, :], ALU.add, reads=[t1_r, t2_r, qrot_r], writes=[qres[b][2]])
            if self.stop in ("B0", "B0b"):
                continue
            rds = [knT_r, vh_r, krT_r]
            jobs = []
            if g.lat:
                for b in range(4):
                    qs = slice(b * 512, (b + 1) * 512)
                    chunks = []
                    for kc in range(NK // 128):
                        ks = slice(kc * 128, (kc + 1) * 128)
                        qrp = qr if kc < 4 else qrot
                        chunks.append(([(knT[:, ks], qn[:, qs]), (krT[:, ks], qrp[:, qs])], vh[:, kc, :]))
                    jobs.append((512, chunks, attnT[:, h, qs], attnT_r, qres[b]))
            else:
                for s_ in range(4):
                    qs = slice(s_ * 256, (s_ + 1) * 256)
                    chunks = []
                    for kc in range(2 * s_, 2 * s_ + 2):
                        ks = slice(kc * 128, (kc + 1) * 128)
                        chunks.append(([(knT[:, ks], qn[:, qs]), (krT[:, ks], qr[:, qs])], vh[:, kc, :]))
                    jobs.append((256, chunks, attnT[:, h, qs], attnT_r, qres[s_ // 2][0:2]))
            self.attn_jobs(jobs, scale, rds, (EB, EBr))
        if self.stop in ("B0", "B", "B0a", "B0b"):
            return
        for b in range(g.NB):
            tb = slice(b * 512, (b + 1) * 512)
            for m in range(KC):
                ps, psr = self.next_ps()
                for h in range(8):
                    self.mm(ps[:, :], wo[:, h, m * 128:(m + 1) * 128], attnT[:, h, tb], h == 0, h == 7, reads=[wo_r, attnT_r], writes=[psr])
                self.resid(b, m, tb, ps[:, :], psr, self.gate(l, 0, m, g.ci))

    def conv(self, g, l):
        kb, ar = self.kb, self.ar
        T, nseq, S = g.T, g.nseq, g.S
        W = S + 30
        glu, glu_r = ar.alloc("glu", [KC, nseq, W], BF16, top=True)
        vec, vec_r = ar.alloc("cvvec", [4, KC], F32)
        b1, b1_r = ar.alloc("cvb1", [16], F32)
        wdw, wdw_r = ar.alloc("wdw", [KC, 31], F32)
        self.load(vec, self.cv_vec_d[:, :, :], vec_r)
        self.load(b1, self.cv_b1_d[:, :], b1_r)
        self.load(wdw, self.cv_wdw_d[:, :, :], wdw_r)
        mk0 = ar.mark()
        hT, hT_r = self.norm1(g, l)
        w1, w1_r = ar.alloc("w1", [KC, 2048], BF16)
        self.loadw(w1[:, :, 0:1024], self.cv_w1_d[:, 0:1024].rearrange("(k p) n -> p k n", p=128), w1_r, "d_w0")
        self.loadw(w1[:, :, 1024:2048], self.cv_w1_d[:, 1024:2048].rearrange("(k p) n -> p k n", p=128), w1_r, "d_w0")
        sig, sig_r = ar.alloc("sig", [512], F32)
        kb.op("dve", lambda e: e.memset(glu.rearrange("p a b c -> p (a b c)"), 0.0), writes=[glu_r])
        NQ = 512
        spb = NQ // S if S < NQ else 1
        for b in range(g.NB):
            tb = slice(b * 512, (b + 1) * 512)
            for m in range(KC):
                pa, par = self.next_ps()
                pg, pgr = self.next_ps()
                for k in range(KC):
                    self.mm(pa[:, :], w1[:, k, m * 128:(m + 1) * 128], hT[:, k, tb], k == 0, k == KC - 1, reads=[w1_r, hT_r], writes=[par])
                for k in range(KC):
                    self.mm(pg[:, :], w1[:, k, 1024 + m * 128:1024 + (m + 1) * 128], hT[:, k, tb], k == 0, k == KC - 1, reads=[w1_r, hT_r], writes=[pgr])
                self.act(sig, pg[:, :], AF.Sigmoid, reads=[pgr, b1_r], writes=[sig_r], bias=b1[:, 8 + m:9 + m])
                if g.lat:
                    dst = glu[:, m, 0, 15 + b * 512:15 + (b + 1) * 512]
                    self.stt(dst, pa[:, :], b1[:, m:m + 1], sig, ALU.add, ALU.mult, reads=[par, b1_r, sig_r], writes=[glu_r])
                else:
                    dst = glu[:, m, 2 * b:2 * b + 2, 15:15 + S]
                    self.stt(dst, pa[:, :].rearrange("p (s t) -> p s t", s=2), b1[:, m:m + 1],
                             sig.rearrange("p (s t) -> p s t", s=2), ALU.add, ALU.mult, reads=[par, b1_r, sig_r], writes=[glu_r])
        ar.release(mk0)
        cv, cv_r = ar.alloc("cv", [KC, T], F32)
        mkd = ar.mark()
        dg = [ar.alloc(f"dg{i}", [31, 128], BF16) for i in range(2)]
        for m in range(KC):
            dgt, dg_r = dg[m % 2]
            for j in range(31):
                self.ts(dgt[:, j, :], self.identb, wdw[:, m, j:j + 1], None, ALU.mult, None, reads=[wdw_r, self.c_r], writes=[dg_r])
            for b in range(g.NB):
                ps, psr = self.next_ps()
                for j in range(31):
                    if g.lat:
                        rhs = glu[:, m, 0, b * 512 + j:b * 512 + j + 512]
                        out = ps[:, :]
                    else:
                        rhs = glu[:, m, 2 * b:2 * b + 2, j:j + S]
                        out = ps[:, :].rearrange("p (s t) -> p s t", s=2)
                    self.mm(out, dgt[:, j, :], rhs, j == 0, j == 30, reads=[dg_r, glu_r], writes=[psr])
                self.act(cv[:, m, b * 512:(b + 1) * 512], ps[:, :], AF.Identity, reads=[psr, vec_r], writes=[cv_r], bias=vec[:, 0, m:m + 1])
        ar.release(mkd)
        ar.release_top()
        w2, w2_r = ar.alloc("w2", [KC, D], BF16)
        self.loadw(w2, self.cv_w2_d.rearrange("(k p) n -> p k n", p=128), w2_r, "d_w1")
        sqf = [ar.alloc(f"sqf{i}", [512], F32) for i in range(2)]
        mean, mean_r = ar.alloc("mean", [512], F32)
        var, var_r = ar.alloc("var", [512], F32)
        lnv, lnv_r = ar.alloc("lnv", [512], F32)
        rstd, rstd_r = ar.alloc("rstd", [512], F32)
        tmf = [ar.alloc(f"tmf{i}", [512], F32) for i in range(2)]
        sb, sb_r = ar.alloc("sb", [KC, 512], BF16)
        tm2, tm2_r = ar.alloc("tm2", [512], F32)
        for b in range(g.NB):
            tb = slice(b * 512, (b + 1) * 512)
            pm, pmr = self.next_ps()
            pq, pqr = self.next_ps()
            for m in range(KC):
                s_, s_r = sqf[m % 2]
                self.act(s_, cv[:, m, tb], AF.Square, reads=[cv_r], writes=[s_r])
                self.mm(pm[:, :], self.onesf, cv[:, m, tb], m == 0, m == KC - 1, reads=[cv_r, self.c_r], writes=[pmr])
                self.mm(pq[:, :], self.onesf, s_, m == 0, m == KC - 1, reads=[s_r, self.c_r], writes=[pqr])
            self.cp("act", mean, pm[:, :], reads=[pmr], writes=[mean_r])
            self.tt(var, mean, mean, ALU.mult, reads=[mean_r], writes=[var_r])
            self.tt(var, pq[:, :], var, ALU.subtract, reads=[pqr, var_r], writes=[var_r])
            self.act(lnv, var, AF.Ln, reads=[var_r, self.c_r], writes=[lnv_r], bias=self.eps_t[:, :])
            self.act(rstd, lnv, AF.Exp, reads=[lnv_r], writes=[rstd_r], scale=-0.5)
            for m in range(KC):
                t_, t_r = tmf[m % 2]
                self.tt(t_, cv[:, m, tb], mean, ALU.subtract, reads=[cv_r, mean_r], writes=[t_r])
                self.tt(t_, t_, rstd, ALU.mult, reads=[t_r, rstd_r], writes=[t_r])
                self.act(sb[:, m, :], t_, AF.Silu, reads=[t_r, vec_r], writes=[sb_r], bias=vec[:, 2, m:m + 1], scale=vec[:, 1, m:m + 1])
            for m in range(KC):
                ps, psr = self.next_ps()
                for k in range(KC):
                    self.mm(ps[:, :], w2[:, k, m * 128:(m + 1) * 128], sb[:, k, :], k == 0, k == KC - 1, reads=[w2_r, sb_r], writes=[psr])
                self.ts(tm2, ps[:, :], vec[:, 3, m:m + 1], self.gate(l, 0, m, g.ci), ALU.add, ALU.mult,
                        reads=[psr, vec_r, self.mod_r], writes=[tm2_r])
                self.tt(self.X[:, m, tb], self.X[:, m, tb], tm2, ALU.add, reads=[tm2_r, self.Xr[b]], writes=[self.Xr[b]])

    def gqa(self, g, l):
        kb, ar = self.kb, self.ar
        T, NT = g.T, g.NT
        hT, hT_r = self.norm1(g, l)
        NKC = 512 if g.lat else 0
        NK = NKC + T
        gg, gg_r = ar.alloc("gg", [2, 128], F32)
        self.load(gg, self.gq_g_d[:, :, :], gg_r)
        if g.lat:
            rtb = [ar.alloc(f"grt{i}", [2, 64], F32) for i in range(2)]

            def load_rt(t):
                kb.dma("sp", [(rtb[t % 2][0], self.gq_rtok_d[:, :, t, :])], f"d_rt{t % 2}", writes=[rtb[t % 2][1]])
        attnT, attnT_r = ar.alloc("attnT", [4, T], BF16)
        QrT, QrT_r = ar.alloc("QrT", [4, T], BF16)
        if g.lat:
            QuT, QuT_r = ar.alloc("QuT", [4, T], BF16)
        KT, KT_r = ar.alloc("KT", [NK], BF16)
        Vt, Vt_r = ar.alloc("Vt", [NK // 128, 128], BF16)
        wq, wq_r = ar.alloc("wq", [KC, 768], BF16)
        wo = wq.rearrange("p a b -> p (a b)")[:, 0:4 * D].rearrange("p (a b) -> p a b", a=4)
        wo_r = wq_r
        _gsq = [ar.alloc(f"gsq{i}", [6, 128], F32) for i in range(2)]
        _gst = [ar.alloc(f"gst{i}", [24], F32) for i in range(2)]
        qf = [ar.alloc(f"qf{i}", [6, 128], F32) for i in range(2)]
        qb = [ar.alloc(f"qb{i}", [11, 128], BF16) for i in range(2)]
        _grt = [ar.alloc(f"grtmp{i}", [2, 5, 64], F32) for i in range(2)]
        _eb = [ar.alloc(f"E{i}", [512], BF16) for i in range(3)]
        EB, EBr = [x[0] for x in _eb], [x[1] for x in _eb]
        self.rden = ar.alloc("rden", [512], F32)
        if g.lat:
            cc, cc_r = ar.alloc("gcc", [4, 2, 128], BF16)
        scale = 128 ** -0.5
        for kvh in range(2):
            wsrc = self.gq_w_d.rearrange("(k p) n -> p k n", p=128)
            self.loadw(wq[:, :, 0:512], wsrc[:, :, kvh * 512:(kvh + 1) * 512], wq_r, "d_w0")
            self.loadw(wq[:, :, 512:640], wsrc[:, :, 1024 + kvh * 128:1024 + (kvh + 1) * 128], wq_r, "d_w0")
            self.loadw(wq[:, :, 640:768], wsrc[:, :, 1280 + kvh * 128:1280 + (kvh + 1) * 128], wq_r, "d_w0")
            if g.lat:
                self.loadw(cc[:, :, 0, :], self.gq_ck_d[:, kvh * 128:(kvh + 1) * 128].rearrange("(j p) c -> p j c", p=128), cc_r, "d_w2")
                self.loadw(cc[:, :, 1, :], self.gq_cv_d[:, kvh * 128:(kvh + 1) * 128].rearrange("(j p) c -> p j c", p=128), cc_r, "d_w2")
                ps, psr = self.next_ps()
                pb = ps[:, :].bitcast(BF16)
                for j in range(4):
                    self.tr(pb[:, j * 128:(j + 1) * 128], cc[:, j, 0, :], self.identb, reads=[cc_r, self.c_r], writes=[psr])
                self.cp("act", KT[:, 0:512], pb[:, 0:512], reads=[psr], writes=[KT_r])
                self.cp("dve", Vt[:, 0:4, :], cc[:, :, 1, :], reads=[cc_r], writes=[Vt_r])
            if g.lat:
                load_rt(0)
            def tile_gen(tt_):
                tsl = slice(tt_ * 128, (tt_ + 1) * 128)
                if g.lat:
                    if tt_ + 1 < NT:
                        load_rt(tt_ + 1)
                    rt, rt_r = rtb[tt_ % 2]
                st, st_r = _gst[tt_ % 2]
                sq, sq_r = _gsq[tt_ % 2]
                rtmp, rtmp_r = _grt[tt_ % 2]
                p1, p1r = self.next_ps()
                p2, p2r = self.next_ps()
                for k in range(KC):
                    self.mm(p1[:, :], hT[:, k, tsl], wq[:, k, 0:512], k == 0, k == KC - 1, reads=[hT_r, wq_r], writes=[p1r])
                for k in range(KC):
                    self.mm(p2[:, 0:256], hT[:, k, tsl], wq[:, k, 512:768], k == 0, k == KC - 1, reads=[hT_r, wq_r], writes=[p2r])
                self.act(sq[:, 0:4, :], p1[:, :].rearrange("p (h d) -> p h d", h=4), AF.Square, reads=[p1r], writes=[sq_r])
                self.act(sq[:, 4, :], p2[:, 0:128], AF.Square, reads=[p2r], writes=[sq_r])
                kb.op("dve", lambda e: e.tensor_reduce(out=st[:, 0:5], in_=sq[:, 0:5, :], axis=AX.X, op=ALU.add), reads=[sq_r], writes=[st_r])
                self.act(st[:, 8:13], st[:, 0:5], AF.Ln, reads=[st_r, self.c_r], writes=[st_r], bias=self.eps_t[:, :], scale=1.0 / 128)
                self.act(st[:, 16:21], st[:, 8:13], AF.Exp, reads=[st_r], writes=[st_r], scale=-0.5)
                q_, q_r = qf[tt_ % 2]
                o_, o_r = qb[tt_ % 2]
                self.tt(q_[:, 0:4, :], p1[:, :].rearrange("p (h d) -> p h d", h=4), st[:, 16:20].unsqueeze(2).broadcast_to([128, 4, 128]),
                        ALU.mult, reads=[p1r, st_r], writes=[q_r])
                self.tt(q_[:, 0:4, :], q_[:, 0:4, :], gg[:, 0, :].unsqueeze(1).broadcast_to([128, 4, 128]), ALU.mult, reads=[q_r, gg_r], writes=[q_r])
                self.stt(q_[:, 4, :], p2[:, 0:128], st[:, 20:21], gg[:, 1, :], ALU.mult, ALU.mult, reads=[p2r, st_r, gg_r], writes=[q_r])
                self.cp("act", q_[:, 5, :], p2[:, 128:256], reads=[p2r], writes=[q_r])
                self.cp("act", o_[:, 9, :], p2[:, 128:256], reads=[p2r], writes=[o_r])
                if g.lat:
                    c_ = rt[:, 0, :].unsqueeze(1).broadcast_to([128, 5, 64])
                    s_ = rt[:, 1, :].unsqueeze(1).broadcast_to([128, 5, 64])
                    x1, x2 = q_[:, 0:5, 0:64], q_[:, 0:5, 64:128]
                    self.tt(rtmp[:, 0, :, :], x1, c_, ALU.mult, reads=[q_r, rt_r], writes=[rtmp_r])
                    self.tt(rtmp[:, 1, :, :], x2, s_, ALU.mult, reads=[q_r, rt_r], writes=[rtmp_r])
                    self.tt(o_[:, 0:5, 0:64], rtmp[:, 0, :, :], rtmp[:, 1, :, :], ALU.subtract, reads=[rtmp_r], writes=[o_r])
                    self.tt(rtmp[:, 0, :, :], x1, s_, ALU.mult, reads=[q_r, rt_r], writes=[rtmp_r])
                    self.tt(rtmp[:, 1, :, :], x2, c_, ALU.mult, reads=[q_r, rt_r], writes=[rtmp_r])
                    self.tt(o_[:, 0:5, 64:128], rtmp[:, 0, :, :], rtmp[:, 1, :, :], ALU.add, reads=[rtmp_r], writes=[o_r])
                    self.cp("act", o_[:, 5:9, :], q_[:, 0:4, :], reads=[q_r], writes=[o_r])
                    ntr = 9
                else:
                    self.cp("act", o_[:, 0:5, :], q_[:, 0:5, :], reads=[q_r], writes=[o_r])
                    ntr = 5
                    kb.dma("sp", [(self.st_k_d[tsl, kvh * 128:(kvh + 1) * 128], q_[:, 4, :])], "d_st", reads=[q_r])
                    kb.dma("sp", [(self.st_v_d[tsl, kvh * 128:(kvh + 1) * 128], q_[:, 5, :])], "d_st", reads=[q_r])
                yield
                pa, par = self.next_ps()
                pba = pa[:, :].bitcast(BF16)
                for i in range(5):
                    self.tr(pba[:, i * 128:(i + 1) * 128], o_[:, i, :], self.identb, reads=[o_r, self.c_r], writes=[par])
                ksl = slice(NKC + tt_ * 128, NKC + (tt_ + 1) * 128)
                self.cp("act", QrT[:, :, tsl], pba[:, 0:512].rearrange("p (h t) -> p h t", h=4), reads=[par], writes=[QrT_r])
                self.cp("act", KT[:, ksl], pba[:, 512:640], reads=[par], writes=[KT_r])
                self.cp("dve", Vt[:, NKC // 128 + tt_, :], o_[:, 9, :], reads=[o_r], writes=[Vt_r])
                if g.lat:
                    pc, pcr = self.next_ps()
                    pbc = pc[:, :].bitcast(BF16)
                    for i in range(4):
                        self.tr(pbc[:, i * 128:(i + 1) * 128], o_[:, 5 + i, :], self.identb, reads=[o_r, self.c_r], writes=[pcr])
                    self.cp("act", QuT[:, :, tsl], pbc[:, 0:512].rearrange("p (h t) -> p h t", h=4), reads=[pcr], writes=[QuT_r])
            _cur = tile_gen(0)
            next(_cur)
            for tt_ in range(NT):
                _nxt = None
                if tt_ + 1 < NT:
                    _nxt = tile_gen(tt_ + 1)
                    next(_nxt)
                for _ in _cur:
                    pass
                _cur = _nxt
            rds = [KT_r, Vt_r, QrT_r] + ([QuT_r] if g.lat else [])
            for hh in range(4):
                jobs = []
                if g.lat:
                    for b in range(4):
                        qs = slice(b * 512, (b + 1) * 512)
                        chunks = []
                        for kc in range(NK // 128):
                            ks = slice(kc * 128, (kc + 1) * 128)
                            qsrc = QuT if kc < 4 else QrT
                            chunks.append(([(KT[:, ks], qsrc[:, hh, qs])], Vt[:, kc, :]))
                        jobs.append((512, chunks, attnT[:, hh, qs], attnT_r))
                else:
                    for s_ in range(4):
                        qs = slice(s_ * 256, (s_ + 1) * 256)
                        chunks = []
                        for kc in range(2 * s_, 2 * s_ + 2):
                            ks = slice(kc * 128, (kc + 1) * 128)
                            chunks.append(([(KT[:, ks], QrT[:, hh, qs])], Vt[:, kc, :]))
                        jobs.append((256, chunks, attnT[:, hh, qs], attnT_r))
                self.attn_jobs(jobs, scale, rds, (EB, EBr))
            self.loadw(wo, self.gq_wo_d[kvh * 512:(kvh + 1) * 512, :].rearrange("(k p) n -> p k n", p=128), wo_r, "d_w0")
            for b in range(g.NB):
                tb = slice(b * 512, (b + 1) * 512)
                for m in range(KC):
                    ps, psr = self.next_ps()
                    for hh in range(4):
                        self.mm(ps[:, :], wo[:, hh, m * 128:(m + 1) * 128], attnT[:, hh, tb], hh == 0, hh == 3, reads=[wo_r, attnT_r], writes=[psr])
                    self.resid(b, m, tb, ps[:, :], psr, self.gate(l, 0, m, g.ci))

    def moe(self, g, l):
        kb, ar = self.kb, self.ar
        T, NT, NB, C, NS, NCC = g.T, g.NT, g.NB, g.C, g.NSLOT, g.NCC
        ci = g.ci
        h2tok, h2tok_r = ar.alloc("h2tok", [NT, D], BF16)
        aff, aff_r = ar.alloc("aff", [NT, NE], F32)
        affhl, affhl_r = ar.alloc("affhl", [NT, NE, 2], BF16)
        posg, posg_r = ar.alloc("posg", [NT, NE], F32)
        mk0 = ar.mark()
        affT, affT_r = ar.alloc("affT", [T], F32)
        mk1 = ar.mark()
        rw, rw_r = ar.alloc("rw", [KC, NE], F32)
        self.load(rw, self.rw_d[:, l, :, :], rw_r)
        tmps = self.norm_tmps()
        h2f = [ar.alloc(f"h2f{i}", [KC, 512], F32) for i in range(2)]
        sm, sm_r = ar.alloc("sm", [16], F32)
        ex, ex_r = ar.alloc("ex", [4, NE], F32)
        def norm_b(b):
            hf, hf_r = h2f[b % 2]
            self.norm_block(b, lambda m: self.gs[:, l, 1, m, ci:ci + 1], lambda m: self.shift(l, 1, m, ci),
                            lambda m: hf[:, m, :], hf_r, tmps, precise=False)

        def route_b(b):
            hf, hf_r = h2f[b % 2]
            pT, pTr = self.next_ps(pin=True)
            pl, plr = self.next_ps(pin=True)
            t4 = slice(b * 4, b * 4 + 4)
            for q in range(4):
                qs = slice(q * 128, (q + 1) * 128)
                for k in range(KC):
                    self.mm(pl[:, q * NE:(q + 1) * NE], hf[:, k, qs], rw[:, k, :], k == 0, k == KC - 1, reads=[hf_r, rw_r], writes=[plr])
            plv = pl[:, 0:4 * NE].rearrange("p (q e) -> p q e", q=4)
            kb.op("dve", lambda e: e.tensor_reduce(out=sm[:, 0:4], in_=plv, axis=AX.X, op=ALU.max), reads=[plr], writes=[sm_r])
            self.tt(ex, plv, sm[:, 0:4].unsqueeze(2).broadcast_to([128, 4, NE]), ALU.subtract, reads=[plr, sm_r], writes=[ex_r])
            self.act(ex, ex, AF.Exp, reads=[ex_r], writes=[ex_r])
            kb.op("dve", lambda e: e.tensor_reduce(out=sm[:, 4:8], in_=ex, axis=AX.X, op=ALU.add), reads=[ex_r], writes=[sm_r])
            kb.op("dve", lambda e: e.reciprocal(out=sm[:, 8:12], in_=sm[:, 4:8]), reads=[sm_r], writes=[sm_r])
            self.tt(aff[:, t4, :], ex, sm[:, 8:12].unsqueeze(2).broadcast_to([128, 4, NE]), ALU.mult, reads=[ex_r, sm_r], writes=[aff_r])
            self.unpin(plr)
            self.cp("dve", affhl[:, t4, :, 0], aff[:, t4, :], reads=[aff_r], writes=[affhl_r])
            self.tt(ex, aff[:, t4, :], affhl[:, t4, :, 0], ALU.subtract, reads=[aff_r, affhl_r], writes=[ex_r])
            self.cp("dve", affhl[:, t4, :, 1], ex, reads=[ex_r], writes=[affhl_r])
            for q in range(4):
                tt_ = b * 4 + q
                qs = slice(q * 128, (q + 1) * 128)
                self.tr(pT[0:NE, qs], aff[:, tt_, :], self.ident, reads=[aff_r, self.c_r], writes=[pTr])
                for half in range(2):
                    ph, phr = self.next_ps()
                    for mm_ in range(4):
                        m = half * 4 + mm_
                        self.tr(ph[:, mm_ * 128:(mm_ + 1) * 128], hf[:, m, qs], self.ident, reads=[hf_r, self.c_r], writes=[phr])
                    self.cp("act" if half == 0 else "dve", h2tok[:, tt_, half * 512:(half + 1) * 512], ph[:, :], reads=[phr], writes=[h2tok_r])
            self.cp("act", affT[0:NE, b * 512:(b + 1) * 512], pT[0:NE, :], reads=[pTr], writes=[affT_r])
            self.unpin(pTr)

        norm_b(0)
        for b in range(NB):
            if b + 1 < NB:
                norm_b(b + 1)
            route_b(b)
        ar.release(mk1)
        S = g.S
        work, work_r = ar.alloc("work", [S], F32)
        vals, vals_r = ar.alloc("vals", [C], F32)
        mask, mask_r = ar.alloc("mask", [S], F32)
        cum, cum_r = ar.alloc("cum", [S], F32)
        zer, zer_r = ar.alloc("zer", [S], F32)
        pgT, pgT_r = ar.alloc("pgT", [T], F32)
        kb.op("dve", lambda e: e.memset(zer[0:NE, :], 0.0), writes=[zer_r])
        ada_next = None
        if g.lat and l + 1 < self.depth:
            wb = [ar.alloc(f"adaw{i}", [KC, 1024], BF16) for i in range(2)]
            ada_next = self.adaln_issue(l + 1, wb)
        for s in range(g.nseq):
            ss = slice(s * S, (s + 1) * S)
            self.cp("dve", work[0:NE, :], affT[0:NE, ss], reads=[affT_r], writes=[work_r])
            for r in range(C // 8):
                kb.op("dve", lambda e: e.max(out=vals[0:NE, r * 8:(r + 1) * 8], in_=work[0:NE, :]), reads=[work_r], writes=[vals_r])
                if r < C // 8 - 1:
                    kb.op("dve", lambda e: e.match_replace(out=work[0:NE, :], in_to_replace=vals[0:NE, r * 8:(r + 1) * 8],
                                                           in_values=work[0:NE, :], imm_value=-1.0), reads=[work_r, vals_r], writes=[work_r])
            self.ts(mask[0:NE, :], affT[0:NE, ss], vals[0:NE, C - 1:C], None, ALU.is_ge, None, reads=[affT_r, vals_r], writes=[mask_r])
            kb.op("dve", lambda e: e.tensor_tensor_scan(out=cum[0:NE, :], data0=mask[0:NE, :], data1=zer[0:NE, :], initial=0.0,
                                                        op0=ALU.add, op1=ALU.add), reads=[mask_r, zer_r], writes=[cum_r])
            self.stt(mask[0:NE, :], cum[0:NE, :], float(C), mask[0:NE, :], ALU.is_le, ALU.mult, reads=[cum_r, mask_r], writes=[mask_r])
            self.ts(cum[0:NE, :], cum[0:NE, :], float(s * C - 1), None, ALU.add, None, reads=[cum_r], writes=[cum_r])
            self.tt(cum[0:NE, :], cum[0:NE, :], mask[0:NE, :], ALU.mult, reads=[cum_r, mask_r], writes=[cum_r])
            self.ts(mask[0:NE, :], mask[0:NE, :], 4096.0, -4096.0, ALU.mult, ALU.add, reads=[mask_r], writes=[mask_r])
            self.tt(pgT[0:NE, ss], cum[0:NE, :], mask[0:NE, :], ALU.add, reads=[cum_r, mask_r], writes=[pgT_r])
        pp, ppr = self.next_ps()
        for tt_ in range(NT):
            self.tr(pp[:, tt_ * NE:(tt_ + 1) * NE], pgT[0:NE, tt_ * 128:(tt_ + 1) * 128], self.ident[0:NE, 0:NE], reads=[pgT_r, self.c_r], writes=[ppr])
        self.cp("dve", posg, pp[:, 0:NT * NE].rearrange("p (t e) -> p t e", e=NE), reads=[ppr], writes=[posg_r])
        if ada_next is not None:
            self.adaln_finish(l + 1, *ada_next)
        self.dbg("aff", aff, aff_r)
        self.dbg("posg", posg, posg_r)
        self.dbg("h2tok", h2tok, h2tok_r)
        self.dbg("affT", affT[0:NE, :], affT_r)
        self.dbg("pgT", pgT[0:NE, :], pgT_r)
        self.dbg("vals", vals[0:NE, :], vals_r)
        ar.release(mk0)
        wslots = []
        NWS = 2 if (g.lat or "L" in self.groups) else 4
        CG = 1 if g.lat else 8
        NBUF = 2 if g.lat else 12
        for i in range(NWS):
            a = ar.alloc(f"wg{i}", [KC, FF], BF16)
            b_ = ar.alloc(f"wu{i}", [KC, FF], BF16)
            c_ = ar.alloc(f"wd{i}", [4, D], BF16)
            wslots.append((a, b_, c_))
        Sel = [ar.alloc(f"Sel{i}", [NT, NS], BF16) for i in range(2)]
        SelT = [ar.alloc(f"SelT{i}", [NCC, T], BF16) for i in range(NBUF)]
        XG = [ar.alloc(f"xg{i}", [KC, NS], BF16) for i in range(2)]
        sg, sg_r = ar.alloc("sg", [NS], F32)
        hid, hid_r = ar.alloc("hid", [4, NS], BF16)
        YO = [ar.alloc(f"yo{i}", [NCC, D], BF16) for i in range(NBUF)]
        GSL = [ar.alloc(f"gsl{i}", [4], F32) for i in range(2)]

        use_scr = (not g.lat) and ("L" in self.groups)

        def load_w(e):
            (wg, wg_r), (wu, wu_r), (wd, wd_r) = wslots[e % NWS]
            if use_scr:
                sc = self.wscr_d[l, e]
                kb.dma("sp", [(wg.rearrange("p a b -> p (a b)"), sc[:, 0:4096])], f"d_m{e % NWS}a", reads=[self.wscr_rs[l]], writes=[wg_r])
                kb.dma("sp", [(wu.rearrange("p a b -> p (a b)"), sc[:, 4096:8192])], f"d_m{e % NWS}b", reads=[self.wscr_rs[l]], writes=[wu_r])
                kb.dma("sp", [(wd.rearrange("p a b -> p (a b)"), sc[:, 8192:12288])], f"d_m{e % NWS}c", reads=[self.wscr_rs[l]], writes=[wd_r])
                return
            self.loadw(wg, self.wg_d[l, e].rearrange("(k p) n -> p k n", p=128), wg_r, f"d_m{e % NWS}a")
            self.loadw(wu, self.wu_d[l, e].rearrange("(k p) n -> p k n", p=128), wu_r, f"d_m{e % NWS}b")
            self.loadw(wd, self.wd_d[l, e].rearrange("(k p) n -> p k n", p=128), wd_r, f"d_m{e % NWS}c")
            if g.lat:
                sc = self.wscr_d[l, e]
                kb.dma("sp", [(sc[:, 0:4096], wg.rearrange("p a b -> p (a b)")),
                              (sc[:, 4096:8192], wu.rearrange("p a b -> p (a b)")),
                              (sc[:, 8192:12288], wd.rearrange("p a b -> p (a b)"))], "d_ws",
                       reads=[wg_r, wu_r, wd_r], writes=[self.wscr_rs[l]])

        def s1_sel(e):
            sel, sel_r = Sel[e % 2]
            for tt_ in range(NT):
                self.ts(sel[:, tt_, :], self.iotab[:, 0:NS], posg[:, tt_, e:e + 1], None, ALU.is_equal, None,
                        reads=[posg_r, self.c_r], writes=[sel_r])

        def s1_rest(e, comb=None):
            sel, sel_r = Sel[e % 2]
            selT, selT_r = SelT[e % NBUF]
            xg, xg_r = XG[e % 2]
            gsl, gsl_r = GSL[e % 2]
            for m in range(KC):
                if m % 2 == 0:
                    pgm, pgmr = self.next_ps(pin=True)
                o = pgm[:, (m % 2) * 256:(m % 2) * 256 + NS]
                for tt_ in range(NT):
                    self.mm(o, h2tok[:, tt_, m * 128:(m + 1) * 128], sel[:, tt_, :], tt_ == 0, tt_ == NT - 1, reads=[h2tok_r, sel_r], writes=[pgmr])
                if comb is not None:
                    for _ in range((NB * KC + KC - 1) // KC):
                        next(comb, None)
                if m % 2 == 1:
                    self.cp("act", xg[:, m - 1:m + 1, :],
                            pgm[:, :].rearrange("p (a b) -> p a b", a=2)[:, :, 0:NS], reads=[pgmr], writes=[xg_r])
                    self.unpin(pgmr)
            if comb is not None:
                for _ in comb:
                    pass
            pg_, pg_r = self.next_ps()
            for cc in range(NCC):
                for tt_ in range(NT):
                    self.mm(pg_[:, 2 * cc:2 * cc + 2], sel[:, tt_, cc * 128:(cc + 1) * 128], affhl[:, tt_, e, :], tt_ == 0, tt_ == NT - 1,
                            reads=[sel_r, affhl_r], writes=[pg_r])
            for cc in range(NCC):
                kb.op("dve", lambda en: en.tensor_reduce(out=gsl[:, cc:cc + 1], in_=pg_[:, 2 * cc:2 * cc + 2], axis=AX.X, op=ALU.add), reads=[pg_r], writes=[gsl_r])
            for cc in range(NCC):
                for t0 in range(0, NT, 8):
                    pt, ptr = self.next_ps()
                    pbt = pt[:, :].bitcast(BF16)
                    for q in range(8):
                        self.tr(pbt[:, q * 128:(q + 1) * 128], sel[:, t0 + q, cc * 128:(cc + 1) * 128], self.identb, reads=[sel_r, self.c_r], writes=[ptr])
                    self.cp("act", selT[:, cc, t0 * 128:(t0 + 8) * 128], pbt[:, :], reads=[ptr], writes=[selT_r])

        def s2a(e):
            (wg, wg_r), (wu, wu_r), (wd, wd_r) = wslots[e % NWS]
            xg, xg_r = XG[e % 2]
            for f in range(4):
                pf, pfr = self.next_ps()
                for k in range(KC):
                    self.mm(pf[:, 0:NS], wg[:, k, f * 128:(f + 1) * 128], xg[:, k, :], k == 0, k == KC - 1, reads=[wg_r, xg_r], writes=[pfr])
                for k in range(KC):
                    self.mm(pf[:, 256:256 + NS], wu[:, k, f * 128:(f + 1) * 128], xg[:, k, :], k == 0, k == KC - 1, reads=[wu_r, xg_r], writes=[pfr])
                self.act(sg, pf[:, 0:NS], AF.Silu, reads=[pfr], writes=[sg_r])
                self.tt(hid[:, f, :], sg, pf[:, 256:256 + NS], ALU.mult, reads=[sg_r, pfr], writes=[hid_r])

        def s2b(e):
            (wg, wg_r), (wu, wu_r), (wd, wd_r) = wslots[e % NWS]
            yo, yo_r = YO[e % NBUF]
            gsl, gsl_r = GSL[e % 2]
            for cc in range(NCC):
                for db in range(2):
                    py, pyr = self.next_ps()
                    for f in range(4):
                        self.mm(py[:, :], hid[:, f, cc * 128:(cc + 1) * 128], wd[:, f, db * 512:(db + 1) * 512], f == 0, f == 3, reads=[hid_r, wd_r], writes=[pyr])
                    self.act(yo[:, cc, db * 512:(db + 1) * 512], py[:, :], AF.Copy, reads=[pyr, gsl_r], writes=[yo_r], scale=gsl[:, cc:cc + 1])

        def s3(es):
            for b in range(NB):
                tb = slice(b * 512, (b + 1) * 512)
                for m in range(KC):
                    pc, pcr = self.next_ps()
                    n = len(es) * NCC
                    i_ = 0
                    for e in es:
                        yo, yo_r = YO[e % NBUF]
                        selT, selT_r = SelT[e % NBUF]
                        for cc in range(NCC):
                            self.mm(pc[:, :], yo[:, cc, m * 128:(m + 1) * 128], selT[:, cc, tb], i_ == 0, i_ == n - 1, reads=[yo_r, selT_r], writes=[pcr])
                            i_ += 1
                    self.resid(b, m, tb, pc[:, :], pcr, self.gate(l, 1, m, ci))
                    yield

        load_w(0)
        s1_sel(0)
        s1_rest(0)
        for i in range(1, min(NWS - 1, NE)):
            load_w(i)
        for i in range(NE):
            if i + NWS - 1 < NE:
                load_w(i + NWS - 1)
            if i + 1 < NE:
                s1_sel(i + 1)
            s2a(i)
            comb = s3(list(range(i - CG, i))) if (i >= CG and i % CG == 0) else None
            if i + 1 < NE:
                s1_rest(i + 1, comb)
            elif comb is not None:
                for _ in comb:
                    pass
            s2b(i)
        for _ in s3(list(range(NE - CG, NE))):
            pass


def _fm(v):
    v = np.asarray(v, np.float32)
    lead = v.shape[:-1]
    n = v.shape[-1] // 128
    return np.ascontiguousarray(np.moveaxis(v.reshape(*lead, n, 128), -1, 0))


def _rope_tables(n_tokens, rot_dim, grid_w=64, theta=10000.0):
    rows = n_tokens // grid_w
    row = np.repeat(np.arange(rows), grid_w).astype(np.float32)
    col = np.tile(np.arange(grid_w), rows).astype(np.float32)
    axis_dim = rot_dim // 2
    inv = (np.float32(theta) ** (-np.arange(0, axis_dim, 2, dtype=np.float32) / np.float32(axis_dim))).astype(np.float32)
    ang = np.concatenate([row[:, None] * inv, col[:, None] * inv], axis=-1).astype(np.float32)
    return np.cos(ang).astype(np.float32), np.sin(ang).astype(np.float32), inv


def _consts():
    c = np.zeros((128, 512), np.float32)
    c[:, 0:128] = np.eye(128, dtype=np.float32)
    c[:, 128:256] = 1.0 / 1024
    c[:, 256:512] = np.arange(256, dtype=np.float32)[None, :]
    return c


def _pool_rc(S):
    out = np.zeros((128, 4, 16), np.float32)
    for gi, win in enumerate((2, 4, 8, 16)):
        t = np.concatenate([np.arange(8), np.arange(S - 8, S)])
        lo = np.clip(t - win // 2, 0, S)
        hi = np.clip(t + win // 2, 0, S)
        out[:, gi, :] = (1.0 / (hi - lo).astype(np.float32))[None, :]
    return out


_PROG_CACHE = {}


def _get_prog(depth=4, **kw):
    if depth not in _PROG_CACHE:
        p = Prog(depth, **kw)
        p.build()
        _PROG_CACHE[depth] = p
    return _PROG_CACHE[depth]


def make_in_maps(inp, cores=range(NCORES)):
    f32 = lambda a: np.ascontiguousarray(np.asarray(a, np.float32))
    L = 4
    shared = {}
    shared["ada_w"] = f32(inp["ada_w"])
    shared["ada_b"] = f32(np.moveaxis(np.asarray(inp["ada_b"], np.float32).reshape(L, 48, 128), -1, 0))
    shared["g1"] = _fm(inp["norm1_g"])
    shared["g2"] = _fm(inp["norm2_g"])
    shared["gf"] = _fm(inp["final_g"])
    shared["cst"] = _consts()
    shared["pool_w"] = f32(inp["pool_w"][0])
    shared["pool_s"] = _fm(inp["pool_scale"][0])
    shared["mla_wd"] = f32(inp["mla_w_down"][0])
    shared["mla_gq"] = f32(np.broadcast_to(np.asarray(inp["mla_g_q"][0], np.float32)[None, :], (128, 384)))
    wuq = np.asarray(inp["mla_w_uq"][0], np.float32).reshape(384, 8, 192)
    rope = wuq[:, :, 128:192]
    swapped = np.concatenate([rope[:, :, 32:64], rope[:, :, 0:32]], axis=-1)
    shared["mla_wuq"] = f32(np.concatenate([wuq[:, :, 0:128], rope, swapped, rope], axis=-1).reshape(384, 2560))
    shared["mla_gkv"] = f32(np.broadcast_to(np.asarray(inp["mla_g_kv"][0], np.float32)[None, :], (128, 256)))
    shared["mla_wukv"] = f32(inp["mla_w_ukv"][0])
    shared["mla_wo"] = f32(inp["mla_w_o"][0])
    cos, sin, _ = _rope_tables(2048, 64)
    rtok = np.stack([cos, sin], 0).reshape(2, 16, 128, 32).transpose(2, 0, 1, 3)
    shared["mla_rtok"] = f32(rtok)
    rows = np.arange(32, dtype=np.float32)
    cols = np.arange(64, dtype=np.float32)
    inv = _rope_tables(2048, 64)[2]
    rA = np.ones((64, 2, 32), np.float32)
    rB = np.ones((64, 2, 64), np.float32)
    for i in range(64):
        ii = i % 32
        sgn = -1.0 if i < 32 else 1.0
        if ii < 16:
            a = (rows * inv[ii]).astype(np.float32)
            rA[i, 0] = np.cos(a)
            rA[i, 1] = sgn * np.sin(a)
        else:
            a = (cols * inv[ii - 16]).astype(np.float32)
            rB[i, 0] = np.cos(a)
            rB[i, 1] = sgn * np.sin(a)
    shared["mla_rA"] = rA
    shared["mla_rB"] = rB
    shared["cv_w1"] = f32(inp["conv_w_pw1"][0])
    shared["cv_b1"] = _fm(inp["conv_b_pw1"][0])
    shared["cv_wdw"] = f32(np.asarray(inp["conv_w_dw"][0], np.float32).T.reshape(8, 128, 31).transpose(1, 0, 2))
    shared["cv_vec"] = f32(np.stack([_fm(inp["conv_b_dw"][0]), _fm(inp["conv_ln_g"][0]), _fm(inp["conv_ln_b"][0]),
                                     _fm(inp["conv_b_pw2"][0])], axis=1))
    shared["cv_w2"] = f32(inp["conv_w_pw2"][0])
    shared["gq_w"] = f32(inp["gqa_w_qkv"][0])
    shared["gq_g"] = f32(np.broadcast_to(np.stack([np.asarray(inp["gqa_g_q"][0], np.float32),
                                                    np.asarray(inp["gqa_g_k"][0], np.float32)], 0)[None], (128, 2, 128)))
    shared["gq_wo"] = f32(inp["gqa_w_o"][0])
    cos, sin, _ = _rope_tables(2048, 128)
    shared["gq_rtok"] = f32(np.stack([cos, sin], 0).reshape(2, 16, 128, 64).transpose(2, 0, 1, 3))
    shared["rw"] = f32(np.asarray(inp["router_w"], np.float32).reshape(L, 8, 128, NE).transpose(2, 0, 1, 3))
    shared["moe_wg"] = f32(inp["moe_w_gate"])
    shared["moe_wu"] = f32(inp["moe_w_up"])
    shared["moe_wd"] = f32(inp["moe_w_down"])
    shared["pool_rc"] = _pool_rc(2048)
    maps = []
    xs = np.asarray(inp["x_sample"], np.float32)
    xp = np.asarray(inp["x_prompt"], np.float32)
    cc = np.asarray(inp["c"], np.float32)
    cctx = np.asarray(inp["c_ctx"], np.float32)
    for i in cores:
        m = dict(shared)
        m["xl"] = f32(xs[i].T.reshape(8, 128, 2048).transpose(1, 0, 2))
        m["xc"] = f32(xp[4 * i:4 * i + 4].reshape(1024, 1024).T.reshape(8, 128, 1024).transpose(1, 0, 2))
        m["cond"] = f32(np.stack([_fm(cc[i]), _fm(cctx)], axis=-1))
        m["mla_cckv"] = f32(inp["cache_mla_ckv"][i, 0])
        m["mla_ckr"] = f32(inp["cache_mla_krope"][i, 0])
        m["gq_ck"] = f32(np.asarray(inp["cache_gqa_k"][i, 0]).reshape(512, 256))
        m["gq_cv"] = f32(np.asarray(inp["cache_gqa_v"][i, 0]).reshape(512, 256))
        maps.append(m)
    return maps


def kernel(**inputs):
    prog = _get_prog(4)
    maps = make_in_maps(inputs)
    res = run_bass_kernel_spmd(prog.nc, maps, core_ids=list(range(NCORES)))
    return assemble(res.results)


def assemble(results):
    n = len(results)
    y_prompt = np.zeros((4 * n, 256, D), np.float32)
    y_sample = np.zeros((n, 2048, D), np.float32)
    s_ckv = np.zeros((4 * n, 1, 256, 256), np.float32)
    s_kr = np.zeros((4 * n, 1, 256, 64), np.float32)
    s_k = np.zeros((4 * n, 1, 256, 2, 128), np.float32)
    s_v = np.zeros((4 * n, 1, 256, 2, 128), np.float32)
    for i, r in enumerate(results):
        y_sample[i] = r["yl"].transpose(1, 0, 2).reshape(D, 2048).T
        y_prompt[4 * i:4 * i + 4] = r["yc"].transpose(1, 0, 2).reshape(D, 1024).T.reshape(4, 256, D)
        s_ckv[4 * i:4 * i + 4, 0] = r["st_ckv"].reshape(4, 256, 256)
        s_kr[4 * i:4 * i + 4, 0] = r["st_kr"].reshape(4, 256, 64)
        s_k[4 * i:4 * i + 4, 0] = r["st_k"].reshape(4, 256, 2, 128)
        s_v[4 * i:4 * i + 4, 0] = r["st_v"].reshape(4, 256, 2, 128)
    return (y_prompt, y_sample, s_ckv, s_kr, s_k, s_v)
```
